# Optimizing a Trainium2 kernel written in Bass

```python
import jax
import jax.numpy as jnp
from jax import lax
import numpy as np

D_MODEL = 2048
BATCH = 2
SEQ = 4096
DEPTH = 2

D_MIX = D_MODEL
HEAD_DIM = 128
MLSTM_HEADS = D_MIX // 4 // HEAD_DIM
FOX_HEADS = D_MIX // 2 // HEAD_DIM
GDN_HEADS = D_MIX // 4 // HEAD_DIM
MLSTM_W = MLSTM_HEADS * HEAD_DIM
FOX_W = FOX_HEADS * HEAD_DIM
GDN_W = GDN_HEADS * HEAD_DIM
D_FF = -(-8 * D_MODEL // (3 * 256)) * 256
MLSTM_CHUNK = 64
FOX_BLOCK = 128
GDN_CHUNK = 64
CONV_WIDTH = 4
GATE_SOFTCAP = 15.0
NORM_EPS = 1e-6
SPLITS = (MLSTM_W, MLSTM_W, MLSTM_W, MLSTM_W, MLSTM_HEADS, MLSTM_HEADS,
          FOX_W, FOX_W, FOX_W, FOX_HEADS,
          GDN_W, GDN_W, GDN_W, GDN_W, GDN_HEADS, GDN_HEADS)
N_IN = 4 * MLSTM_W + 2 * MLSTM_HEADS + 3 * FOX_W + FOX_HEADS + 4 * GDN_W + 2 * GDN_HEADS

kernel_name = 'hymba_style_mlstm_fox_gdn_hybrid'


def rms_norm(x, w):
    xf = x.astype(jnp.float32)
    y = xf * lax.rsqrt(jnp.mean(xf * xf, axis=-1, keepdims=True) + NORM_EPS)
    return (y * w.astype(jnp.float32)).astype(x.dtype)


def split_columns(t, sizes):
    out = []
    off = 0
    for s in sizes:
        out.append(t[..., off:off + s])
        off += s
    return out


def to_heads(t, n_heads):
    b, s, _ = t.shape
    return t.reshape(b, s, n_heads, -1).transpose(0, 2, 1, 3)


def merge_heads(t):
    b, h, s, d = t.shape
    return t.transpose(0, 2, 1, 3).reshape(b, s, h * d)


def to_chunks(t, size):
    b, h, s = t.shape[:3]
    t = t.reshape(b, h, s // size, size, *t.shape[3:])
    return jnp.moveaxis(t, 2, 0)


def from_chunks(t):
    t = jnp.moveaxis(t, 0, 2)
    b, h, n, l, d = t.shape
    return t.reshape(b, h, n * l, d)


def head_rms_norm(t, w):
    y = t * lax.rsqrt(jnp.mean(t * t, axis=-1, keepdims=True) + NORM_EPS)
    return merge_heads(y) * w.astype(jnp.float32)


def head_layer_norm(t, w):
    mu = jnp.mean(t, axis=-1, keepdims=True)
    c = t - mu
    var = jnp.mean(c * c, axis=-1, keepdims=True)
    return merge_heads(c * lax.rsqrt(var + NORM_EPS)) * w.astype(jnp.float32)


def l2_normalize(t):
    return t * lax.rsqrt(jnp.sum(t * t, axis=-1, keepdims=True) + NORM_EPS)


def soft_cap(t):
    return GATE_SOFTCAP * jnp.tanh(t / GATE_SOFTCAP)


def causal_depthwise_conv(x, w):
    k, c = w.shape
    return lax.conv_general_dilated(x, w[:, None, :], window_strides=(1,), padding=[(k - 1, 0)],
                                    dimension_numbers=('NWC', 'WIO', 'NWC'), feature_group_count=c)


def mlstm_chunkwise(q, k, v, i_pre, f_pre):
    b, h, s, d = q.shape
    L = MLSTM_CHUNK
    k = k * (d ** -0.5)
    logf = jax.nn.log_sigmoid(f_pre)
    qc, kc, vc = to_chunks(q, L), to_chunks(k, L), to_chunks(v, L)
    ic = to_chunks(i_pre, L)
    bc = jnp.cumsum(to_chunks(logf, L), axis=-1)
    causal = jnp.tril(jnp.ones((L, L), dtype=bool))

    def step(carry, xs):
        C, nv, m = carry
        q_, k_, v_, i_, b_ = xs
        log_intra = jnp.where(causal, b_[..., :, None] - b_[..., None, :] + i_[..., None, :], -jnp.inf)
        log_inter = b_ + m[..., None]
        m_t = jnp.maximum(log_inter, jnp.max(log_intra, axis=-1))
        w_inter = jnp.exp(log_inter - m_t)
        p = jnp.exp(log_intra - m_t[..., None]) * jnp.einsum('bhtd,bhsd->bhts', q_, k_)
        num = w_inter[..., None] * jnp.einsum('bhtd,bhde->bhte', q_, C) + jnp.einsum('bhts,bhse->bhte', p, v_)
        den = w_inter * jnp.einsum('bhtd,bhd->bht', q_, nv) + jnp.sum(p, axis=-1)
        h_out = num / jnp.maximum(jnp.abs(den), jnp.exp(-m_t))[..., None]
        b_last = b_[..., -1]
        log_end = b_last[..., None] - b_ + i_
        m_new = jnp.maximum(b_last + m, jnp.max(log_end, axis=-1))
        w_state = jnp.exp(b_last + m - m_new)
        w_end = jnp.exp(log_end - m_new[..., None])
        C = w_state[..., None, None] * C + jnp.einsum('bhs,bhsd,bhse->bhde', w_end, k_, v_)
        nv = w_state[..., None] * nv + jnp.einsum('bhs,bhsd->bhd', w_end, k_)
        return (C, nv, m_new), h_out

    init = (jnp.zeros((b, h, d, d), q.dtype), jnp.zeros((b, h, d), q.dtype), jnp.zeros((b, h), q.dtype))
    _, hs = lax.scan(step, init, (qc, kc, vc, ic, bc))
    return from_chunks(hs)


def forgetting_attention(q, k, v, f_pre):
    b, h, s, d = q.shape
    nb = s // FOX_BLOCK
    c = jnp.cumsum(jax.nn.log_sigmoid(f_pre), axis=-1)
    qb = to_chunks(q * (d ** -0.5), FOX_BLOCK)
    cb = to_chunks(c, FOX_BLOCK)
    starts = jnp.arange(nb, dtype=jnp.int32) * FOX_BLOCK
    kpos = jnp.arange(s, dtype=jnp.int32)

    def block(args):
        q_, c_, start = args
        qpos = start + jnp.arange(FOX_BLOCK, dtype=jnp.int32)
        logits = jnp.einsum('bhqd,bhkd->bhqk', q_, k) + (c_[..., :, None] - c[:, :, None, :])
        logits = jnp.where(kpos[None, :] <= qpos[:, None], logits, -jnp.inf)
        p = jax.nn.softmax(logits, axis=-1)
        return jnp.einsum('bhqk,bhkd->bhqd', p, v)

    return from_chunks(lax.map(block, (qb, cb, starts)))


def gated_delta_rule(q, k, v, g, beta):
    b, h, s, d = q.shape
    L = GDN_CHUNK
    qc, kc, vc = to_chunks(q * (d ** -0.5), L), to_chunks(k, L), to_chunks(v, L)
    gc = jnp.cumsum(to_chunks(g, L), axis=-1)
    bc = to_chunks(beta, L)
    incl = jnp.tril(jnp.ones((L, L), dtype=bool))
    strict = jnp.tril(jnp.ones((L, L), dtype=bool), -1)
    diff = gc[..., :, None] - gc[..., None, :]
    decay = jnp.where(incl, jnp.exp(jnp.where(incl, diff, 0.0)), 0.0)
    kb = kc * bc[..., None]
    a_mat = jnp.where(strict, jnp.einsum('nbhtd,nbhsd->nbhts', kb, kc) * decay, 0.0)
    lhs = a_mat + jnp.eye(L, dtype=a_mat.dtype)
    w_val = lax.linalg.triangular_solve(lhs, vc * bc[..., None], left_side=True, lower=True, unit_diagonal=True)
    w_key = lax.linalg.triangular_solve(lhs, kb * jnp.exp(gc)[..., None], left_side=True, lower=True, unit_diagonal=True)
    attn = jnp.einsum('nbhtd,nbhsd->nbhts', qc, kc) * decay
    q_dec = qc * jnp.exp(gc)[..., None]
    g_last = gc[..., -1]
    k_end = kc * jnp.exp(g_last[..., None] - gc)[..., None]

    def step(state, xs):
        wv, wk, at, qd, ke, gl = xs
        u = wv - jnp.einsum('bhtd,bhde->bhte', wk, state)
        o = jnp.einsum('bhtd,bhde->bhte', qd, state) + jnp.einsum('bhts,bhse->bhte', at, u)
        state = jnp.exp(gl)[..., None, None] * state + jnp.einsum('bhsd,bhse->bhde', ke, u)
        return state, o

    _, os_ = lax.scan(step, jnp.zeros((b, h, d, d), q.dtype), (w_val, w_key, attn, q_dec, k_end, g_last))
    return from_chunks(os_)


def hybrid_mixer(h, w_in, mlstm_i_bias, mlstm_f_bias, fox_f_bias, gdn_conv_w, gdn_a_log, gdn_dt_bias,
                 mlstm_out_norm_w, fox_out_norm_w, gdn_out_norm_w, w_out):
    f32 = jnp.float32
    proj = (h @ w_in).astype(f32)
    (mq, mk, mv, mo, mi, mf, fq, fk, fv, ff, gq, gk, gv, gz, ga, gb) = split_columns(proj, SPLITS)

    mi = soft_cap(mi + mlstm_i_bias.astype(f32)).transpose(0, 2, 1)
    mf = soft_cap(mf + mlstm_f_bias.astype(f32)).transpose(0, 2, 1)
    hm = mlstm_chunkwise(to_heads(mq, MLSTM_HEADS), to_heads(mk, MLSTM_HEADS), to_heads(mv, MLSTM_HEADS), mi, mf)
    y_m = head_layer_norm(hm, mlstm_out_norm_w) * jax.nn.sigmoid(mo)

    hf = forgetting_attention(to_heads(fq, FOX_HEADS), to_heads(fk, FOX_HEADS), to_heads(fv, FOX_HEADS),
                              (ff + fox_f_bias.astype(f32)).transpose(0, 2, 1))
    y_f = head_rms_norm(hf, fox_out_norm_w)

    qkv = jax.nn.silu(causal_depthwise_conv(jnp.concatenate([gq, gk, gv], axis=-1), gdn_conv_w.astype(f32)))
    gq, gk, gv = split_columns(qkv, (GDN_W, GDN_W, GDN_W))
    g = (-jnp.exp(gdn_a_log.astype(f32)) * jax.nn.softplus(ga + gdn_dt_bias.astype(f32))).transpose(0, 2, 1)
    beta = jax.nn.sigmoid(gb).transpose(0, 2, 1)
    hg = gated_delta_rule(l2_normalize(to_heads(gq, GDN_HEADS)), l2_normalize(to_heads(gk, GDN_HEADS)),
                          to_heads(gv, GDN_HEADS), g, beta)
    y_g = head_rms_norm(hg, gdn_out_norm_w) * jax.nn.silu(gz)

    y = jnp.concatenate([y_m, y_f, y_g], axis=-1).astype(h.dtype)
    return y @ w_out


def swiglu(h, w_gate, w_up, w_down):
    return (jax.nn.silu(h @ w_gate) * (h @ w_up)) @ w_down


def setup_inputs(seed: int = 0) -> dict:
    key = jax.random.key(seed)
    ks = jax.random.split(key, 18)
    f32 = jnp.float32
    nrm = lambda k, shape, scale: jax.random.normal(k, shape, f32) * scale
    gain = lambda k, shape: 1.0 + 0.02 * jax.random.normal(k, shape, f32)
    dt = jnp.exp(jax.random.uniform(ks[7], (DEPTH, GDN_HEADS), f32, np.log(1e-3), np.log(1e-1)))
    return {
        'x': jax.random.normal(ks[0], (BATCH, SEQ, D_MODEL), f32),
        'mix_norm_w': gain(ks[1], (DEPTH, D_MODEL)),
        'w_in': nrm(ks[2], (DEPTH, D_MODEL, N_IN), D_MODEL ** -0.5),
        'mlstm_i_bias': nrm(ks[3], (DEPTH, MLSTM_HEADS), 0.1),
        'mlstm_f_bias': jax.random.uniform(ks[4], (DEPTH, MLSTM_HEADS), f32, 3.0, 6.0),
        'fox_f_bias': jax.random.uniform(ks[5], (DEPTH, FOX_HEADS), f32, 2.0, 5.0),
        'gdn_conv_w': nrm(ks[6], (DEPTH, CONV_WIDTH, 3 * GDN_W), CONV_WIDTH ** -0.5),
        'gdn_a_log': jnp.log(jax.random.uniform(ks[8], (DEPTH, GDN_HEADS), f32, 1.0, 16.0)),
        'gdn_dt_bias': dt + jnp.log(-jnp.expm1(-dt)),
        'mlstm_out_norm_w': gain(ks[9], (DEPTH, MLSTM_W)),
        'fox_out_norm_w': gain(ks[10], (DEPTH, FOX_W)),
        'gdn_out_norm_w': gain(ks[11], (DEPTH, GDN_W)),
        'w_out': nrm(ks[12], (DEPTH, D_MIX, D_MODEL), D_MIX ** -0.5),
        'ffn_norm_w': gain(ks[13], (DEPTH, D_MODEL)),
        'w_gate': nrm(ks[14], (DEPTH, D_MODEL, D_FF), D_MODEL ** -0.5),
        'w_up': nrm(ks[15], (DEPTH, D_MODEL, D_FF), D_MODEL ** -0.5),
        'w_down': nrm(ks[16], (DEPTH, D_FF, D_MODEL), D_FF ** -0.5),
        'final_norm_w': gain(ks[17], (D_MODEL,)),
    }


def reference(x, mix_norm_w, w_in, mlstm_i_bias, mlstm_f_bias, fox_f_bias, gdn_conv_w, gdn_a_log, gdn_dt_bias,
              mlstm_out_norm_w, fox_out_norm_w, gdn_out_norm_w, w_out, ffn_norm_w, w_gate, w_up, w_down,
              final_norm_w):
    for l in range(DEPTH):
        h = rms_norm(x, mix_norm_w[l])
        x = x + hybrid_mixer(h, w_in[l], mlstm_i_bias[l], mlstm_f_bias[l], fox_f_bias[l], gdn_conv_w[l],
                             gdn_a_log[l], gdn_dt_bias[l], mlstm_out_norm_w[l], fox_out_norm_w[l],
                             gdn_out_norm_w[l], w_out[l]).astype(x.dtype)
        h = rms_norm(x, ffn_norm_w[l])
        x = x + swiglu(h, w_gate[l], w_up[l], w_down[l]).astype(x.dtype)
    return rms_norm(x, final_norm_w)
```

```python
from collections import defaultdict
import numpy as np
import ml_dtypes
import concourse.bass as bass
from concourse.bass_utils import run_bass_kernel_spmd
import concourse.mybir as mybir

F32 = mybir.dt.float32
BF16 = mybir.dt.bfloat16
AF = mybir.ActivationFunctionType
ALU = mybir.AluOpType
AX = mybir.AxisListType

ENGS = ("pe", "act", "dve", "pool", "sp")
NDMASEM = 32
NPOOLSEM = 8
NCCSEM = 48


class Sched:
    def __init__(self, nc, same_engine_sync=True):
        self.nc = nc
        self.same = same_engine_sync
        self.ops = {e: [] for e in ENGS}
        self.seen = {e: {} for e in ENGS}
        self.w = {}
        self.r = {}
        self.kids = defaultdict(set)
        self.dma_uses = [0] * NDMASEM
        self.dma_last = [None] * NDMASEM
        self.dma_rr = 0
        self.dma_rr_pool = 0
        self.ncc = 0

    def _related(self, key):
        ks = []
        for i in range(1, len(key) + 1):
            p = key[:i]
            if p in self.w or p in self.r:
                ks.append(p)
        for c in self.kids.get(key, ()):
            if c != key and (c in self.w or c in self.r):
                ks.append(c)
        return ks

    def _reg(self, key):
        for i in range(1, len(key)):
            self.kids[key[:i]].add(key)

    def op(self, eng, fn, reads=(), writes=(), dma=False):
        reads = [k if isinstance(k, tuple) else (k,) for k in reads]
        writes = [k if isinstance(k, tuple) else (k,) for k in writes]
        deps = set()
        for k in reads:
            for rk in self._related(k):
                if rk in self.w:
                    deps.add(self.w[rk])
        for k in writes:
            for rk in self._related(k):
                if rk in self.w:
                    deps.add(self.w[rk])
                for t in self.r.get(rk, ()):
                    deps.add(t)
        idx = len(self.ops[eng])
        if dma == "cc":
            tok = (("cc", self.ncc), 1)
            dmainfo = self.ncc
            self.ncc += 1
            assert self.ncc <= NCCSEM
        elif dma:
            if eng == "pool":
                j = self.dma_rr_pool
                self.dma_rr_pool = (j + 1) % NPOOLSEM
            else:
                j = NPOOLSEM + self.dma_rr
                self.dma_rr = (self.dma_rr + 1) % (NDMASEM - NPOOLSEM)
            if self.dma_last[j] is not None:
                deps.add(self.dma_last[j])
            self.dma_uses[j] += 1
            tok = (("dma", j), self.dma_uses[j])
            self.dma_last[j] = tok
            dmainfo = j
        else:
            tok = (eng, idx)
            dmainfo = None
        waits = {}
        for (sk, v) in deps:
            if sk == eng:
                if eng == "pe" or not self.same:
                    continue
            if self.seen[eng].get(sk, -1) >= v:
                continue
            if waits.get(sk, -1) < v:
                waits[sk] = v
        for sk, v in waits.items():
            self.seen[eng][sk] = v
            if not isinstance(sk, tuple):
                self.ops[sk][v][3] = True
        self.ops[eng].append([list(waits.items()), fn, dma, False, dmainfo])
        for k in writes:
            for rk in self._related(k):
                if len(rk) > len(k):
                    self.w.pop(rk, None)
                    self.r.pop(rk, None)
            self.w[k] = tok
            self.r[k] = []
            self._reg(k)
        for k in reads:
            self.r.setdefault(k, []).append(tok)
            self._reg(k)
        return tok

    def pe(self, fn, reads=(), writes=()):
        return self.op("pe", fn, reads, writes)

    def act(self, fn, reads=(), writes=()):
        return self.op("act", fn, reads, writes)

    def dve(self, fn, reads=(), writes=()):
        return self.op("dve", fn, reads, writes)

    def pool(self, fn, reads=(), writes=()):
        return self.op("pool", fn, reads, writes)

    def dma(self, q, fn, reads=(), writes=()):
        return self.op(q, fn, reads, writes, dma=True)

    def cc(self, fn, reads=(), writes=()):
        return self.op("pool", fn, reads, writes, dma="cc")

    def emit(self, final_wait_tokens=()):
        nc = self.nc
        import contextlib
        with contextlib.ExitStack() as st:
            esem = {e: st.enter_context(nc.semaphore("s_" + e)) for e in ENGS}
            dsem = [st.enter_context(nc.semaphore("d%d" % j)) for j in range(NDMASEM)]
            ccsem = [st.enter_context(nc.semaphore("cc%d" % j)) for j in range(self.ncc)]
            block = st.enter_context(nc.Block())
            rank = {}
            for e in ENGS:
                c = 0
                rk = []
                for o in self.ops[e]:
                    if o[3] and not o[2]:
                        c += 1
                    rk.append(c)
                rank[e] = rk

            def semval(sk, v):
                if isinstance(sk, tuple):
                    if sk[0] == "cc":
                        return ccsem[sk[1]], v
                    return dsem[sk[1]], 16 * v
                return esem[sk], rank[sk][v]

            def run(e, engobj):
                for (waits, fn, isdma, marked, dmainfo) in self.ops[e]:
                    for sk, v in waits:
                        s, val = semval(sk, v)
                        engobj.wait_ge(s, val)
                    ins = fn(engobj)
                    if isdma == "cc":
                        ins.then_inc(ccsem[dmainfo])
                    elif isdma:
                        ins.then_inc(dsem[dmainfo], 16)
                    elif marked:
                        ins.then_inc(esem[e], 1)
                if e == "sp":
                    for j in range(NDMASEM):
                        if self.dma_uses[j] > 0:
                            engobj.wait_ge(dsem[j], 16 * self.dma_uses[j])

            @block.tensor
            def _(eng):
                run("pe", eng)

            @block.scalar
            def _(eng):
                run("act", eng)

            @block.vector
            def _(eng):
                run("dve", eng)

            @block.gpsimd
            def _(eng):
                run("pool", eng)

            @block.sync
            def _(eng):
                run("sp", eng)


class Big:
    def __init__(self, A, n, width=4264):
        self.t = [A.sb([128, width], F32) for _ in range(n)]

    def v(self, i, a, b=None):
        if b is None:
            return self.t[i][:, 0:a]
        return self.t[i][:, 0:a * b].rearrange("p (a b) -> p a b", a=a)


class Alloc:
    def __init__(self, nc, st):
        self.nc = nc
        self.st = st
        self.n = 0
        self.cache = {}
        self.tag = None
        self.cnt = 0

    def begin(self, tag):
        self.tag = tag
        self.cnt = 0

    def end(self):
        self.tag = None

    def sb(self, shape, dt):
        if self.tag is not None:
            key = (self.tag, self.cnt)
            self.cnt += 1
            if key in self.cache:
                return self.cache[key]
        self.n += 1
        t = self.st.enter_context(self.nc.sbuf_tensor("sb%d" % self.n, list(shape), dt))
        if self.tag is not None:
            self.cache[key] = t
        return t

    def ps(self, shape, dt):
        self.n += 1
        return self.st.enter_context(self.nc.psum_tensor("ps%d" % self.n, list(shape), dt))


D_MODEL = 2048
N_IN = 7192
D_FF = 5632
EPS = 1e-6
import contextlib


def rms_stage(S, A, xt_ap, key_x, ss, rs, junk, nwB, hb, key_hb, D=D_MODEL):
    S.act(lambda e: e.activation(out=junk[:], in_=xt_ap, func=AF.Square, accum_out=ss[:]),
          reads=[key_x], writes=[("junk",), ("ss",)])
    S.dve(lambda e: e.tensor_scalar(out=rs[:], in0=ss[:], scalar1=1.0 / D, scalar2=EPS, op0=ALU.mult, op1=ALU.add),
          reads=[("ss",)], writes=[("rs",)])
    S.act(lambda e: e.activation(out=rs[:], in_=rs[:], func=AF.Sqrt), reads=[("rs",)], writes=[("rs",)])
    S.dve(lambda e: e.reciprocal(out=rs[:], in_=rs[:]), reads=[("rs",)], writes=[("rs",)])
    S.dve(lambda e: e.scalar_tensor_tensor(out=hb, in0=xt_ap, scalar=rs[:], in1=nwB[:], op0=ALU.mult, op1=ALU.mult),
          reads=[key_x, ("rs",), ("nwB",)], writes=[key_hb])


def build_k1(TOK=1024, D=D_MODEL, N=N_IN, NCT=None, TEV=2, NTL=None):
    nc = bass.Bass("TRN2", target_bir_lowering=False)
    x = nc.dram_tensor("x", [TOK, D], F32, kind="ExternalInput").ap()
    nw = nc.dram_tensor("nw", [D], F32, kind="ExternalInput").ap()
    w = nc.dram_tensor("w", [D, N], F32, kind="ExternalInput").ap()
    identd = nc.dram_tensor("ident", [128, 128], F32, kind="ExternalInput").ap()
    out = nc.dram_tensor("proj", [TOK, N], F32, kind="ExternalOutput").ap()
    NT = TOK // 128 if NTL is None else NTL
    KC = D // 128
    S = Sched(nc)
    with contextlib.ExitStack() as st:
        A = Alloc(nc, st)
        nwB = A.sb([128, D], F32)
        identf = A.sb([128, 128], F32)
        identb = A.sb([128, 128], BF16)
        xt = [A.sb([128, D], F32) for _ in range(2)]
        hb = [A.sb([128, D], BF16) for _ in range(2)]
        junk = A.sb([128, D], BF16)
        ss = A.sb([128, 1], F32)
        rs = A.sb([128, 1], F32)
        hT = A.sb([128, NT, KC, 128], BF16)
        wt = [A.sb([128, KC, 512], BF16) for _ in range(2)]
        ot = [A.sb([128, 512], F32) for _ in range(3)]
        tp = [A.ps([128, 1024], BF16) for _ in range(2)]
        po = [A.ps([128, 512], F32) for _ in range(4)]

        S.dma("sp", lambda e: e.dma_start(out=nwB[:], in_=nw.partition_broadcast(128)), writes=[("nwB",)])
        S.dma("sp", lambda e: e.dma_start(out=identf[:], in_=identd), writes=[("identf",)])
        S.dve(lambda e: e.tensor_copy(out=identb[:], in_=identf[:]), reads=[("identf",)], writes=[("identb",)])
        ev = 0
        for tt in range(NT):
            b = tt % 2
            S.dma("sp", lambda e, tt=tt, b=b: e.dma_start(out=xt[b][:], in_=x[tt * 128:(tt + 1) * 128, :]),
                  writes=[("xt", b)])
            rms_stage(S, A, xt[b][:], ("xt", b), ss, rs, junk, nwB, hb[b][:], ("hb", b))
            for g in range(KC // 4):
                pb = g % 2
                for j in range(4):
                    kc = g * 4 + j
                    S.pe(lambda e, pb=pb, j=j, kc=kc, b=b: e.transpose(
                        out=tp[pb][:, j * 128:(j + 1) * 128], in_=hb[b][:, kc * 128:(kc + 1) * 128], identity=identb[:]),
                        reads=[("hb", b), ("identb",)], writes=[("tp", pb)])
                dst = hT[:, tt, g * 4:(g + 1) * 4, :]
                if ev % TEV == 0:
                    S.act(lambda e, pb=pb, dst=dst: e.copy(out=dst, in_=tp[pb][:, 0:512].rearrange("p (a b) -> p a b", a=4)),
                          reads=[("tp", pb)], writes=[("hT", tt, g)])
                else:
                    S.dve(lambda e, pb=pb, dst=dst: e.tensor_copy(out=dst, in_=tp[pb][:, 0:512].rearrange("p (a b) -> p a b", a=4)),
                          reads=[("tp", pb)], writes=[("hT", tt, g)])
                ev += 1
        wv = w.rearrange("(kc p) n -> p kc n", p=128)
        nct = (N + 511) // 512 if NCT is None else NCT
        it = 0
        for ct in range(nct):
            c0 = ct * 512
            cw = min(512, N - c0)
            wb = ct % 2
            S.dma("pool", lambda e, wb=wb, c0=c0, cw=cw: e.dma_start(out=wt[wb][:, :, 0:cw], in_=wv[:, :, c0:c0 + cw]),
                  writes=[("wt", wb)])
            for tt in range(NT):
                pb = it % 4
                ob = it % 3
                it += 1
                for kc in range(KC):
                    S.pe(lambda e, pb=pb, tt=tt, kc=kc, wb=wb, cw=cw: e.matmul(
                        out=po[pb][:, 0:cw], lhsT=hT[:, tt, kc, :], rhs=wt[wb][:, kc, 0:cw],
                        start=(kc == 0), stop=(kc == KC - 1)),
                        reads=[("hT", tt), ("wt", wb)], writes=[("po", pb)])
                if it % 2 == 0:
                    S.act(lambda e, pb=pb, ob=ob, cw=cw: e.copy(out=ot[ob][:, 0:cw], in_=po[pb][:, 0:cw]),
                          reads=[("po", pb)], writes=[("ot", ob)])
                else:
                    S.dve(lambda e, pb=pb, ob=ob, cw=cw: e.tensor_copy(out=ot[ob][:, 0:cw], in_=po[pb][:, 0:cw]),
                          reads=[("po", pb)], writes=[("ot", ob)])
                S.dma("sp", lambda e, ob=ob, tt=tt, c0=c0, cw=cw: e.dma_start(
                    out=out[tt * 128:(tt + 1) * 128, c0:c0 + cw], in_=ot[ob][:, 0:cw]),
                    reads=[("ot", ob)])
        S.emit()
    return nc


HD = 128
G4 = [[0, 1, 2, 3], [4, 5, 6, 7]]
ISQ = HD ** -0.5


def bc(ap, shape):
    return ap.broadcast_to(list(shape))


def mixer_prelude(S, A, NB, R):
    parB, gates, C = R["parB"], R["gates"], R["consts"]
    T = {}
    for n in ["ti", "tf", "lf", "w", "e", "a", "tmp", "ff_lf", "ff_c", "ff_cum", "ff_tot", "gg", "gbeta", "ggc", "ggl", "ones32"]:
        T[n] = A.sb([128, NB], F32)
    par15 = A.sb([128, 8], F32)
    T["par15"] = par15
    S.dve(lambda e: e.tensor_scalar(out=par15[:], in0=parB[:], scalar1=1.0 / 15.0, scalar2=None, op0=ALU.mult),
          reads=[("parB",)], writes=[("par15",)])
    S.dve(lambda e: e.memset(T["ones32"][:], 1.0), writes=[("ones32",)])
    return T


def logsig_inplace(S, t, key):
    S.act(lambda e: e.activation(out=t, in_=t, func=AF.Exp, scale=-1.0), reads=[key], writes=[key])
    S.act(lambda e: e.activation(out=t, in_=t, func=AF.Ln, bias=1.0), reads=[key], writes=[key])
    S.dve(lambda e: e.tensor_scalar(out=t, in0=t, scalar1=-1.0, scalar2=None, op0=ALU.mult), reads=[key], writes=[key])


def mlstm_phase(S, A, NB, R, T, PS, dr, ytok):
    C = R["consts"]
    parB, par15, gates = R["parB"], T["par15"], R["gates"]
    SEQ = NB * 128
    B = R["big"]
    qT = B.v(0, SEQ)
    kT = B.v(1, SEQ)
    ktok = B.v(2, NB, 128)
    v1 = B.v(3, NB, 129)
    mo = B.v(4, NB, 128)
    kw = B.v(5, NB, 128)
    Sst = B.v(6, NB + 1, 129)
    hraw = B.v(7, NB, 129)
    Tt = A.sb([128, 129], F32)
    PT = [A.sb([128, 128], F32) for _ in range(2)]
    tokv = dr["tokm"].rearrange("(b p) c -> p b c", p=128)
    S.dma("sp", lambda e: e.dma_start(out=qT, in_=dr["m_qT"]), reads=[("fm_d",)], writes=[("B", 0)])
    S.dma("sp", lambda e: e.dma_start(out=kT, in_=dr["m_kT"]), reads=[("fm_d",)], writes=[("B", 1)])
    S.dma("sp", lambda e: e.dma_start(out=ktok, in_=tokv[:, :, 0:128]), reads=[("tokm_d",)], writes=[("B", 2)])
    S.dma("sp", lambda e: e.dma_start(out=v1[:, :, 0:128], in_=tokv[:, :, 128:256]), reads=[("tokm_d",)], writes=[("B", 3, 0)])
    S.dma("sp", lambda e: e.dma_start(out=mo, in_=tokv[:, :, 256:384]), reads=[("tokm_d",)], writes=[("B", 4)])
    S.pool(lambda e: e.memset(v1[:, :, 128:129], 1.0), writes=[("B", 3, 1)])
    S.pool(lambda e: e.memset(Sst[:, 0, :], 0.0), writes=[("B", 6, 0)])
    ti, tf, lf, w, ee, aa, tmp = T["ti"], T["tf"], T["lf"], T["w"], T["e"], T["a"], T["tmp"]
    S.act(lambda e: e.activation(out=ti[:], in_=gates[:, :, 0], func=AF.Tanh, bias=par15[:, 0:1], scale=1.0 / 15.0),
          reads=[("gates",), ("par15",)], writes=[("ti",)])
    S.dve(lambda e: e.tensor_scalar(out=ti[:], in0=ti[:], scalar1=15.0, scalar2=None, op0=ALU.mult), reads=[("ti",)], writes=[("ti",)])
    S.act(lambda e: e.activation(out=lf[:], in_=gates[:, :, 1], func=AF.Tanh, bias=par15[:, 1:2], scale=1.0 / 15.0),
          reads=[("gates",), ("par15",)], writes=[("lf",)])
    S.dve(lambda e: e.tensor_scalar(out=lf[:], in0=lf[:], scalar1=15.0, scalar2=None, op0=ALU.mult), reads=[("lf",)], writes=[("lf",)])
    logsig_inplace(S, lf[:], ("lf",))
    pb, pbl = PS[0], PS[1]
    S.pe(lambda e: e.matmul(out=pb[:, 0:NB], lhsT=C[:, 1, :], rhs=lf[:], start=True, stop=True),
         reads=[("consts",), ("lf",)], writes=[("ps", 0)])
    S.pe(lambda e: e.matmul(out=pbl[:, 0:NB], lhsT=C[:, 2, :], rhs=lf[:], start=True, stop=True),
         reads=[("consts",), ("lf",)], writes=[("ps", 1)])
    S.dve(lambda e: e.tensor_tensor(out=tmp[:], in0=ti[:], in1=pb[:, 0:NB], op=ALU.subtract), reads=[("ti",), ("ps", 0)], writes=[("tmp",)])
    S.act(lambda e: e.activation(out=w[:], in_=tmp[:], func=AF.Exp), reads=[("tmp",)], writes=[("w",)])
    S.dve(lambda e: e.tensor_scalar(out=w[:], in0=w[:], scalar1=ISQ, scalar2=None, op0=ALU.mult), reads=[("w",)], writes=[("w",)])
    S.act(lambda e: e.activation(out=ee[:], in_=pb[:, 0:NB], func=AF.Exp), reads=[("ps", 0)], writes=[("e",)])
    S.act(lambda e: e.activation(out=aa[:], in_=pbl[:, 0:NB], func=AF.Exp), reads=[("ps", 1)], writes=[("a",)])
    S.dve(lambda e: e.tensor_tensor(out=kw, in0=ktok, in1=bc(w[:].unsqueeze(2), [128, NB, 128]), op=ALU.mult),
          reads=[("B", 2), ("w",)], writes=[("B", 5)])
    for c in range(NB):
        pu = PS[2 + c % 2]
        S.pe(lambda e, c=c, pu=pu: e.matmul(out=pu[:, 0:129], lhsT=kw[:, c, :], rhs=v1[:, c, :], start=True, stop=True),
             reads=[("B", 5), ("B", 3)], writes=[("ps", 2 + c % 2)])
        S.dve(lambda e, c=c, pu=pu: e.tensor_tensor(out=Tt[:], in0=Sst[:, c, :], in1=pu[:, 0:129], op=ALU.add),
              reads=[("B", 6, c), ("ps", 2 + c % 2)], writes=[("m_Tt",)])
        S.dve(lambda e, c=c: e.tensor_scalar(out=Sst[:, c + 1, :], in0=Tt[:], scalar1=aa[:, c:c + 1], scalar2=None, op0=ALU.mult),
              reads=[("m_Tt",), ("a",)], writes=[("B", 6, c + 1)])
    for c in range(NB):
        ps_s = PS[4 + c % 2]
        ps_o = PS[6 + c % 2]
        pt = PT[c % 2]
        sl = slice(c * 128, (c + 1) * 128)
        S.pe(lambda e, sl=sl, ps_s=ps_s: e.matmul(out=ps_s[:, 0:128], lhsT=kT[:, sl], rhs=qT[:, sl], start=True, stop=True),
             reads=[("B", 1), ("B", 0)], writes=[("ps", 4 + c % 2)])
        S.dve(lambda e, c=c, ps_s=ps_s, pt=pt: e.scalar_tensor_tensor(out=pt[:], in0=ps_s[:, 0:128], scalar=w[:, c:c + 1], in1=C[:, 1, :],
                                                                      op0=ALU.mult, op1=ALU.mult),
              reads=[("ps", 4 + c % 2), ("w",), ("consts",)], writes=[("m_PT", c % 2)])
        S.pe(lambda e, c=c, ps_o=ps_o, pt=pt: e.matmul(out=ps_o[:, 0:129], lhsT=pt[:], rhs=v1[:, c, :], start=True, stop=False),
             reads=[("m_PT", c % 2), ("B", 3)], writes=[("ps", 6 + c % 2)])
        S.pe(lambda e, c=c, sl=sl, ps_o=ps_o: e.matmul(out=ps_o[:, 0:129], lhsT=qT[:, sl], rhs=Sst[:, c, :], start=False, stop=True),
             reads=[("B", 0), ("B", 6, c)], writes=[("ps", 6 + c % 2)])
        S.act(lambda e, c=c, ps_o=ps_o: e.copy(out=hraw[:, c, :], in_=ps_o[:, 0:129]), reads=[("ps", 6 + c % 2)], writes=[("B", 7, c)])
    den = T["tmp"]
    S.dve(lambda e: e.tensor_tensor(out=den[:], in0=hraw[:, :, 128], in1=ee[:], op=ALU.mult), reads=[("B", 7), ("e",)], writes=[("tmp",)])
    nden = T["gg"]
    S.dve(lambda e: e.tensor_scalar(out=nden[:], in0=den[:], scalar1=-1.0, scalar2=None, op0=ALU.mult), reads=[("tmp",)], writes=[("gg",)])
    S.dve(lambda e: e.tensor_tensor(out=den[:], in0=den[:], in1=nden[:], op=ALU.max), reads=[("tmp",), ("gg",)], writes=[("tmp",)])
    S.dve(lambda e: e.tensor_scalar(out=den[:], in0=den[:], scalar1=1.0, scalar2=None, op0=ALU.max), reads=[("tmp",)], writes=[("tmp",)])
    S.dve(lambda e: e.reciprocal(out=den[:], in_=den[:]), reads=[("tmp",)], writes=[("tmp",)])
    S.dve(lambda e: e.tensor_tensor(out=den[:], in0=den[:], in1=ee[:], op=ALU.mult), reads=[("tmp",), ("e",)], writes=[("tmp",)])
    hn = kw
    S.dve(lambda e: e.tensor_tensor(out=hn, in0=hraw[:, :, 0:128], in1=bc(den[:].unsqueeze(2), [128, NB, 128]), op=ALU.mult),
          reads=[("B", 7), ("tmp",)], writes=[("B", 5)])
    mu, var = T["ti"], T["tf"]
    S.dve(lambda e: e.tensor_reduce(out=mu[:], in_=hn, axis=AX.X, op=ALU.add), reads=[("B", 5)], writes=[("ti",)])
    S.dve(lambda e: e.tensor_scalar(out=mu[:], in0=mu[:], scalar1=1.0 / 128, scalar2=None, op0=ALU.mult), reads=[("ti",)], writes=[("ti",)])
    S.dve(lambda e: e.tensor_tensor(out=hn, in0=hn, in1=bc(mu[:].unsqueeze(2), [128, NB, 128]), op=ALU.subtract),
          reads=[("B", 5), ("ti",)], writes=[("B", 5)])
    sq = ktok
    S.act(lambda e: e.activation(out=sq, in_=hn, func=AF.Square), reads=[("B", 5)], writes=[("B", 2)])
    S.dve(lambda e: e.tensor_reduce(out=var[:], in_=sq, axis=AX.X, op=ALU.add), reads=[("B", 2)], writes=[("tf",)])
    S.dve(lambda e: e.tensor_scalar(out=var[:], in0=var[:], scalar1=1.0 / 128, scalar2=EPS, op0=ALU.mult, op1=ALU.add), reads=[("tf",)], writes=[("tf",)])
    S.act(lambda e: e.activation(out=var[:], in_=var[:], func=AF.Sqrt), reads=[("tf",)], writes=[("tf",)])
    S.dve(lambda e: e.reciprocal(out=var[:], in_=var[:]), reads=[("tf",)], writes=[("tf",)])
    S.dve(lambda e: e.tensor_tensor(out=hn, in0=hn, in1=bc(var[:].unsqueeze(2), [128, NB, 128]), op=ALU.mult),
          reads=[("B", 5), ("tf",)], writes=[("B", 5)])
    S.dve(lambda e: e.tensor_tensor(out=hn, in0=hn, in1=bc(R["normw"][:, 0, :].unsqueeze(1), [128, NB, 128]), op=ALU.mult),
          reads=[("B", 5), ("normw",)], writes=[("B", 5)])
    S.act(lambda e: e.activation(out=mo, in_=mo, func=AF.Sigmoid), reads=[("B", 4)], writes=[("B", 4)])
    S.dve(lambda e: e.tensor_tensor(out=hn, in0=hn, in1=mo, op=ALU.mult),
          reads=[("B", 5), ("B", 4)], writes=[("B", 5)])
    S.dma("sp", lambda e: e.dma_start(out=ytok.rearrange("(b p) c -> p b c", p=128)[:, :, 0:128], in_=hn),
          reads=[("B", 5)], writes=[("ytok_d", 0)])


def consts_np():
    i = np.arange(128)
    c = np.zeros((128, 5, 128), np.float32)
    c[:, 0, :] = np.eye(128)
    c[:, 1, :] = (i[:, None] <= i[None, :])
    c[:, 2, :] = 1.0
    c[:, 3, :] = (i[None, :] < i[:, None])
    c[:, 4, :] = (i[None, :] <= i[:, None])
    return c


def build_mix_test(NB, which):
    nc = bass.Bass("TRN2", target_bir_lowering=False)
    SEQ = NB * 128
    dr = {}
    dr["m_qT"] = nc.dram_tensor("m_qT", [128, SEQ], F32, kind="ExternalInput").ap()
    dr["m_kT"] = nc.dram_tensor("m_kT", [128, SEQ], F32, kind="ExternalInput").ap()
    dr["tokm"] = nc.dram_tensor("tokm", [SEQ, 768], F32, kind="ExternalInput").ap()
    dr["f_qkT"] = nc.dram_tensor("f_qkT", [4, 128, SEQ], BF16, kind="ExternalInput").ap()
    dr["g_T"] = nc.dram_tensor("g_T", [3, 128, SEQ], F32, kind="ExternalInput").ap()
    gates_d = nc.dram_tensor("gates_d", [SEQ, 6], F32, kind="ExternalInput").ap()
    params = nc.dram_tensor("params", [8], F32, kind="ExternalInput").ap()
    normw_d = nc.dram_tensor("normw", [4 * 128], F32, kind="ExternalInput").ap()
    convw_d = nc.dram_tensor("convw", [128, 12], F32, kind="ExternalInput").ap()
    consts_d = nc.dram_tensor("consts", [128, 5, 128], F32, kind="ExternalInput").ap()
    ytok = nc.dram_tensor("ytok", [SEQ, 512], F32, kind="ExternalOutput").ap()
    S = Sched(nc)
    with contextlib.ExitStack() as st:
        A = Alloc(nc, st)
        R = {}
        R["consts"] = A.sb([128, 5, 128], F32)
        R["parB"] = A.sb([128, 8], F32)
        R["gates"] = A.sb([128, NB, 6], F32)
        R["normw"] = A.sb([128, 4, 128], F32)
        R["convw"] = A.sb([128, 12], F32)
        R["big"] = Big(A, 9)
        PS = [A.ps([128, 512], F32) for _ in range(8)]
        S.dma("sp", lambda e: e.dma_start(out=R["consts"][:], in_=consts_d), writes=[("consts",)])
        S.dma("sp", lambda e: e.dma_start(out=R["parB"][:], in_=params.partition_broadcast(128)), writes=[("parB",)])
        S.dma("sp", lambda e: e.dma_start(out=R["gates"][:], in_=gates_d.rearrange("(b p) c -> p b c", p=128)), writes=[("gates",)])
        S.dma("sp", lambda e: e.dma_start(out=R["normw"][:].rearrange("p a b -> p (a b)"), in_=normw_d.partition_broadcast(128)), writes=[("normw",)])
        S.dma("sp", lambda e: e.dma_start(out=R["convw"][:], in_=convw_d), writes=[("convw",)])
        T = mixer_prelude(S, A, NB, R)
        if "m" in which:
            mlstm_phase(S, A, NB, R, T, PS, dr, ytok)
        if "f" in which:
            fox_phase(S, A, NB, R, T, PS, dr, ytok)
        if "g" in which:
            gdn_phase(S, A, NB, R, T, PS, dr, ytok)
        S.emit()
    return nc


def fox_phase(S, A, NB, R, T, PS, dr, ytok):
    C = R["consts"]
    parB, gates, B = R["parB"], R["gates"], R["big"]
    SEQ = NB * 128
    qk2 = [B.t[2 + i][:, 0:SEQ].bitcast(BF16).rearrange("p (a b) -> p a b", a=2) for i in range(2)]

    class _QK:
        def __getitem__(self, idx):
            p, j, sl = idx
            return qk2[j // 2][p, j % 2, sl]
    qk = _QK()
    v1b = B.t[4][:, 0:NB * 130].bitcast(BF16).rearrange("p (h n c) -> p h n c", h=2, n=NB)
    PTb = [A.sb([128, 128], BF16) for _ in range(3)]
    bias = A.sb([128, NB, NB], F32)
    rr = A.sb([128, 1], F32)
    tokv = dr["tokm"].rearrange("(b p) c -> p b c", p=128)
    yv = ytok.rearrange("(b p) c -> p b c", p=128)
    for j in range(4):
        S.dma("sp", lambda e, j=j: e.dma_start(out=qk[:, j, :], in_=dr["f_qkT"][j]), reads=[("f_qkT_d",)], writes=[("B", 2 + j // 2, j % 2)])
    for hh in range(2):
        S.dma("pool", lambda e, hh=hh: e.dma_start(out=v1b[:, hh, :, 0:128], in_=tokv[:, :, 384 + 128 * hh:512 + 128 * hh]),
              reads=[("tokm_d",)], writes=[("B", 4, hh, 0)])
        S.dve(lambda e, hh=hh: e.memset(v1b[:, hh, :, 128:130], 1.0), writes=[("B", 4, hh, 1)])
    lf, cc, cum, tot = T["ff_lf"], T["ff_c"], T["ff_cum"], T["ff_tot"]
    for hh in range(2):
        oacc = B.v(hh, NB, 128)
        S.dve(lambda e, hh=hh: e.tensor_scalar(out=lf[:], in0=gates[:, :, 2 + hh], scalar1=parB[:, 2 + hh:3 + hh], scalar2=None, op0=ALU.add),
              reads=[("gates",), ("parB",)], writes=[("ff_lf",)])
        logsig_inplace(S, lf[:], ("ff_lf",))
        S.pe(lambda e: e.matmul(out=PS[0][:, 0:NB], lhsT=C[:, 1, :], rhs=lf[:], start=True, stop=True),
             reads=[("consts",), ("ff_lf",)], writes=[("ps", 0)])
        S.pe(lambda e: e.matmul(out=PS[1][:, 0:NB], lhsT=C[:, 2, :], rhs=lf[:], start=True, stop=True),
             reads=[("consts",), ("ff_lf",)], writes=[("ps", 1)])
        S.act(lambda e: e.copy(out=tot[:], in_=PS[1][:, 0:NB]), reads=[("ps", 1)], writes=[("ff_tot",)])
        S.dve(lambda e: e.tensor_copy(out=cum[:], in_=tot[:]), reads=[("ff_tot",)], writes=[("ff_cum",)])
        for j in range(1, NB):
            S.dve(lambda e, j=j: e.tensor_tensor(out=cum[:, j:j + 1], in0=cum[:, j - 1:j], in1=tot[:, j:j + 1], op=ALU.add),
                  reads=[("ff_cum",), ("ff_tot",)], writes=[("ff_cum",)])
        S.dve(lambda e: e.tensor_tensor(out=cc[:], in0=cum[:], in1=PS[0][:, 0:NB], op=ALU.add), reads=[("ff_cum",), ("ps", 0)], writes=[("ff_c",)])
        S.dve(lambda e: e.tensor_tensor(out=cc[:], in0=cc[:], in1=tot[:], op=ALU.subtract), reads=[("ff_c",), ("ff_tot",)], writes=[("ff_c",)])
        for kb in range(NB):
            S.dve(lambda e, kb=kb: e.tensor_scalar(out=bias[:, kb, :], in0=cum[:], scalar1=cc[:, kb:kb + 1], scalar2=None, op0=ALU.subtract),
                  reads=[("ff_cum",), ("ff_c",)], writes=[("f_bias", kb)])
        pairs = [(qb, kb) for qb in range(NB) for kb in range(qb + 1)]

        def emit_s(i, hh=hh):
            qb, kb = pairs[i]
            S.pe(lambda e: e.matmul(out=PS[i % 3][:, 0:128], lhsT=qk[:, 2 + hh, kb * 128:(kb + 1) * 128],
                                    rhs=qk[:, hh, qb * 128:(qb + 1) * 128], start=True, stop=True),
                 reads=[("B", 3, hh), ("B", 2, hh)], writes=[("ps", i % 3)])
        emit_s(0)
        if len(pairs) > 1:
            emit_s(1)
        for i, (qb, kb) in enumerate(pairs):
            pt = PTb[i % 3]
            S.act(lambda e, i=i, qb=qb, kb=kb, pt=pt: e.activation(out=pt[:], in_=PS[i % 3][:, 0:128], func=AF.Exp, scale=ISQ,
                                                                 bias=bias[:, kb, qb:qb + 1]),
                  reads=[("ps", i % 3), ("f_bias", kb)], writes=[("f_PT", i % 3)])
            if kb == qb:
                S.dve(lambda e, pt=pt: e.tensor_tensor(out=pt[:], in0=pt[:], in1=C[:, 1, :], op=ALU.mult),
                      reads=[("f_PT", i % 3), ("consts",)], writes=[("f_PT", i % 3)])
            ob = 3 + qb % 2
            S.pe(lambda e, pt=pt, hh=hh, kb=kb, qb=qb, ob=ob: e.matmul(out=PS[ob][:, 0:129], lhsT=pt[:], rhs=v1b[:, hh, kb, 0:129],
                                                                     start=(kb == 0), stop=(kb == qb)),
                 reads=[("f_PT", i % 3), ("B", 4, hh)], writes=[("ps", ob)])
            if i + 2 < len(pairs):
                emit_s(i + 2)
            if kb == qb:
                S.dve(lambda e, ob=ob: e.reciprocal(out=rr[:], in_=PS[ob][:, 128:129]), reads=[("ps", ob)], writes=[("f_rr",)])
                S.dve(lambda e, ob=ob, qb=qb, oacc=oacc: e.tensor_scalar(out=oacc[:, qb, :], in0=PS[ob][:, 0:128], scalar1=rr[:], scalar2=None,
                                                                        op0=ALU.mult),
                      reads=[("ps", ob), ("f_rr",)], writes=[("B", hh, qb)])
        sq = B.v(5, NB, 128)
        ssq = T["tmp"]
        S.act(lambda e, oacc=oacc: e.activation(out=sq, in_=oacc, func=AF.Square), reads=[("B", hh)], writes=[("B", 5)])
        S.dve(lambda e: e.tensor_reduce(out=ssq[:], in_=sq, axis=AX.X, op=ALU.add), reads=[("B", 5)], writes=[("tmp",)])
        S.dve(lambda e: e.tensor_scalar(out=ssq[:], in0=ssq[:], scalar1=1.0 / 128, scalar2=EPS, op0=ALU.mult, op1=ALU.add), reads=[("tmp",)], writes=[("tmp",)])
        S.act(lambda e: e.activation(out=ssq[:], in_=ssq[:], func=AF.Sqrt), reads=[("tmp",)], writes=[("tmp",)])
        S.dve(lambda e: e.reciprocal(out=ssq[:], in_=ssq[:]), reads=[("tmp",)], writes=[("tmp",)])
        S.dve(lambda e, oacc=oacc: e.tensor_tensor(out=oacc, in0=oacc, in1=bc(ssq[:].unsqueeze(2), [128, NB, 128]), op=ALU.mult),
              reads=[("B", hh), ("tmp",)], writes=[("B", hh)])
        S.dve(lambda e, oacc=oacc, hh=hh: e.tensor_tensor(out=oacc, in0=oacc, in1=bc(R["normw"][:, 1 + hh, :].unsqueeze(1), [128, NB, 128]), op=ALU.mult),
              reads=[("B", hh), ("normw",)], writes=[("B", hh)])
        S.dma("sp", lambda e, oacc=oacc, hh=hh: e.dma_start(out=yv[:, :, 128 + 128 * hh:256 + 128 * hh], in_=oacc),
              reads=[("B", hh)], writes=[("ytok_d", 1 + hh)])


def gdn_phase(S, A, NB, R, T, PS, dr, ytok):
    C = R["consts"]
    parB, gates, B, convw = R["parB"], R["gates"], R["big"], R["convw"]
    SEQ = NB * 128
    G = 2
    tokv = dr["tokm"].rearrange("(b p) c -> p b c", p=128)
    yv = ytok.rearrange("(b p) c -> p b c", p=128)
    ident, Uincl, ones, Lstrict, Lincl = (C[:, i, :] for i in range(5))
    gg, beta, gc, gl = T["gg"], T["gbeta"], T["ggc"], T["ggl"]
    be, kes, egl, na = T["ti"], T["tf"], T["lf"], T["par15"]
    S.act(lambda e: e.activation(out=na[:, 7:8], in_=parB[:, 4:5], func=AF.Exp), reads=[("parB",), ("par15",)], writes=[("par15",)])
    S.act(lambda e: e.activation(out=gg[:], in_=gates[:, :, 4], func=AF.Exp, bias=parB[:, 5:6]), reads=[("gates",), ("parB",)], writes=[("gg",)])
    S.act(lambda e: e.activation(out=gg[:], in_=gg[:], func=AF.Ln, bias=1.0), reads=[("gg",)], writes=[("gg",)])
    S.dve(lambda e: e.tensor_scalar(out=gg[:], in0=gg[:], scalar1=na[:, 7:8], scalar2=-1.0, op0=ALU.mult, op1=ALU.mult),
          reads=[("gg",), ("par15",)], writes=[("gg",)])
    S.act(lambda e: e.activation(out=beta[:], in_=gates[:, :, 5], func=AF.Sigmoid), reads=[("gates",)], writes=[("gbeta",)])
    S.pe(lambda e: e.matmul(out=PS[0][:, 0:NB], lhsT=Uincl, rhs=gg[:], start=True, stop=True), reads=[("consts",), ("gg",)], writes=[("ps", 0)])
    S.pe(lambda e: e.matmul(out=PS[1][:, 0:NB], lhsT=ones, rhs=gg[:], start=True, stop=True), reads=[("consts",), ("gg",)], writes=[("ps", 1)])
    S.act(lambda e: e.copy(out=gc[:], in_=PS[0][:, 0:NB]), reads=[("ps", 0)], writes=[("ggc",)])
    S.act(lambda e: e.copy(out=gl[:], in_=PS[1][:, 0:NB]), reads=[("ps", 1)], writes=[("ggl",)])
    S.act(lambda e: e.activation(out=be[:], in_=gc[:], func=AF.Exp), reads=[("ggc",)], writes=[("ti",)])
    S.dve(lambda e: e.tensor_tensor(out=be[:], in0=be[:], in1=beta[:], op=ALU.mult), reads=[("ti",), ("gbeta",)], writes=[("ti",)])
    S.dve(lambda e: e.tensor_tensor(out=kes[:], in0=gl[:], in1=gc[:], op=ALU.subtract), reads=[("ggl",), ("ggc",)], writes=[("tf",)])
    S.act(lambda e: e.activation(out=kes[:], in_=kes[:], func=AF.Exp), reads=[("tf",)], writes=[("tf",)])
    S.act(lambda e: e.activation(out=egl[:], in_=gl[:], func=AF.Exp), reads=[("ggl",)], writes=[("lf",)])
    for t in range(3):
        xin = B.t[0 if t % 2 == 0 else 4]
        xk = ("B", 0 if t % 2 == 0 else 4)
        acc = B.v(1 + t, SEQ)
        ak = ("B", 1 + t)
        S.dve(lambda e, xin=xin: e.memset(xin[:, 0:3], 0.0), writes=[xk + (0,)])
        S.dma("sp", lambda e, xin=xin, t=t: e.dma_start(out=xin[:, 3:3 + SEQ], in_=dr["g_T"][t]), reads=[("fm_d",)], writes=[xk + (1,)])
        S.dve(lambda e, xin=xin, acc=acc, t=t: e.tensor_scalar(out=acc, in0=xin[:, 3:3 + SEQ], scalar1=convw[:, 4 * t + 3:4 * t + 4], scalar2=None, op0=ALU.mult),
              reads=[xk, ("convw",)], writes=[ak])
        for j in range(3):
            S.dve(lambda e, xin=xin, acc=acc, t=t, j=j: e.scalar_tensor_tensor(out=acc, in0=xin[:, j:j + SEQ], scalar=convw[:, 4 * t + j:4 * t + j + 1],
                                                                               in1=acc, op0=ALU.mult, op1=ALU.add),
                  reads=[xk, ("convw",), ak], writes=[ak])
        S.act(lambda e, acc=acc: e.activation(out=acc, in_=acc, func=AF.Silu), reads=[ak], writes=[ak])
    sq = B.v(5, SEQ)
    rn = B.v(6, SEQ)
    for t in range(2):
        src = B.v(1 + t, SEQ)
        S.act(lambda e, src=src: e.activation(out=sq, in_=src, func=AF.Square), reads=[("B", 1 + t)], writes=[("B", 5)])
        for sl in range(0, SEQ, 512):
            pb = PS[(sl // 512) % 2]
            S.pe(lambda e, sl=sl, pb=pb: e.matmul(out=pb[:, 0:512], lhsT=ones, rhs=sq[:, sl:sl + 512], start=True, stop=True),
                 reads=[("consts",), ("B", 5)], writes=[("ps", (sl // 512) % 2)])
            S.dve(lambda e, sl=sl, pb=pb: e.tensor_scalar(out=rn[:, sl:sl + 512], in0=pb[:, 0:512], scalar1=EPS, scalar2=None, op0=ALU.add),
                  reads=[("ps", (sl // 512) % 2)], writes=[("B", 6, sl)])
        S.act(lambda e: e.activation(out=rn, in_=rn, func=AF.Sqrt), reads=[("B", 6)], writes=[("B", 6)])
        S.dve(lambda e: e.reciprocal(out=rn, in_=rn), reads=[("B", 6)], writes=[("B", 6)])
        if t == 0:
            S.dve(lambda e, src=src: e.scalar_tensor_tensor(out=src, in0=src, scalar=ISQ, in1=rn, op0=ALU.mult, op1=ALU.mult),
                  reads=[("B", 1), ("B", 6)], writes=[("B", 1)])
        else:
            S.dve(lambda e, src=src: e.tensor_tensor(out=src, in0=src, in1=rn, op=ALU.mult), reads=[("B", 2), ("B", 6)], writes=[("B", 2)])
    qT, kT, vT = B.v(1, SEQ), B.v(2, SEQ), B.v(3, SEQ)
    ktok, vtok = B.v(4, NB, 128), B.v(0, NB, 128)
    for (src, dst, sk, dk) in ((kT, ktok, ("B", 2), ("B", 4)), (vT, vtok, ("B", 3), ("B", 0))):
        for g0 in range(0, NB, 4):
            pb = PS[2 + (g0 // 4) % 2]
            for j in range(4):
                S.pe(lambda e, pb=pb, j=j, g0=g0, src=src: e.transpose(out=pb[:, j * 128:(j + 1) * 128], in_=src[:, (g0 + j) * 128:(g0 + j + 1) * 128], identity=ident),
                     reads=[sk, ("consts",)], writes=[("ps", 2 + (g0 // 4) % 2)])
            S.act(lambda e, pb=pb, g0=g0, dst=dst: e.copy(out=dst[:, g0:g0 + 4, :], in_=pb[:, 0:512].rearrange("p (a b) -> p a b", a=4)),
                  reads=[("ps", 2 + (g0 // 4) % 2)], writes=[dk + (g0,)])
    diagG = B.v(6, NB, 128)
    Grow = B.v(5, SEQ)
    S.dve(lambda e: e.tensor_tensor(out=diagG, in0=bc(ident.unsqueeze(1), [128, NB, 128]), in1=bc(gc[:].unsqueeze(2), [128, NB, 128]), op=ALU.mult),
          reads=[("consts",), ("ggc",)], writes=[("B", 6)])
    dflat = B.v(6, SEQ)
    for sl in range(0, SEQ, 512):
        pb = PS[(sl // 512) % 2]
        S.pe(lambda e, sl=sl, pb=pb: e.matmul(out=pb[:, 0:512], lhsT=ones, rhs=dflat[:, sl:sl + 512], start=True, stop=True),
             reads=[("consts",), ("B", 6)], writes=[("ps", (sl // 512) % 2)])
        S.act(lambda e, sl=sl, pb=pb: e.copy(out=Grow[:, sl:sl + 512], in_=pb[:, 0:512]), reads=[("ps", (sl // 512) % 2)], writes=[("B", 5, sl)])
    G3 = B.v(5, NB, 128)
    Dm, DTm = B.v(6, NB, 128), B.v(7, NB, 128)
    gcb = bc(gc[:].unsqueeze(2), [128, NB, 128])
    S.dve(lambda e: e.tensor_tensor(out=Dm, in0=gcb, in1=G3, op=ALU.subtract), reads=[("ggc",), ("B", 5)], writes=[("B", 6)])
    S.dve(lambda e: e.tensor_scalar(out=Dm, in0=Dm, scalar1=0.0, scalar2=None, op0=ALU.min), reads=[("B", 6)], writes=[("B", 6)])
    S.act(lambda e: e.activation(out=Dm, in_=Dm, func=AF.Exp), reads=[("B", 6)], writes=[("B", 6)])
    S.dve(lambda e: e.tensor_tensor(out=Dm, in0=Dm, in1=bc(Lstrict.unsqueeze(1), [128, NB, 128]), op=ALU.mult), reads=[("B", 6), ("consts",)], writes=[("B", 6)])
    S.dve(lambda e: e.tensor_tensor(out=Dm, in0=Dm, in1=bc(beta[:].unsqueeze(2), [128, NB, 128]), op=ALU.mult), reads=[("B", 6), ("gbeta",)], writes=[("B", 6)])
    S.dve(lambda e: e.tensor_tensor(out=DTm, in0=G3, in1=gcb, op=ALU.subtract), reads=[("ggc",), ("B", 5)], writes=[("B", 7)])
    S.dve(lambda e: e.tensor_scalar(out=DTm, in0=DTm, scalar1=0.0, scalar2=None, op0=ALU.min), reads=[("B", 7)], writes=[("B", 7)])
    S.act(lambda e: e.activation(out=DTm, in_=DTm, func=AF.Exp), reads=[("B", 7)], writes=[("B", 7)])
    S.dve(lambda e: e.tensor_tensor(out=DTm, in0=DTm, in1=bc(Uincl.unsqueeze(1), [128, NB, 128]), op=ALU.mult), reads=[("B", 7), ("consts",)], writes=[("B", 7)])
    S.act(lambda e: e.activation(out=Grow, in_=Grow, func=AF.Exp), reads=[("B", 5)], writes=[("B", 5)])
    qdT = B.v(3, SEQ)
    S.dve(lambda e: e.tensor_tensor(out=qdT, in0=qT, in1=Grow, op=ALU.mult), reads=[("B", 1), ("B", 5)], writes=[("B", 3)])
    zt = B.v(5, NB, 128)
    S.dma("sp", lambda e: e.dma_start(out=zt, in_=tokv[:, :, 640:768]), reads=[("tokm_d",)], writes=[("B", 5)])
    oall = B.v(8, NB, 128)
    Sg = A.sb([128, 128], F32)
    S.dve(lambda e: e.memset(Sg[:], 0.0), writes=[("g_S",)])
    names = ["Ag", "ATg", "P0", "P1", "PT0", "PT1", "Rg", "attnT", "kbe", "vb", "kend", "Wval", "WkT"]
    X = {n: A.sb([128, G, 128], F32) for n in names}
    ub = [A.sb([128, 128], F32) for _ in range(2)]

    def mm4(out_bank, bank_idx, lhs_fn, rhs_fn, reads):
        for j in range(G):
            l_, r_ = lhs_fn(j), rhs_fn(j)
            S.pe(lambda e, j=j, l_=l_, r_=r_: e.matmul(out=out_bank[:, j * 128:(j + 1) * 128], lhsT=l_, rhs=r_, start=True, stop=True),
                 reads=reads, writes=[("ps", bank_idx)])

    def v4(bank):
        return bank[:, 0:G * 128].rearrange("p (a b) -> p a b", a=G)
    for c0 in range(0, NB, G):
        blk = lambda j: slice((c0 + j) * 128, (c0 + j + 1) * 128)
        mm4(PS[0], 0, lambda j: kT[:, blk(j)], lambda j: kT[:, blk(j)], [("B", 2)])
        S.dve(lambda e, c0=c0: e.tensor_tensor(out=X["Ag"][:], in0=v4(PS[0]), in1=Dm[:, c0:c0 + G, :], op=ALU.mult),
              reads=[("ps", 0), ("B", 6)], writes=[("Ag",)])
        for j in range(G):
            S.pe(lambda e, j=j: e.transpose(out=PS[2][:, j * 128:(j + 1) * 128], in_=X["Ag"][:, j, :], identity=ident),
                 reads=[("Ag",), ("consts",)], writes=[("ps", 2)])
        S.act(lambda e: e.copy(out=X["ATg"][:], in_=v4(PS[2])), reads=[("ps", 2)], writes=[("ATg",)])
        S.dve(lambda e: e.tensor_tensor(out=X["Rg"][:], in0=bc(ident.unsqueeze(1), [128, G, 128]), in1=X["ATg"][:], op=ALU.subtract),
              reads=[("consts",), ("ATg",)], writes=[("Rg",)])
        Pc, PTc = "Ag", "ATg"
        for lvl in range(1, 7):
            Pn = "P%d" % (lvl % 2)
            PTn = "PT%d" % (lvl % 2)
            mm4(PS[3], 3, lambda j, PTc=PTc: X[PTc][:, j, :], lambda j, Pc=Pc: X[Pc][:, j, :], [(Pc,), (PTc,)])
            S.act(lambda e, Pn=Pn: e.copy(out=X[Pn][:], in_=v4(PS[3])), reads=[("ps", 3)], writes=[(Pn,)])
            if lvl < 6:
                mm4(PS[4], 4, lambda j, Pc=Pc: X[Pc][:, j, :], lambda j, PTc=PTc: X[PTc][:, j, :], [(Pc,), (PTc,)])
                S.act(lambda e, PTn=PTn: e.copy(out=X[PTn][:], in_=v4(PS[4])), reads=[("ps", 4)], writes=[(PTn,)])
            mm4(PS[5], 5, lambda j, Pn=Pn: X[Pn][:, j, :], lambda j: X["Rg"][:, j, :], [(Pn,), ("Rg",)])
            S.dve(lambda e: e.tensor_tensor(out=X["Rg"][:], in0=X["Rg"][:], in1=v4(PS[5]), op=ALU.add), reads=[("Rg",), ("ps", 5)], writes=[("Rg",)])
            Pc, PTc = Pn, PTn
        mm4(PS[1], 1, lambda j: kT[:, blk(j)], lambda j: qT[:, blk(j)], [("B", 2), ("B", 1)])
        S.dve(lambda e, c0=c0: e.tensor_tensor(out=X["attnT"][:], in0=v4(PS[1]), in1=DTm[:, c0:c0 + G, :], op=ALU.mult),
              reads=[("ps", 1), ("B", 7)], writes=[("attnT",)])
        S.pool(lambda e, c0=c0: e.tensor_tensor(out=X["kbe"][:], in0=ktok[:, c0:c0 + G, :], in1=bc(be[:, c0:c0 + G].unsqueeze(2), [128, G, 128]), op=ALU.mult),
               reads=[("B", 4), ("ti",)], writes=[("kbe",)])
        S.pool(lambda e, c0=c0: e.tensor_tensor(out=X["vb"][:], in0=vtok[:, c0:c0 + G, :], in1=bc(beta[:, c0:c0 + G].unsqueeze(2), [128, G, 128]), op=ALU.mult),
               reads=[("B", 0), ("gbeta",)], writes=[("vb",)])
        S.pool(lambda e, c0=c0: e.tensor_tensor(out=X["kend"][:], in0=ktok[:, c0:c0 + G, :], in1=bc(kes[:, c0:c0 + G].unsqueeze(2), [128, G, 128]), op=ALU.mult),
               reads=[("B", 4), ("tf",)], writes=[("kend",)])
        mm4(PS[0], 0, lambda j: X["Rg"][:, j, :], lambda j: X["vb"][:, j, :], [("Rg",), ("vb",)])
        S.act(lambda e: e.copy(out=X["Wval"][:], in_=v4(PS[0])), reads=[("ps", 0)], writes=[("Wval",)])
        mm4(PS[1], 1, lambda j: X["kbe"][:, j, :], lambda j: X["Rg"][:, j, :], [("kbe",), ("Rg",)])
        S.act(lambda e: e.copy(out=X["WkT"][:], in_=v4(PS[1])), reads=[("ps", 1)], writes=[("WkT",)])
        for j in range(G):
            c = c0 + j
            u = ub[c % 2]
            uk = ("g_u", c % 2)
            S.pe(lambda e, j=j: e.matmul(out=PS[6][:, 0:128], lhsT=X["WkT"][:, j, :], rhs=Sg[:], start=True, stop=True),
                 reads=[("WkT",), ("g_S",)], writes=[("ps", 6)])
            S.dve(lambda e, j=j, u=u: e.tensor_tensor(out=u[:], in0=X["Wval"][:, j, :], in1=PS[6][:, 0:128], op=ALU.subtract),
                  reads=[("Wval",), ("ps", 6)], writes=[uk])
            S.pe(lambda e, c=c: e.matmul(out=PS[6][:, 128:256], lhsT=qdT[:, c * 128:(c + 1) * 128], rhs=Sg[:], start=True, stop=False),
                 reads=[("B", 3), ("g_S",)], writes=[("ps", 6)])
            S.pe(lambda e, j=j, u=u: e.matmul(out=PS[6][:, 128:256], lhsT=X["attnT"][:, j, :], rhs=u[:], start=False, stop=True),
                 reads=[("attnT",), uk], writes=[("ps", 6)])
            S.act(lambda e, c=c: e.copy(out=oall[:, c, :], in_=PS[6][:, 128:256]), reads=[("ps", 6)], writes=[("B", 8, c)])
            S.pe(lambda e, j=j, u=u: e.matmul(out=PS[7][:, 0:128], lhsT=X["kend"][:, j, :], rhs=u[:], start=True, stop=True),
                 reads=[("kend",), uk], writes=[("ps", 7)])
            S.dve(lambda e, c=c: e.scalar_tensor_tensor(out=Sg[:], in0=Sg[:], scalar=egl[:, c:c + 1], in1=PS[7][:, 0:128], op0=ALU.mult, op1=ALU.add),
                  reads=[("g_S",), ("lf",), ("ps", 7)], writes=[("g_S",)])
    sq2 = B.v(6, NB, 128)
    ssq = T["tmp"]
    S.act(lambda e: e.activation(out=sq2, in_=oall, func=AF.Square), reads=[("B", 8)], writes=[("B", 6)])
    S.dve(lambda e: e.tensor_reduce(out=ssq[:], in_=sq2, axis=AX.X, op=ALU.add), reads=[("B", 6)], writes=[("tmp",)])
    S.dve(lambda e: e.tensor_scalar(out=ssq[:], in0=ssq[:], scalar1=1.0 / 128, scalar2=EPS, op0=ALU.mult, op1=ALU.add), reads=[("tmp",)], writes=[("tmp",)])
    S.act(lambda e: e.activation(out=ssq[:], in_=ssq[:], func=AF.Sqrt), reads=[("tmp",)], writes=[("tmp",)])
    S.dve(lambda e: e.reciprocal(out=ssq[:], in_=ssq[:]), reads=[("tmp",)], writes=[("tmp",)])
    S.dve(lambda e: e.tensor_tensor(out=oall, in0=oall, in1=bc(ssq[:].unsqueeze(2), [128, NB, 128]), op=ALU.mult), reads=[("B", 8), ("tmp",)], writes=[("B", 8)])
    S.dve(lambda e: e.tensor_tensor(out=oall, in0=oall, in1=bc(R["normw"][:, 3, :].unsqueeze(1), [128, NB, 128]), op=ALU.mult),
          reads=[("B", 8), ("normw",)], writes=[("B", 8)])
    S.act(lambda e: e.activation(out=zt, in_=zt, func=AF.Silu), reads=[("B", 5)], writes=[("B", 5)])
    S.dve(lambda e: e.tensor_tensor(out=oall, in0=oall, in1=zt, op=ALU.mult), reads=[("B", 8), ("B", 5)], writes=[("B", 8)])
    S.dma("sp", lambda e: e.dma_start(out=yv[:, :, 384:512], in_=oall), reads=[("B", 8)], writes=[("ytok_d", 3)])


def pre_tiles(A):
    hst = [A.sb([128, 16, 128], BF16) for _ in range(2)]
    ss = [A.sb([128, 1], F32) for _ in range(2)]
    rs = [A.sb([128, 1], F32) for _ in range(2)]
    stf = [A.sb([128, 512], F32) for _ in range(2)]
    stb = [A.sb([128, 512], BF16) for _ in range(2)]
    stt_ = [A.sb([128, 896], F32) for _ in range(2)]
    return hst, ss, rs, stf, stb, stt_


def pre_phase(S, A, NB, R, PS, x, nw, w, dr, add_src=None, store_dst=None, xkey=None):
    C = R["consts"]
    B = R["big"]
    ident = C[:, 0, :]
    SEQ = NB * 128
    hTv = dr["hTd"].rearrange("k p s -> p k s")
    nwB = R["big"].t[8][:, 0:2048]
    hst, ss, rs, stf, stb, stt_ = R["pre_tiles"]
    S.dma("sp", lambda e: e.dma_start(out=nwB, in_=nw.partition_broadcast(128)), reads=[("B", 8)], writes=[("nwB",), ("B", 8)])
    wv = w.rearrange("(kc p) n -> p kc n", p=128)
    Wb = []
    for q in range(4):
        t = B.t[4 + q][:, 0:4096].bitcast(BF16).rearrange("p (a b) -> p a b", a=4)
        Wb.append(t)
        for h2 in range(2):
            S.dma("pool", lambda e, t=t, q=q, h2=h2: e.dma_start(out=t[:, 2 * h2:2 * h2 + 2, :], in_=wv[:, 4 * q + 2 * h2:4 * q + 2 * h2 + 2, :]),
                  writes=[("B", 4 + q, h2)])
    ev = [0]

    def stage_a(tt):
        b = tt % 2
        xt = B.t[b][:, 0:2048]
        hf = B.t[2 + b][:, 0:2048]
        ssb, rsb = ss[b], rs[b]
        S.dma("sp", lambda e: e.dma_start(out=xt, in_=x[tt * 128:(tt + 1) * 128, :]), reads=([xkey] if xkey else []), writes=[("B", b)])
        if add_src is not None:
            S.dma("sp", lambda e: e.dma_start(out=hf, in_=add_src[tt * 128:(tt + 1) * 128, :]), reads=[("sumb", tt // 4)], writes=[("B", 2 + b)])
            S.dve(lambda e: e.tensor_tensor(out=xt, in0=xt, in1=hf, op=ALU.add), reads=[("B", b), ("B", 2 + b)], writes=[("B", b)])
            S.dma("sp", lambda e: e.dma_start(out=store_dst[tt * 128:(tt + 1) * 128, :], in_=xt), reads=[("B", b)], writes=[("xa_d", tt)])
        S.act(lambda e: e.activation(out=hf, in_=xt, func=AF.Square, accum_out=ssb[:]), reads=[("B", b)], writes=[("B", 2 + b), ("ss", b)])
        S.dve(lambda e: e.tensor_scalar(out=rsb[:], in0=ssb[:], scalar1=1.0 / D_MODEL, scalar2=EPS, op0=ALU.mult, op1=ALU.add), reads=[("ss", b)], writes=[("rs", b)])
        S.act(lambda e: e.activation(out=rsb[:], in_=rsb[:], func=AF.Sqrt), reads=[("rs", b)], writes=[("rs", b)])
        S.dve(lambda e: e.reciprocal(out=rsb[:], in_=rsb[:]), reads=[("rs", b)], writes=[("rs", b)])
        S.dve(lambda e: e.scalar_tensor_tensor(out=hf, in0=xt, scalar=rsb[:], in1=nwB, op0=ALU.mult, op1=ALU.mult),
              reads=[("B", b), ("rs", b), ("nwB",)], writes=[("B", 2 + b)])

    def stage_b(tt):
        b = tt % 2
        hf = B.t[2 + b][:, 0:2048]
        for g in range(4):
            pb = g % 2
            for j in range(4):
                kc = g * 4 + j
                S.pe(lambda e, pb=pb, j=j, kc=kc: e.transpose(out=PS[pb][:, j * 128:(j + 1) * 128], in_=hf[:, kc * 128:(kc + 1) * 128], identity=ident),
                     reads=[("B", 2 + b), ("consts",)], writes=[("ps", pb)])
            src = PS[pb][:, 0:512].rearrange("p (a b) -> p a b", a=4)
            dst = hst[b][:, g * 4:(g + 1) * 4, :]
            if ev[0] % 2 == 0:
                S.act(lambda e, src=src, dst=dst: e.copy(out=dst, in_=src), reads=[("ps", pb)], writes=[("hst", b, g)])
            else:
                S.dve(lambda e, src=src, dst=dst: e.tensor_copy(out=dst, in_=src), reads=[("ps", pb)], writes=[("hst", b, g)])
            ev[0] += 1
        S.dma("sp", lambda e: e.dma_start(out=hTv[:, :, tt * 128:(tt + 1) * 128], in_=hst[b][:]), reads=[("hst", b)], writes=[("hTd", tt)])
    for t in range(NB + 1):
        if t < NB:
            stage_a(t)
        if t >= 1:
            stage_b(t - 1)
    tokv = dr["tokm"]
    it = 0
    for sb in range(SEQ // 512):
        hs = B.t[sb % 2][:, 0:4096].bitcast(BF16).rearrange("p (a b) -> p a b", a=16)
        hk = ("B", sb % 2)
        S.dma("sp", lambda e, sb=sb, hs=hs: e.dma_start(out=hs, in_=hTv[:, :, sb * 512:(sb + 1) * 512]), reads=[("hTd",)], writes=[hk])
        cs = slice(sb * 512, (sb + 1) * 512)
        for fg in range(9):
            pb = 2 + fg % 2
            for kc in range(16):
                S.pe(lambda e, pb=pb, kc=kc, fg=fg, hs=hs: e.matmul(out=PS[pb][:, 0:512], lhsT=Wb[kc // 4][:, kc % 4, fg * 128:(fg + 1) * 128],
                                                                  rhs=hs[:, kc, :], start=(kc == 0), stop=(kc == 15)),
                     reads=[("B", 4 + kc // 4), hk], writes=[("ps", pb)])
            it += 1
            if 2 <= fg <= 5:
                st_ = stb[it % 2]
                S.act(lambda e, pb=pb, st_=st_: e.copy(out=st_[:], in_=PS[pb][:, 0:512]), reads=[("ps", pb)], writes=[("stb", it % 2)])
                S.dma("sp", lambda e, st_=st_, fg=fg, cs=cs: e.dma_start(out=dr["f_qkT"][fg - 2][:, cs], in_=st_[:]), reads=[("stb", it % 2)], writes=[("f_qkT_d", sb, fg)])
            else:
                st_ = stf[it % 2]
                dst = dr["m_qT"] if fg == 0 else dr["m_kT"] if fg == 1 else dr["g_T"][fg - 6]
                S.dve(lambda e, pb=pb, st_=st_: e.tensor_copy(out=st_[:], in_=PS[pb][:, 0:512]), reads=[("ps", pb)], writes=[("stf", it % 2)])
                S.dma("sp", lambda e, st_=st_, dst=dst, cs=cs: e.dma_start(out=dst[:, cs], in_=st_[:]), reads=[("stf", it % 2)], writes=[("fm_d", sb, fg)])
        for t4 in range(4):
            blk = sb * 4 + t4
            st_ = stt_[blk % 2]
            for (pb, c0, cw, o0) in ((4, 1152, 512, 0), (5, 1664, 384, 512)):
                for kc in range(16):
                    S.pe(lambda e, pb=pb, kc=kc, c0=c0, cw=cw, t4=t4, hs=hs: e.matmul(out=PS[pb][:, 0:cw], lhsT=hs[:, kc, t4 * 128:(t4 + 1) * 128],
                                                                                   rhs=Wb[kc // 4][:, kc % 4, c0:c0 + cw], start=(kc == 0), stop=(kc == 15)),
                         reads=[("B", 4 + kc // 4), hk], writes=[("ps", pb)])
                if pb == 4:
                    S.act(lambda e, pb=pb, st_=st_, cw=cw, o0=o0: e.copy(out=st_[:, o0:o0 + cw], in_=PS[pb][:, 0:cw]), reads=[("ps", pb)], writes=[("stt", blk % 2, 0)])
                else:
                    S.dve(lambda e, pb=pb, st_=st_, cw=cw, o0=o0: e.tensor_copy(out=st_[:, o0:o0 + cw], in_=PS[pb][:, 0:cw]), reads=[("ps", pb)], writes=[("stt", blk % 2, 1)])
            S.dma("sp", lambda e, st_=st_, blk=blk: e.dma_start(out=tokv[blk * 128:(blk + 1) * 128, :], in_=st_[:, 0:768]), reads=[("stt", blk % 2)], writes=[("tokm_d", blk)])
            S.act(lambda e, st_=st_, blk=blk: e.copy(out=R["gates"][:, blk, :], in_=st_[:, 768:774]), reads=[("stt", blk % 2)], writes=[("gates", blk)])


def build_A(NB=32):
    nc = bass.Bass("TRN2", target_bir_lowering=False)
    SEQ = NB * 128
    x = nc.dram_tensor("x", [SEQ, D_MODEL], F32, kind="ExternalInput").ap()
    nw = nc.dram_tensor("nw", [D_MODEL], F32, kind="ExternalInput").ap()
    w = nc.dram_tensor("w", [D_MODEL, 2048], F32, kind="ExternalInput").ap()
    params = nc.dram_tensor("params", [8], F32, kind="ExternalInput").ap()
    normw_d = nc.dram_tensor("normw", [4 * 128], F32, kind="ExternalInput").ap()
    convw_d = nc.dram_tensor("convw", [128, 12], F32, kind="ExternalInput").ap()
    consts_d = nc.dram_tensor("consts", [128, 5, 128], F32, kind="ExternalInput").ap()
    ytok = nc.dram_tensor("ytok", [SEQ, 512], F32, kind="ExternalOutput").ap()
    dr = {}
    dr["hTd"] = nc.dram_tensor("hTd", [16, 128, SEQ], BF16, kind="Internal").ap()
    dr["m_qT"] = nc.dram_tensor("m_qT", [128, SEQ], F32, kind="Internal").ap()
    dr["m_kT"] = nc.dram_tensor("m_kT", [128, SEQ], F32, kind="Internal").ap()
    dr["tokm"] = nc.dram_tensor("tokm", [SEQ, 768], F32, kind="Internal").ap()
    dr["f_qkT"] = nc.dram_tensor("f_qkT", [4, 128, SEQ], BF16, kind="Internal").ap()
    dr["g_T"] = nc.dram_tensor("g_T", [3, 128, SEQ], F32, kind="Internal").ap()
    S = Sched(nc)
    with contextlib.ExitStack() as st:
        A = Alloc(nc, st)
        R = {}
        R["consts"] = A.sb([128, 5, 128], F32)
        R["parB"] = A.sb([128, 8], F32)
        R["gates"] = A.sb([128, NB, 6], F32)
        R["normw"] = A.sb([128, 4, 128], F32)
        R["convw"] = A.sb([128, 12], F32)
        R["big"] = Big(A, 9)
        R["pre_tiles"] = pre_tiles(A)
        PS = [A.ps([128, 512], F32) for _ in range(8)]
        S.dma("sp", lambda e: e.dma_start(out=R["consts"][:], in_=consts_d), writes=[("consts",)])
        S.dma("sp", lambda e: e.dma_start(out=R["parB"][:], in_=params.partition_broadcast(128)), writes=[("parB",)])
        S.dma("sp", lambda e: e.dma_start(out=R["normw"][:].rearrange("p a b -> p (a b)"), in_=normw_d.partition_broadcast(128)), writes=[("normw",)])
        S.dma("sp", lambda e: e.dma_start(out=R["convw"][:], in_=convw_d), writes=[("convw",)])
        pre_phase(S, A, NB, R, PS, x, nw, w, dr)
        for k in ("m_qT", "m_kT", "tokm", "f_qkT", "g_T"):
            pass
        T = mixer_prelude(S, A, NB, R)
        mlstm_phase(S, A, NB, R, T, PS, dr, ytok)
        fox_phase(S, A, NB, R, T, PS, dr, ytok)
        gdn_phase(S, A, NB, R, T, PS, dr, ytok)
        S.emit()
    return nc


def build_B(NTOK=8192, FINAL=False):
    nc = bass.Bass("TRN2", target_bir_lowering=False)
    D, F = D_MODEL, D_FF
    x = nc.dram_tensor("x", [NTOK, D], F32, kind="ExternalInput").ap()
    y = nc.dram_tensor("y", [NTOK, D], F32, kind="ExternalInput").ap()
    wo = nc.dram_tensor("wo", [D, D], F32, kind="ExternalInput").ap()
    wg = nc.dram_tensor("wg", [D, F], F32, kind="ExternalInput").ap()
    wu = nc.dram_tensor("wu", [D, F], F32, kind="ExternalInput").ap()
    wd = nc.dram_tensor("wd", [F, D], F32, kind="ExternalInput").ap()
    nw2 = nc.dram_tensor("nw2", [D], F32, kind="ExternalInput").ap()
    fnw = nc.dram_tensor("fnw", [D], F32, kind="ExternalInput").ap()
    identd = nc.dram_tensor("ident", [128, 128], F32, kind="ExternalInput").ap()
    xo = nc.dram_tensor("xo", [NTOK, D], F32, kind="ExternalOutput").ap()
    FC = F // 128
    S = Sched(nc)
    with contextlib.ExitStack() as st:
        A = Alloc(nc, st)
        ident = A.sb([128, 128], F32)
        nwB = A.sb([128, D], F32)
        fnB = A.sb([128, D], F32)
        x1 = A.sb([128, 4, D], F32)
        yt = [A.sb([128, D], F32) for _ in range(2)]
        yT = A.sb([128, 16, 512], BF16)
        hT = A.sb([128, 16, 512], BF16)
        aT = A.sb([128, FC, 512], BF16)
        wot = [A.sb([128, 16, 512], BF16) for _ in range(1)]
        wgt = [A.sb([128, 16, 128], BF16) for _ in range(2)]
        wut = [A.sb([128, 16, 128], BF16) for _ in range(2)]
        wdt = [A.sb([128, FC, 256], BF16) for _ in range(1)]
        sg = [A.sb([128, 512], BF16) for _ in range(2)]
        ss = A.sb([128, 1], F32)
        rs = A.sb([128, 1], F32)
        PS = [A.ps([128, 512], F32) for _ in range(8)]
        S.dma("sp", lambda e: e.dma_start(out=ident[:], in_=identd), writes=[("ident",)])
        S.dma("sp", lambda e: e.dma_start(out=nwB[:], in_=nw2.partition_broadcast(128)), writes=[("nwB",)])
        S.dma("sp", lambda e: e.dma_start(out=fnB[:], in_=fnw.partition_broadcast(128)), writes=[("fnB",)])
        wov = wo.rearrange("(kc p) n -> p kc n", p=128)
        wgv = wg.rearrange("(kc p) n -> p kc n", p=128)
        wuv = wu.rearrange("(kc p) n -> p kc n", p=128)
        wdv = wd.rearrange("(fc p) n -> p fc n", p=128)
        ev = [0]

        def transposes(src, srckey, dstT, dstkey, tt):
            for g in range(4):
                pb = g % 2
                for j in range(4):
                    kc = g * 4 + j
                    S.pe(lambda e, pb=pb, j=j, kc=kc: e.transpose(out=PS[pb][:, j * 128:(j + 1) * 128], in_=src[:, kc * 128:(kc + 1) * 128], identity=ident[:]),
                         reads=[srckey, ("ident",)], writes=[("ps", pb)])
                s_ = PS[pb][:, 0:512].rearrange("p (a b) -> p a b", a=4)
                d_ = dstT[:, g * 4:(g + 1) * 4, tt * 128:(tt + 1) * 128]
                if ev[0] % 2 == 0:
                    S.act(lambda e, s_=s_, d_=d_: e.copy(out=d_, in_=s_), reads=[("ps", pb)], writes=[dstkey + (tt, g)])
                else:
                    S.dve(lambda e, s_=s_, d_=d_: e.tensor_copy(out=d_, in_=s_), reads=[("ps", pb)], writes=[dstkey + (tt, g)])
                ev[0] += 1

        def rms(src, srckey, wB, wkey, dst, dstkey):
            S.act(lambda e: e.activation(out=dst, in_=src, func=AF.Square, accum_out=ss[:]), reads=[srckey], writes=[dstkey, ("ss",)])
            S.dve(lambda e: e.tensor_scalar(out=rs[:], in0=ss[:], scalar1=1.0 / D, scalar2=EPS, op0=ALU.mult, op1=ALU.add), reads=[("ss",)], writes=[("rs",)])
            S.act(lambda e: e.activation(out=rs[:], in_=rs[:], func=AF.Sqrt), reads=[("rs",)], writes=[("rs",)])
            S.dve(lambda e: e.reciprocal(out=rs[:], in_=rs[:]), reads=[("rs",)], writes=[("rs",)])
            S.dve(lambda e: e.scalar_tensor_tensor(out=dst, in0=src, scalar=rs[:], in1=wB[:], op0=ALU.mult, op1=ALU.mult),
                  reads=[srckey, ("rs",), wkey], writes=[dstkey])
        it = 0
        for sl in range(NTOK // 512):
            s0 = sl * 512
            for tt in range(4):
                r0 = s0 + tt * 128
                b = tt % 2
                S.dma("sp", lambda e, r0=r0, b=b: e.dma_start(out=yt[b][:], in_=y[r0:r0 + 128, :]), writes=[("yt", b)])
                S.dma("sp", lambda e, r0=r0, tt=tt: e.dma_start(out=x1[:, tt, :], in_=x[r0:r0 + 128, :]), writes=[("x1", tt)])
                transposes(yt[b], ("yt", b), yT, ("yT",), tt)
            for ct in range(4):
                S.dma("pool", lambda e, ct=ct: e.dma_start(out=wot[0][:], in_=wov[:, :, ct * 512:(ct + 1) * 512]), writes=[("wot",)])
                for tt in range(4):
                    pb = 2 + it % 2
                    it += 1
                    for kc in range(16):
                        S.pe(lambda e, pb=pb, kc=kc, tt=tt: e.matmul(out=PS[pb][:, 0:512], lhsT=yT[:, kc, tt * 128:(tt + 1) * 128], rhs=wot[0][:, kc, :],
                                                                   start=(kc == 0), stop=(kc == 15)),
                             reads=[("yT", tt), ("wot",)], writes=[("ps", pb)])
                    S.dve(lambda e, pb=pb, tt=tt, ct=ct: e.tensor_tensor(out=x1[:, tt, ct * 512:(ct + 1) * 512], in0=x1[:, tt, ct * 512:(ct + 1) * 512],
                                                                       in1=PS[pb][:, 0:512], op=ALU.add),
                          reads=[("x1", tt), ("ps", pb)], writes=[("x1", tt)])
            for tt in range(4):
                b = tt % 2
                rms(x1[:, tt, :], ("x1", tt), nwB, ("nwB",), yt[b][:], ("yt", b))
                transposes(yt[b], ("yt", b), hT, ("hT",), tt)
            for fc in range(FC):
                b = fc % 2
                S.dma("pool", lambda e, fc=fc, b=b: e.dma_start(out=wgt[b][:], in_=wgv[:, :, fc * 128:(fc + 1) * 128]), writes=[("wgt", b)])
                S.dma("pool", lambda e, fc=fc, b=b: e.dma_start(out=wut[b][:], in_=wuv[:, :, fc * 128:(fc + 1) * 128]), writes=[("wut", b)])
                pg, pu = 4 + b, 6 + b
                for kc in range(16):
                    S.pe(lambda e, pg=pg, kc=kc, b=b: e.matmul(out=PS[pg][:, 0:512], lhsT=wgt[b][:, kc, :], rhs=hT[:, kc, :], start=(kc == 0), stop=(kc == 15)),
                         reads=[("wgt", b), ("hT",)], writes=[("ps", pg)])
                for kc in range(16):
                    S.pe(lambda e, pu=pu, kc=kc, b=b: e.matmul(out=PS[pu][:, 0:512], lhsT=wut[b][:, kc, :], rhs=hT[:, kc, :], start=(kc == 0), stop=(kc == 15)),
                         reads=[("wut", b), ("hT",)], writes=[("ps", pu)])
                S.act(lambda e, pg=pg, b=b: e.activation(out=sg[b][:], in_=PS[pg][:, 0:512], func=AF.Silu), reads=[("ps", pg)], writes=[("sg", b)])
                S.dve(lambda e, pu=pu, b=b, fc=fc: e.tensor_tensor(out=aT[:, fc, :], in0=sg[b][:], in1=PS[pu][:, 0:512], op=ALU.mult),
                      reads=[("sg", b), ("ps", pu)], writes=[("aT", fc)])
            for ct in range(8):
                S.dma("pool", lambda e, ct=ct: e.dma_start(out=wdt[0][:], in_=wdv[:, :, ct * 256:(ct + 1) * 256]), writes=[("wdt",)])
                for tt in range(4):
                    pb = 2 + it % 2
                    it += 1
                    for fc in range(FC):
                        S.pe(lambda e, pb=pb, fc=fc, tt=tt: e.matmul(out=PS[pb][:, 0:256], lhsT=aT[:, fc, tt * 128:(tt + 1) * 128], rhs=wdt[0][:, fc, :],
                                                                   start=(fc == 0), stop=(fc == FC - 1)),
                             reads=[("aT",), ("wdt",)], writes=[("ps", pb)])
                    S.dve(lambda e, pb=pb, tt=tt, ct=ct: e.tensor_tensor(out=x1[:, tt, ct * 256:(ct + 1) * 256], in0=x1[:, tt, ct * 256:(ct + 1) * 256],
                                                                       in1=PS[pb][:, 0:256], op=ALU.add),
                          reads=[("x1", tt), ("ps", pb)], writes=[("x1", tt)])
            for tt in range(4):
                r0 = s0 + tt * 128
                b = tt % 2
                if FINAL:
                    rms(x1[:, tt, :], ("x1", tt), fnB, ("fnB",), yt[b][:], ("yt", b))
                    S.dma("sp", lambda e, r0=r0, b=b: e.dma_start(out=xo[r0:r0 + 128, :], in_=yt[b][:]), reads=[("yt", b)])
                else:
                    S.dma("sp", lambda e, r0=r0, tt=tt: e.dma_start(out=xo[r0:r0 + 128, :], in_=x1[:, tt, :]), reads=[("x1", tt)])
        S.emit()
    return nc


_OFF = dict(mq=0, mk=512, mv=1024, mo=1536, mi=2048, mf=2052, fq=2056, fk=3080, fv=4104, ff=5128, gq=5136, gk=5648, gv=6160, gz=6672, ga=7184, gb=7188)


def _wslice(w_in, g):
    c = lambda n, h: list(range(_OFF[n] + 128 * h, _OFF[n] + 128 * (h + 1)))
    cols = (c("mq", g) + c("mk", g) + c("fq", 2 * g) + c("fq", 2 * g + 1) + c("fk", 2 * g) + c("fk", 2 * g + 1) + c("gq", g) + c("gk", g) + c("gv", g)
            + c("mk", g) + c("mv", g) + c("mo", g) + c("fv", 2 * g) + c("fv", 2 * g + 1) + c("gz", g)
            + [_OFF["mi"] + g, _OFF["mf"] + g, _OFF["ff"] + 2 * g, _OFF["ff"] + 2 * g + 1, _OFF["ga"] + g, _OFF["gb"] + g])
    out = np.zeros((D_MODEL, 2048), np.float32)
    out[:, :len(cols)] = w_in[:, cols]
    return out


_CACHE = {}


def kernel_unfused(x, mix_norm_w, w_in, mlstm_i_bias, mlstm_f_bias, fox_f_bias, gdn_conv_w, gdn_a_log, gdn_dt_bias,
           mlstm_out_norm_w, fox_out_norm_w, gdn_out_norm_w, w_out, ffn_norm_w, w_gate, w_up, w_down, final_norm_w):
    f = lambda a: np.ascontiguousarray(np.asarray(a, dtype=np.float32))
    x = f(x)
    Bsz, SEQ, D = x.shape
    cur = x.reshape(Bsz * SEQ, D)
    consts = consts_np()
    if "A" not in _CACHE:
        _CACHE["A"] = build_A(SEQ // 128)
    depth = np.asarray(w_in).shape[0]
    for l in range(depth):
        in_maps = []
        for c in range(8):
            b, g = c // 4, c % 4
            sl = lambda a, h: f(a)[l][128 * h:128 * (h + 1)]
            cw = f(gdn_conv_w)[l]
            in_maps.append({
                "x": np.ascontiguousarray(cur[b * SEQ:(b + 1) * SEQ]),
                "nw": f(mix_norm_w)[l],
                "w": _wslice(f(w_in)[l], g),
                "params": np.array([f(mlstm_i_bias)[l][g], f(mlstm_f_bias)[l][g], f(fox_f_bias)[l][2 * g], f(fox_f_bias)[l][2 * g + 1],
                                    f(gdn_a_log)[l][g], f(gdn_dt_bias)[l][g], 0, 0], np.float32),
                "normw": np.concatenate([sl(mlstm_out_norm_w, g), sl(fox_out_norm_w, 2 * g), sl(fox_out_norm_w, 2 * g + 1), sl(gdn_out_norm_w, g)]),
                "convw": np.ascontiguousarray(np.stack([cw[:, t * 512 + g * 128:t * 512 + (g + 1) * 128].T for t in range(3)], 1).reshape(128, 12)),
                "consts": consts,
            })
        res = run_bass_kernel_spmd(_CACHE["A"], in_maps, core_ids=list(range(8)))
        y = np.zeros((Bsz * SEQ, D), np.float32)
        for c in range(8):
            b, g = c // 4, c % 4
            yt = res.results[c]["ytok"]
            rows = slice(b * SEQ, (b + 1) * SEQ)
            y[rows, g * 128:(g + 1) * 128] = yt[:, 0:128]
            y[rows, 512 + g * 256:512 + (g + 1) * 256] = yt[:, 128:384]
            y[rows, 1536 + g * 128:1536 + (g + 1) * 128] = yt[:, 384:512]
        final = (l == depth - 1)
        key = "B%d" % int(final)
        if key not in _CACHE:
            _CACHE[key] = build_B(Bsz * SEQ, FINAL=final)
        resb = run_bass_kernel_spmd(_CACHE[key], [{
            "x": np.ascontiguousarray(cur), "y": y, "wo": f(w_out)[l], "wg": f(w_gate)[l], "wu": f(w_up)[l], "wd": f(w_down)[l],
            "nw2": f(ffn_norm_w)[l], "fnw": f(final_norm_w), "ident": np.eye(128, dtype=np.float32)}], core_ids=[0])
        cur = resb.results[0]["xo"]
    return np.ascontiguousarray(cur.reshape(Bsz, SEQ, D).astype(np.float32))


def allreduce_chunk(S, dr, k):
    rows = slice(k * 512, (k + 1) * 512)
    S.cc(lambda e: e.collective_compute("AllReduce", ALU.add, replica_groups=G4, ins=[dr["part"][rows, :]], outs=[dr["sumb"][rows, :]]),
         reads=[("part_d", 4 * k + j) for j in range(4)], writes=[("sumb", k)])


def wo_phase(S, A, NB, R, PS, wo_l, dr):
    B = R["big"]
    ident = R["consts"][:, 0, :]
    A.begin("wo")
    yT = [A.sb([128, 4, 128], BF16) for _ in range(2)]
    A.end()
    hst, ss, rs, stf, stb, stt_ = R["pre_tiles"]
    wob = B.t[0][:, 0:4096].bitcast(BF16).rearrange("p (a b) -> p a b", a=4)
    S.dma("pool", lambda e: e.dma_start(out=wob, in_=wo_l.rearrange("(kc p) n -> p kc n", p=128)), writes=[("B", 0)])
    it = 0
    for tt in range(NB):
        b = tt % 2
        yt = B.t[1 + b][:, 0:512]
        S.dma("sp", lambda e, tt=tt, yt=yt: e.dma_start(out=yt, in_=dr["ytok_d"][tt * 128:(tt + 1) * 128, :]), reads=[("ytok_d",)], writes=[("B", 1 + b)])
        for j in range(4):
            S.pe(lambda e, j=j, yt=yt, b=b: e.transpose(out=PS[b][:, j * 128:(j + 1) * 128], in_=yt[:, j * 128:(j + 1) * 128], identity=ident),
                 reads=[("B", 1 + b), ("consts",)], writes=[("ps", b)])
        S.act(lambda e, b=b: e.copy(out=yT[b][:], in_=PS[b][:, 0:512].rearrange("p (a b) -> p a b", a=4)), reads=[("ps", b)], writes=[("yT", b)])
        for ct in range(4):
            pb = 2 + it % 4
            st_ = stf[it % 2]
            for kc in range(4):
                S.pe(lambda e, pb=pb, kc=kc, ct=ct, b=b: e.matmul(out=PS[pb][:, 0:512], lhsT=yT[b][:, kc, :], rhs=wob[:, kc, ct * 512:(ct + 1) * 512],
                                                                start=(kc == 0), stop=(kc == 3)),
                     reads=[("yT", b), ("B", 0)], writes=[("ps", pb)])
            if it % 2 == 0:
                S.act(lambda e, pb=pb, st_=st_: e.copy(out=st_[:], in_=PS[pb][:, 0:512]), reads=[("ps", pb)], writes=[("stf", it % 2)])
            else:
                S.dve(lambda e, pb=pb, st_=st_: e.tensor_copy(out=st_[:], in_=PS[pb][:, 0:512]), reads=[("ps", pb)], writes=[("stf", it % 2)])
            S.dma("sp", lambda e, st_=st_, tt=tt, ct=ct: e.dma_start(out=dr["part"][tt * 128:(tt + 1) * 128, ct * 512:(ct + 1) * 512], in_=st_[:]),
                  reads=[("stf", it % 2)], writes=[("part_d", tt, ct)])
            it += 1
        if tt % 4 == 3:
            allreduce_chunk(S, dr, tt // 4)


def ffn_precast(S, wg_l, wu_l, wd_l, dr):
    for (src, dst, key) in ((wg_l, dr["wg_bf"], "wg_bf"), (wu_l, dr["wu_bf"], "wu_bf"), (wd_l, dr["wd_bf"], "wd_bf")):
        S.dma("pool", lambda e, src=src, dst=dst: e.dma_start(out=dst, in_=src), reads=[(key,)], writes=[(key,)])


def ffn_phase(S, A, NB, R, PS, xsrc, xkey, xdst, nw2_l, wg_l, wu_l, wd_l, dr, final):
    B = R["big"]
    ident = R["consts"][:, 0, :]
    FS = wg_l.shape[1]
    FCN = FS // 128
    hst, ss, rs, stf, stb, stt_ = R["pre_tiles"]
    A.begin("ffn")
    sg = [A.sb([128, 512], BF16) for _ in range(2)]
    A.end()
    nwB = B.t[8][:, 0:2048]
    S.dma("sp", lambda e: e.dma_start(out=nwB, in_=nw2_l.partition_broadcast(128)), reads=[("nwB",)], writes=[("B", 8, 0), ("nwB",)])
    wgv = dr["wg_bf"].rearrange("(kc p) n -> p kc n", p=128)
    wuv = dr["wu_bf"].rearrange("(kc p) n -> p kc n", p=128)
    wdv = dr["wd_bf"].rearrange("(fc p) n -> p fc n", p=128)
    NH = 2 if NB >= 8 else 1
    hT = [B.t[h][:, 0:4096].bitcast(BF16).rearrange("p (a b) -> p a b", a=16) for h in range(2)]
    aT = [B.t[2 + h][:, 0:FCN * 256].bitcast(BF16).rearrange("p (a b) -> p a b", a=FCN) for h in range(2)]
    wgt = [B.t[4][:, 1024 * i:1024 * (i + 1)].bitcast(BF16).rearrange("p (a b) -> p a b", a=16) for i in range(2)]
    wut = [B.t[4][:, 2048 + 1024 * i:2048 + 1024 * (i + 1)].bitcast(BF16).rearrange("p (a b) -> p a b", a=16) for i in range(2)]
    wdt = [B.t[5 + i][:, 0:FCN * 256].bitcast(BF16).rearrange("p (a b) -> p a b", a=FCN) for i in range(2)]
    xts = [B.t[7][:, 0:2048], B.t[8][:, 2048:4096]]
    xks = [("B", 7, 0), ("B", 8, 1)]
    hf = B.t[7][:, 2048:4096]
    hfk = ("B", 7, 1)
    ev = 0
    it = 0
    hfs = [B.t[5][:, 0:2048], B.t[6][:, 0:2048]]
    hfks = [("B", 5), ("B", 6)]

    def x1_a(h, t4, tt):
        b = tt % 2
        xt, xk = xts[b], xks[b]
        hf_, hfk_ = hfs[b], hfks[b]
        ssb, rsb = ss[b], rs[b]
        rows = slice(tt * 128, (tt + 1) * 128)
        S.dma("sp", lambda e: e.dma_start(out=xt, in_=xsrc[rows, :]), reads=([xkey] if xkey else []), writes=[xk])
        for hc in range(4):
            S.dma("sp", lambda e, hc=hc: e.dma_start(out=stf[hc % 2][:], in_=dr["sumb"][rows, hc * 512:(hc + 1) * 512]),
                  reads=[("sumb", tt // 4)], writes=[("stf", hc % 2)])
            S.dve(lambda e, hc=hc: e.tensor_tensor(out=xt[:, hc * 512:(hc + 1) * 512], in0=xt[:, hc * 512:(hc + 1) * 512], in1=stf[hc % 2][:], op=ALU.add),
                  reads=[xk, ("stf", hc % 2)], writes=[xk])
        S.dma("sp", lambda e: e.dma_start(out=xdst[rows, :], in_=xt), reads=[xk], writes=[("xb_d", tt)])
        S.act(lambda e: e.activation(out=hf_, in_=xt, func=AF.Square, accum_out=ssb[:]), reads=[xk], writes=[hfk_, ("ss", b)])
        S.dve(lambda e: e.tensor_scalar(out=rsb[:], in0=ssb[:], scalar1=1.0 / D_MODEL, scalar2=EPS, op0=ALU.mult, op1=ALU.add), reads=[("ss", b)], writes=[("rs", b)])
        S.act(lambda e: e.activation(out=rsb[:], in_=rsb[:], func=AF.Sqrt), reads=[("rs", b)], writes=[("rs", b)])
        S.dve(lambda e: e.reciprocal(out=rsb[:], in_=rsb[:]), reads=[("rs", b)], writes=[("rs", b)])
        S.dve(lambda e: e.scalar_tensor_tensor(out=hf_, in0=xt, scalar=rsb[:], in1=nwB, op0=ALU.mult, op1=ALU.mult),
              reads=[xk, ("rs", b), ("nwB",)], writes=[hfk_])

    def x1_b(h, t4, tt):
        b = tt % 2
        hf_, hfk_ = hfs[b], hfks[b]
        for g in range(4):
            pb = g % 2
            for j in range(4):
                kc = g * 4 + j
                S.pe(lambda e, pb=pb, j=j, kc=kc: e.transpose(out=PS[pb][:, j * 128:(j + 1) * 128], in_=hf_[:, kc * 128:(kc + 1) * 128], identity=ident),
                     reads=[hfk_, ("consts",)], writes=[("ps", pb)])
            s_ = PS[pb][:, 0:512].rearrange("p (a b) -> p a b", a=4)
            d_ = hT[h][:, g * 4:(g + 1) * 4, t4 * 128:(t4 + 1) * 128]
            if evc[0] % 2 == 0:
                S.act(lambda e, s_=s_, d_=d_: e.copy(out=d_, in_=s_), reads=[("ps", pb)], writes=[("B", h, t4, g)])
            else:
                S.dve(lambda e, s_=s_, d_=d_: e.tensor_copy(out=d_, in_=s_), reads=[("ps", pb)], writes=[("B", h, t4, g)])
            evc[0] += 1
    evc = [0]
    for ss_ in range(NB // (4 * NH)):
        tiles = [(h, t4, (ss_ * NH + h) * 4 + t4) for h in range(NH) for t4 in range(4)]
        for i in range(len(tiles) + 1):
            if i < len(tiles):
                x1_a(*tiles[i])
            if i >= 1:
                x1_b(*tiles[i - 1])
        for fc in range(FCN):
            b = fc % 2
            S.dma("sp", lambda e, fc=fc, b=b: e.dma_start(out=wgt[b], in_=wgv[:, :, fc * 128:(fc + 1) * 128]), reads=[("wg_bf",)], writes=[("B", 4, 0, b)])
            S.dma("sp", lambda e, fc=fc, b=b: e.dma_start(out=wut[b], in_=wuv[:, :, fc * 128:(fc + 1) * 128]), reads=[("wu_bf",)], writes=[("B", 4, 1, b)])
            for h in range(NH):
                pg, pu = 2 + (fc * NH + h) % 2, 4 + (fc * NH + h) % 2
                sgi = (fc * NH + h) % 2
                for kc in range(16):
                    S.pe(lambda e, pg=pg, kc=kc, b=b, h=h: e.matmul(out=PS[pg][:, 0:512], lhsT=wgt[b][:, kc, :], rhs=hT[h][:, kc, :], start=(kc == 0), stop=(kc == 15)),
                         reads=[("B", 4, 0, b), ("B", h)], writes=[("ps", pg)])
                for kc in range(16):
                    S.pe(lambda e, pu=pu, kc=kc, b=b, h=h: e.matmul(out=PS[pu][:, 0:512], lhsT=wut[b][:, kc, :], rhs=hT[h][:, kc, :], start=(kc == 0), stop=(kc == 15)),
                         reads=[("B", 4, 1, b), ("B", h)], writes=[("ps", pu)])
                S.act(lambda e, pg=pg, sgi=sgi: e.activation(out=sg[sgi][:], in_=PS[pg][:, 0:512], func=AF.Silu), reads=[("ps", pg)], writes=[("sg", sgi)])
                S.dve(lambda e, pu=pu, sgi=sgi, fc=fc, h=h: e.tensor_tensor(out=aT[h][:, fc, :], in0=sg[sgi][:], in1=PS[pu][:, 0:512], op=ALU.mult),
                      reads=[("sg", sgi), ("ps", pu)], writes=[("B", 2 + h, fc)])
        for ct in range(4):
            wb = ct % 2
            S.dma("sp", lambda e, ct=ct, wb=wb: e.dma_start(out=wdt[wb], in_=wdv[:, :, ct * 512:(ct + 1) * 512]), reads=[("wd_bf",)], writes=[("B", 5 + wb)])
            for h in range(NH):
                sl = ss_ * NH + h
                for t4 in range(4):
                    tt = sl * 4 + t4
                    rows = slice(tt * 128, (tt + 1) * 128)
                    pb = 6 + it % 2
                    st_ = stt_[it % 2]
                    for fc in range(FCN):
                        S.pe(lambda e, pb=pb, fc=fc, t4=t4, wb=wb, h=h: e.matmul(out=PS[pb][:, 0:512], lhsT=aT[h][:, fc, t4 * 128:(t4 + 1) * 128], rhs=wdt[wb][:, fc, :],
                                                                               start=(fc == 0), stop=(fc == FCN - 1)),
                             reads=[("B", 2 + h), ("B", 5 + wb)], writes=[("ps", pb)])
                    if final:
                        S.dma("sp", lambda e, st_=st_, rows=rows, ct=ct: e.dma_start(out=st_[:, 0:512], in_=xdst[rows, ct * 512:(ct + 1) * 512]),
                              reads=[("xb_d", tt)], writes=[("stt", it % 2)])
                        S.dve(lambda e, st_=st_, pb=pb: e.scalar_tensor_tensor(out=st_[:, 0:512], in0=st_[:, 0:512], scalar=R["parB"][:, 6:7], in1=PS[pb][:, 0:512],
                                                                             op0=ALU.mult, op1=ALU.add),
                              reads=[("stt", it % 2), ("parB",), ("ps", pb)], writes=[("stt", it % 2)])
                    elif it % 2 == 0:
                        S.act(lambda e, st_=st_, pb=pb: e.copy(out=st_[:, 0:512], in_=PS[pb][:, 0:512]), reads=[("ps", pb)], writes=[("stt", it % 2)])
                    else:
                        S.dve(lambda e, st_=st_, pb=pb: e.tensor_copy(out=st_[:, 0:512], in_=PS[pb][:, 0:512]), reads=[("ps", pb)], writes=[("stt", it % 2)])
                    if final:
                        TPB = NB // 4
                        pb_ = (tt % TPB) * 4 + tt // TPB
                        prows = slice(pb_ * 128, (pb_ + 1) * 128)
                    else:
                        pb_ = tt
                        prows = rows
                    S.dma("sp", lambda e, st_=st_, prows=prows, ct=ct: e.dma_start(out=dr["part"][prows, ct * 512:(ct + 1) * 512], in_=st_[:, 0:512]),
                          reads=[("stt", it % 2)], writes=[("part_d", pb_, ct)])
                    it += 1
        if not final:
            for h in range(NH):
                allreduce_chunk(S, dr, ss_ * NH + h)
    if final:
        for k in range(NB // 4):
            S.cc(lambda e, k=k: e.collective_compute("ReduceScatter", ALU.add, replica_groups=G4, ins=[dr["part"][k * 512:(k + 1) * 512, :]],
                                                     outs=[dr["rsout"][k * 128:(k + 1) * 128, :]]),
                 reads=[("part_d", 4 * k + j) for j in range(4)], writes=[("rsout", k)])


def build_fused(NB=32, DEPTH=2, DEBUG=False):
    nc = bass.Bass("TRN2", target_bir_lowering=False)
    SEQ = NB * 128
    D = D_MODEL
    FS = D_FF // 4
    ein = lambda name, shape: nc.dram_tensor(name, list(shape), F32, kind="ExternalInput").ap()
    x = ein("x", [SEQ, D])
    nw = ein("nw", [DEPTH, D])
    w = ein("w", [DEPTH, D, 2048])
    params = ein("params", [DEPTH, 8])
    normw_d = ein("normw", [DEPTH, 512])
    convw_d = ein("convw", [DEPTH, 128, 12])
    consts_d = ein("consts", [128, 5, 128])
    wo = ein("wo", [DEPTH, 512, D])
    nw2 = ein("nw2", [DEPTH, D])
    wg = ein("wg", [DEPTH, D, FS])
    wu = ein("wu", [DEPTH, D, FS])
    wd = ein("wd", [DEPTH, FS, D])
    fnw = ein("fnw", [D])
    out = nc.dram_tensor("out", [SEQ // 4, D], F32, kind="ExternalOutput").ap()
    it_ = lambda name, shape, dt=F32, **kw: nc.dram_tensor(name, list(shape), dt, kind="Internal", **kw).ap()
    dr = {}
    dr["hTd"] = it_("hTd", [16, 128, SEQ], BF16)
    dr["m_qT"] = it_("m_qT", [128, SEQ])
    dr["m_kT"] = it_("m_kT", [128, SEQ])
    dr["tokm"] = it_("tokm", [SEQ, 768])
    dr["f_qkT"] = it_("f_qkT", [4, 128, SEQ], BF16)
    dr["g_T"] = it_("g_T", [3, 128, SEQ])
    dr["ytok_d"] = it_("ytok_d", [SEQ, 512])
    dr["part"] = it_("part", [SEQ, D])
    dr["sumb"] = it_("sumb", [SEQ, D], addr_space="Local")
    dr["rsout"] = it_("rsout", [SEQ // 4, D], addr_space="Local")
    dr["xa"] = it_("xa", [SEQ, D])
    dr["xb"] = it_("xb", [SEQ, D])
    dr["wg_bf"] = it_("wg_bf", [D, FS], BF16)
    dr["wu_bf"] = it_("wu_bf", [D, FS], BF16)
    dr["wd_bf"] = it_("wd_bf", [FS, D], BF16)
    S = Sched(nc)
    with contextlib.ExitStack() as st:
        A = Alloc(nc, st)
        R = {}
        R["consts"] = A.sb([128, 5, 128], F32)
        R["parB"] = A.sb([128, 8], F32)
        R["gates"] = A.sb([128, NB, 6], F32)
        R["normw"] = A.sb([128, 4, 128], F32)
        R["convw"] = A.sb([128, 12], F32)
        R["big"] = Big(A, 9)
        R["pre_tiles"] = pre_tiles(A)
        PS = [A.ps([128, 512], F32) for _ in range(8)]
        B = R["big"]
        S.dma("sp", lambda e: e.dma_start(out=R["consts"][:], in_=consts_d), writes=[("consts",)])
        for l in range(DEPTH):
            final = (l == DEPTH - 1)
            S.dma("sp", lambda e, l=l: e.dma_start(out=R["parB"][:], in_=params[l].partition_broadcast(128)), writes=[("parB",)])
            S.dma("sp", lambda e, l=l: e.dma_start(out=R["normw"][:].rearrange("p a b -> p (a b)"), in_=normw_d[l].partition_broadcast(128)), writes=[("normw",)])
            S.dma("sp", lambda e, l=l: e.dma_start(out=R["convw"][:], in_=convw_d[l]), writes=[("convw",)])
            ffn_precast(S, wg[l], wu[l], wd[l], dr)
            if l == 0:
                pre_phase(S, A, NB, R, PS, x, nw[l], w[l], dr)
                xcur, xcur_key = x, None
            else:
                pre_phase(S, A, NB, R, PS, dr["xb"], nw[l], w[l], dr, add_src=dr["sumb"], store_dst=dr["xa"], xkey=("xb_d",))
                xcur, xcur_key = dr["xa"], ("xa_d",)
            A.begin("mix")
            T = mixer_prelude(S, A, NB, R)
            mlstm_phase(S, A, NB, R, T, PS, dr, dr["ytok_d"])
            fox_phase(S, A, NB, R, T, PS, dr, dr["ytok_d"])
            gdn_phase(S, A, NB, R, T, PS, dr, dr["ytok_d"])
            A.end()
            wo_phase(S, A, NB, R, PS, wo[l], dr)
            ffn_phase(S, A, NB, R, PS, xcur, xcur_key, dr["xb"], nw2[l], wg[l], wu[l], wd[l], dr, final)
        hst, ss, rs, stf, stb, stt_ = R["pre_tiles"]
        fnB = B.t[8][:, 0:2048]
        S.dma("sp", lambda e: e.dma_start(out=fnB, in_=fnw.partition_broadcast(128)), reads=[("nwB",)], writes=[("B", 8), ("nwB",)])
        for tt in range(NB // 4):
            b = tt % 2
            xt = B.t[b][:, 0:2048]
            hf = B.t[2 + b][:, 0:2048]
            rows = slice(tt * 128, (tt + 1) * 128)
            S.dma("sp", lambda e, xt=xt, rows=rows: e.dma_start(out=xt, in_=dr["rsout"][rows, :]), reads=[("rsout", tt)], writes=[("B", b)])
            S.act(lambda e, xt=xt, hf=hf, b=b: e.activation(out=hf, in_=xt, func=AF.Square, accum_out=ss[b][:]), reads=[("B", b)], writes=[("B", 2 + b), ("ss", b)])
            S.dve(lambda e, b=b: e.tensor_scalar(out=rs[b][:], in0=ss[b][:], scalar1=1.0 / D, scalar2=EPS, op0=ALU.mult, op1=ALU.add), reads=[("ss", b)], writes=[("rs", b)])
            S.act(lambda e, b=b: e.activation(out=rs[b][:], in_=rs[b][:], func=AF.Sqrt), reads=[("rs", b)], writes=[("rs", b)])
            S.dve(lambda e, b=b: e.reciprocal(out=rs[b][:], in_=rs[b][:]), reads=[("rs", b)], writes=[("rs", b)])
            S.dve(lambda e, xt=xt, hf=hf, b=b: e.scalar_tensor_tensor(out=hf, in0=xt, scalar=rs[b][:], in1=fnB, op0=ALU.mult, op1=ALU.mult),
                  reads=[("B", b), ("rs", b), ("nwB",)], writes=[("B", 2 + b)])
            S.dma("sp", lambda e, hf=hf, rows=rows: e.dma_start(out=out[rows, :], in_=hf), reads=[("B", 2 + b)])
        if DEBUG:
            for name, src, key in (("dbg_sumb", dr["sumb"], ("sumb",)), ("dbg_xb", dr["xb"], ("xb_d",)), ("dbg_part", dr["part"], ("part_d",)),
                                   ("dbg_ytok", dr["ytok_d"], ("ytok_d",))):
                dst = nc.dram_tensor(name, list(src.shape), F32, kind="ExternalOutput").ap()
                S.dma("sp", lambda e, dst=dst, src=src: e.dma_start(out=dst, in_=src), reads=[key])
        S.emit()
    return nc


def _worows(g):
    return list(range(g * 128, (g + 1) * 128)) + list(range(512 + g * 256, 512 + (g + 1) * 256)) + list(range(1536 + g * 128, 1536 + (g + 1) * 128))


def make_in_maps(x, mix_norm_w, w_in, mlstm_i_bias, mlstm_f_bias, fox_f_bias, gdn_conv_w, gdn_a_log, gdn_dt_bias,
                 mlstm_out_norm_w, fox_out_norm_w, gdn_out_norm_w, w_out, ffn_norm_w, w_gate, w_up, w_down, final_norm_w):
    f = lambda a: np.ascontiguousarray(np.asarray(a, dtype=np.float32))
    x = f(x)
    Bsz, SEQ, D = x.shape
    depth = np.asarray(w_in).shape[0]
    FS = D_FF // 4
    consts = consts_np()
    w_in, w_out, w_gate, w_up, w_down, cwall = f(w_in), f(w_out), f(w_gate), f(w_up), f(w_down), f(gdn_conv_w)
    in_maps = []
    for c in range(8):
        b, g = c // 4, c % 4
        sl = lambda a, l, h: f(a)[l][128 * h:128 * (h + 1)]
        m = {"x": np.ascontiguousarray(x[b]), "nw": f(mix_norm_w), "consts": consts, "nw2": f(ffn_norm_w), "fnw": f(final_norm_w)}
        m["w"] = np.stack([_wslice(w_in[l], g) for l in range(depth)])
        m["params"] = np.stack([np.array([f(mlstm_i_bias)[l][g], f(mlstm_f_bias)[l][g], f(fox_f_bias)[l][2 * g], f(fox_f_bias)[l][2 * g + 1],
                                          f(gdn_a_log)[l][g], f(gdn_dt_bias)[l][g], 1.0 if g == 0 else 0.0, 0.0], np.float32) for l in range(depth)])
        m["normw"] = np.stack([np.concatenate([sl(mlstm_out_norm_w, l, g), sl(fox_out_norm_w, l, 2 * g), sl(fox_out_norm_w, l, 2 * g + 1),
                                               sl(gdn_out_norm_w, l, g)]) for l in range(depth)])
        m["convw"] = np.stack([np.ascontiguousarray(np.stack([cwall[l][:, t * 512 + g * 128:t * 512 + (g + 1) * 128].T for t in range(3)], 1).reshape(128, 12))
                               for l in range(depth)])
        m["wo"] = np.ascontiguousarray(w_out[:, _worows(g), :])
        m["wg"] = np.ascontiguousarray(w_gate[:, :, g * FS:(g + 1) * FS])
        m["wu"] = np.ascontiguousarray(w_up[:, :, g * FS:(g + 1) * FS])
        m["wd"] = np.ascontiguousarray(w_down[:, g * FS:(g + 1) * FS, :])
        in_maps.append(m)
    return in_maps, (Bsz, SEQ, D)


_DEBUG = [False, None]


def kernel(**inputs):
    in_maps, (Bsz, SEQ, D) = make_in_maps(**inputs)
    depth = in_maps[0]["w"].shape[0]
    key = ("F", SEQ, depth, _DEBUG[0])
    if key not in _CACHE:
        _CACHE[key] = build_fused(SEQ // 128, depth, DEBUG=_DEBUG[0])
    res = run_bass_kernel_spmd(_CACHE[key], in_maps, core_ids=list(range(8)))
    _DEBUG[1] = res
    out = np.zeros((Bsz, SEQ, D), np.float32)
    q = SEQ // 4
    for c in range(8):
        b, g = c // 4, c % 4
        out[b, g * q:(g + 1) * q] = res.results[c]["out"]
    return out
```

```python
from collections import defaultdict
import numpy as np
import ml_dtypes
import concourse.bass as bass
from concourse.bass_utils import run_bass_kernel_spmd
import concourse.mybir as mybir

F32 = mybir.dt.float32
BF16 = mybir.dt.bfloat16
AF = mybir.ActivationFunctionType
ALU = mybir.AluOpType
AX = mybir.AxisListType

ENGS = ("pe", "act", "dve", "pool", "sp")
NDMASEM = 32
NPOOLSEM = 8
NCCSEM = 48


class Sched:
    def __init__(self, nc, same_engine_sync=True):
        self.nc = nc
        self.same = same_engine_sync
        self.ops = {e: [] for e in ENGS}
        self.seen = {e: {} for e in ENGS}
        self.w = {}
        self.r = {}
        self.kids = defaultdict(set)
        self.dma_uses = [0] * NDMASEM
        self.dma_last = [None] * NDMASEM
        self.dma_rr = 0
        self.dma_rr_pool = 0
        self.ncc = 0

    def _related(self, key):
        ks = []
        for i in range(1, len(key) + 1):
            p = key[:i]
            if p in self.w or p in self.r:
                ks.append(p)
        for c in self.kids.get(key, ()):
            if c != key and (c in self.w or c in self.r):
                ks.append(c)
        return ks

    def _reg(self, key):
        for i in range(1, len(key)):
            self.kids[key[:i]].add(key)

    def op(self, eng, fn, reads=(), writes=(), dma=False):
        reads = [k if isinstance(k, tuple) else (k,) for k in reads]
        writes = [k if isinstance(k, tuple) else (k,) for k in writes]
        deps = set()
        for k in reads:
            for rk in self._related(k):
                if rk in self.w:
                    deps.add(self.w[rk])
        for k in writes:
            for rk in self._related(k):
                if rk in self.w:
                    deps.add(self.w[rk])
                for t in self.r.get(rk, ()):
                    deps.add(t)
        idx = len(self.ops[eng])
        if dma == "cc":
            tok = (("cc", self.ncc), 1)
            dmainfo = self.ncc
            self.ncc += 1
            assert self.ncc <= NCCSEM
        elif dma:
            if eng == "pool":
                j = self.dma_rr_pool
                self.dma_rr_pool = (j + 1) % NPOOLSEM
            else:
                j = NPOOLSEM + self.dma_rr
                self.dma_rr = (self.dma_rr + 1) % (NDMASEM - NPOOLSEM)
            if self.dma_last[j] is not None:
                deps.add(self.dma_last[j])
            self.dma_uses[j] += 1
            tok = (("dma", j), self.dma_uses[j])
            self.dma_last[j] = tok
            dmainfo = j
        else:
            tok = (eng, idx)
            dmainfo = None
        waits = {}
        for (sk, v) in deps:
            if sk == eng:
                if eng == "pe" or not self.same:
                    continue
            if self.seen[eng].get(sk, -1) >= v:
                continue
            if waits.get(sk, -1) < v:
                waits[sk] = v
        for sk, v in waits.items():
            self.seen[eng][sk] = v
            if not isinstance(sk, tuple):
                self.ops[sk][v][3] = True
        self.ops[eng].append([list(waits.items()), fn, dma, False, dmainfo])
        for k in writes:
            for rk in self._related(k):
                if len(rk) > len(k):
                    self.w.pop(rk, None)
                    self.r.pop(rk, None)
            self.w[k] = tok
            self.r[k] = []
            self._reg(k)
        for k in reads:
            self.r.setdefault(k, []).append(tok)
            self._reg(k)
        return tok

    def pe(self, fn, reads=(), writes=()):
        return self.op("pe", fn, reads, writes)

    def act(self, fn, reads=(), writes=()):
        return self.op("act", fn, reads, writes)

    def dve(self, fn, reads=(), writes=()):
        return self.op("dve", fn, reads, writes)

    def pool(self, fn, reads=(), writes=()):
        return self.op("pool", fn, reads, writes)

    def dma(self, q, fn, reads=(), writes=()):
        return self.op(q, fn, reads, writes, dma=True)

    def cc(self, fn, reads=(), writes=()):
        return self.op("pool", fn, reads, writes, dma="cc")

    def emit(self, final_wait_tokens=()):
        nc = self.nc
        import contextlib
        with contextlib.ExitStack() as st:
            esem = {e: st.enter_context(nc.semaphore("s_" + e)) for e in ENGS}
            dsem = [st.enter_context(nc.semaphore("d%d" % j)) for j in range(NDMASEM)]
            ccsem = [st.enter_context(nc.semaphore("cc%d" % j)) for j in range(self.ncc)]
            block = st.enter_context(nc.Block())
            rank = {}
            for e in ENGS:
                c = 0
                rk = []
                for o in self.ops[e]:
                    if o[3] and not o[2]:
                        c += 1
                    rk.append(c)
                rank[e] = rk

            def semval(sk, v):
                if isinstance(sk, tuple):
                    if sk[0] == "cc":
                        return ccsem[sk[1]], v
                    return dsem[sk[1]], 16 * v
                return esem[sk], rank[sk][v]

            def run(e, engobj):
                for (waits, fn, isdma, marked, dmainfo) in self.ops[e]:
                    for sk, v in waits:
                        s, val = semval(sk, v)
                        engobj.wait_ge(s, val)
                    ins = fn(engobj)
                    if isdma == "cc":
                        ins.then_inc(ccsem[dmainfo])
                    elif isdma:
                        ins.then_inc(dsem[dmainfo], 16)
                    elif marked:
                        ins.then_inc(esem[e], 1)
                if e == "sp":
                    for j in range(NDMASEM):
                        if self.dma_uses[j] > 0:
                            engobj.wait_ge(dsem[j], 16 * self.dma_uses[j])

            @block.tensor
            def _(eng):
                run("pe", eng)

            @block.scalar
            def _(eng):
                run("act", eng)

            @block.vector
            def _(eng):
                run("dve", eng)

            @block.gpsimd
            def _(eng):
                run("pool", eng)

            @block.sync
            def _(eng):
                run("sp", eng)


class Big:
    def __init__(self, A, n, width=4264):
        self.t = [A.sb([128, width], F32) for _ in range(n)]

    def v(self, i, a, b=None):
        if b is None:
            return self.t[i][:, 0:a]
        return self.t[i][:, 0:a * b].rearrange("p (a b) -> p a b", a=a)


class Alloc:
    def __init__(self, nc, st):
        self.nc = nc
        self.st = st
        self.n = 0
        self.cache = {}
        self.tag = None
        self.cnt = 0

    def begin(self, tag):
        self.tag = tag
        self.cnt = 0

    def end(self):
        self.tag = None

    def sb(self, shape, dt):
        if self.tag is not None:
            key = (self.tag, self.cnt)
            self.cnt += 1
            if key in self.cache:
                return self.cache[key]
        self.n += 1
        t = self.st.enter_context(self.nc.sbuf_tensor("sb%d" % self.n, list(shape), dt))
        if self.tag is not None:
            self.cache[key] = t
        return t

    def ps(self, shape, dt):
        self.n += 1
        return self.st.enter_context(self.nc.psum_tensor("ps%d" % self.n, list(shape), dt))


D_MODEL = 2048
N_IN = 7192
D_FF = 5632
EPS = 1e-6
import contextlib


def rms_stage(S, A, xt_ap, key_x, ss, rs, junk, nwB, hb, key_hb, D=D_MODEL):
    S.act(lambda e: e.activation(out=junk[:], in_=xt_ap, func=AF.Square, accum_out=ss[:]),
          reads=[key_x], writes=[("junk",), ("ss",)])
    S.dve(lambda e: e.tensor_scalar(out=rs[:], in0=ss[:], scalar1=1.0 / D, scalar2=EPS, op0=ALU.mult, op1=ALU.add),
          reads=[("ss",)], writes=[("rs",)])
    S.act(lambda e: e.activation(out=rs[:], in_=rs[:], func=AF.Sqrt), reads=[("rs",)], writes=[("rs",)])
    S.dve(lambda e: e.reciprocal(out=rs[:], in_=rs[:]), reads=[("rs",)], writes=[("rs",)])
    S.dve(lambda e: e.scalar_tensor_tensor(out=hb, in0=xt_ap, scalar=rs[:], in1=nwB[:], op0=ALU.mult, op1=ALU.mult),
          reads=[key_x, ("rs",), ("nwB",)], writes=[key_hb])


def build_k1(TOK=1024, D=D_MODEL, N=N_IN, NCT=None, TEV=2, NTL=None):
    nc = bass.Bass("TRN2", target_bir_lowering=False)
    x = nc.dram_tensor("x", [TOK, D], F32, kind="ExternalInput").ap()
    nw = nc.dram_tensor("nw", [D], F32, kind="ExternalInput").ap()
    w = nc.dram_tensor("w", [D, N], F32, kind="ExternalInput").ap()
    identd = nc.dram_tensor("ident", [128, 128], F32, kind="ExternalInput").ap()
    out = nc.dram_tensor("proj", [TOK, N], F32, kind="ExternalOutput").ap()
    NT = TOK // 128 if NTL is None else NTL
    KC = D // 128
    S = Sched(nc)
    with contextlib.ExitStack() as st:
        A = Alloc(nc, st)
        nwB = A.sb([128, D], F32)
        identf = A.sb([128, 128], F32)
        identb = A.sb([128, 128], BF16)
        xt = [A.sb([128, D], F32) for _ in range(2)]
        hb = [A.sb([128, D], BF16) for _ in range(2)]
        junk = A.sb([128, D], BF16)
        ss = A.sb([128, 1], F32)
        rs = A.sb([128, 1], F32)
        hT = A.sb([128, NT, KC, 128], BF16)
        wt = [A.sb([128, KC, 512], BF16) for _ in range(2)]
        ot = [A.sb([128, 512], F32) for _ in range(3)]
        tp = [A.ps([128, 1024], BF16) for _ in range(2)]
        po = [A.ps([128, 512], F32) for _ in range(4)]

        S.dma("sp", lambda e: e.dma_start(out=nwB[:], in_=nw.partition_broadcast(128)), writes=[("nwB",)])
        S.dma("sp", lambda e: e.dma_start(out=identf[:], in_=identd), writes=[("identf",)])
        S.dve(lambda e: e.tensor_copy(out=identb[:], in_=identf[:]), reads=[("identf",)], writes=[("identb",)])
        ev = 0
        for tt in range(NT):
            b = tt % 2
            S.dma("sp", lambda e, tt=tt, b=b: e.dma_start(out=xt[b][:], in_=x[tt * 128:(tt + 1) * 128, :]),
                  writes=[("xt", b)])
            rms_stage(S, A, xt[b][:], ("xt", b), ss, rs, junk, nwB, hb[b][:], ("hb", b))
            for g in range(KC // 4):
                pb = g % 2
                for j in range(4):
                    kc = g * 4 + j
                    S.pe(lambda e, pb=pb, j=j, kc=kc, b=b: e.transpose(
                        out=tp[pb][:, j * 128:(j + 1) * 128], in_=hb[b][:, kc * 128:(kc + 1) * 128], identity=identb[:]),
                        reads=[("hb", b), ("identb",)], writes=[("tp", pb)])
                dst = hT[:, tt, g * 4:(g + 1) * 4, :]
                if ev % TEV == 0:
                    S.act(lambda e, pb=pb, dst=dst: e.copy(out=dst, in_=tp[pb][:, 0:512].rearrange("p (a b) -> p a b", a=4)),
                          reads=[("tp", pb)], writes=[("hT", tt, g)])
                else:
                    S.dve(lambda e, pb=pb, dst=dst: e.tensor_copy(out=dst, in_=tp[pb][:, 0:512].rearrange("p (a b) -> p a b", a=4)),
                          reads=[("tp", pb)], writes=[("hT", tt, g)])
                ev += 1
        wv = w.rearrange("(kc p) n -> p kc n", p=128)
        nct = (N + 511) // 512 if NCT is None else NCT
        it = 0
        for ct in range(nct):
            c0 = ct * 512
            cw = min(512, N - c0)
            wb = ct % 2
            S.dma("pool", lambda e, wb=wb, c0=c0, cw=cw: e.dma_start(out=wt[wb][:, :, 0:cw], in_=wv[:, :, c0:c0 + cw]),
                  writes=[("wt", wb)])
            for tt in range(NT):
                pb = it % 4
                ob = it % 3
                it += 1
                for kc in range(KC):
                    S.pe(lambda e, pb=pb, tt=tt, kc=kc, wb=wb, cw=cw: e.matmul(
                        out=po[pb][:, 0:cw], lhsT=hT[:, tt, kc, :], rhs=wt[wb][:, kc, 0:cw],
                        start=(kc == 0), stop=(kc == KC - 1)),
                        reads=[("hT", tt), ("wt", wb)], writes=[("po", pb)])
                if it % 2 == 0:
                    S.act(lambda e, pb=pb, ob=ob, cw=cw: e.copy(out=ot[ob][:, 0:cw], in_=po[pb][:, 0:cw]),
                          reads=[("po", pb)], writes=[("ot", ob)])
                else:
                    S.dve(lambda e, pb=pb, ob=ob, cw=cw: e.tensor_copy(out=ot[ob][:, 0:cw], in_=po[pb][:, 0:cw]),
                          reads=[("po", pb)], writes=[("ot", ob)])
                S.dma("sp", lambda e, ob=ob, tt=tt, c0=c0, cw=cw: e.dma_start(
                    out=out[tt * 128:(tt + 1) * 128, c0:c0 + cw], in_=ot[ob][:, 0:cw]),
                    reads=[("ot", ob)])
        S.emit()
    return nc


HD = 128
G4 = [[0, 1, 2, 3], [4, 5, 6, 7]]
ISQ = HD ** -0.5


def bc(ap, shape):
    return ap.broadcast_to(list(shape))


def mixer_prelude(S, A, NB, R):
    parB, gates, C = R["parB"], R["gates"], R["consts"]
    T = {}
    for n in ["ti", "tf", "lf", "w", "e", "a", "tmp", "ff_lf", "ff_c", "ff_cum", "ff_tot", "gg", "gbeta", "ggc", "ggl", "ones32"]:
        T[n] = A.sb([128, NB], F32)
    par15 = A.sb([128, 8], F32)
    T["par15"] = par15
    S.dve(lambda e: e.tensor_scalar(out=par15[:], in0=parB[:], scalar1=1.0 / 15.0, scalar2=None, op0=ALU.mult),
          reads=[("parB",)], writes=[("par15",)])
    S.dve(lambda e: e.memset(T["ones32"][:], 1.0), writes=[("ones32",)])
    return T


def logsig_inplace(S, t, key):
    S.act(lambda e: e.activation(out=t, in_=t, func=AF.Exp, scale=-1.0), reads=[key], writes=[key])
    S.act(lambda e: e.activation(out=t, in_=t, func=AF.Ln, bias=1.0), reads=[key], writes=[key])
    S.dve(lambda e: e.tensor_scalar(out=t, in0=t, scalar1=-1.0, scalar2=None, op0=ALU.mult), reads=[key], writes=[key])


def mlstm_phase(S, A, NB, R, T, PS, dr, ytok):
    C = R["consts"]
    parB, par15, gates = R["parB"], T["par15"], R["gates"]
    SEQ = NB * 128
    B = R["big"]
    qT = B.v(0, SEQ)
    kT = B.v(1, SEQ)
    ktok = B.v(2, NB, 128)
    v1 = B.v(3, NB, 129)
    mo = B.v(4, NB, 128)
    kw = B.v(5, NB, 128)
    Sst = B.v(6, NB + 1, 129)
    hraw = B.v(7, NB, 129)
    Tt = A.sb([128, 129], F32)
    PT = [A.sb([128, 128], F32) for _ in range(2)]
    tokv = dr["tokm"].rearrange("(b p) c -> p b c", p=128)
    S.dma("sp", lambda e: e.dma_start(out=qT, in_=dr["m_qT"]), reads=[("fm_d",)], writes=[("B", 0)])
    S.dma("sp", lambda e: e.dma_start(out=kT, in_=dr["m_kT"]), reads=[("fm_d",)], writes=[("B", 1)])
    S.dma("sp", lambda e: e.dma_start(out=ktok, in_=tokv[:, :, 0:128]), reads=[("tokm_d",)], writes=[("B", 2)])
    S.dma("sp", lambda e: e.dma_start(out=v1[:, :, 0:128], in_=tokv[:, :, 128:256]), reads=[("tokm_d",)], writes=[("B", 3, 0)])
    S.dma("sp", lambda e: e.dma_start(out=mo, in_=tokv[:, :, 256:384]), reads=[("tokm_d",)], writes=[("B", 4)])
    S.pool(lambda e: e.memset(v1[:, :, 128:129], 1.0), writes=[("B", 3, 1)])
    S.pool(lambda e: e.memset(Sst[:, 0, :], 0.0), writes=[("B", 6, 0)])
    ti, tf, lf, w, ee, aa, tmp = T["ti"], T["tf"], T["lf"], T["w"], T["e"], T["a"], T["tmp"]
    S.act(lambda e: e.activation(out=ti[:], in_=gates[:, :, 0], func=AF.Tanh, bias=par15[:, 0:1], scale=1.0 / 15.0),
          reads=[("gates",), ("par15",)], writes=[("ti",)])
    S.dve(lambda e: e.tensor_scalar(out=ti[:], in0=ti[:], scalar1=15.0, scalar2=None, op0=ALU.mult), reads=[("ti",)], writes=[("ti",)])
    S.act(lambda e: e.activation(out=lf[:], in_=gates[:, :, 1], func=AF.Tanh, bias=par15[:, 1:2], scale=1.0 / 15.0),
          reads=[("gates",), ("par15",)], writes=[("lf",)])
    S.dve(lambda e: e.tensor_scalar(out=lf[:], in0=lf[:], scalar1=15.0, scalar2=None, op0=ALU.mult), reads=[("lf",)], writes=[("lf",)])
    logsig_inplace(S, lf[:], ("lf",))
    pb, pbl = PS[0], PS[1]
    S.pe(lambda e: e.matmul(out=pb[:, 0:NB], lhsT=C[:, 1, :], rhs=lf[:], start=True, stop=True),
         reads=[("consts",), ("lf",)], writes=[("ps", 0)])
    S.pe(lambda e: e.matmul(out=pbl[:, 0:NB], lhsT=C[:, 2, :], rhs=lf[:], start=True, stop=True),
         reads=[("consts",), ("lf",)], writes=[("ps", 1)])
    S.dve(lambda e: e.tensor_tensor(out=tmp[:], in0=ti[:], in1=pb[:, 0:NB], op=ALU.subtract), reads=[("ti",), ("ps", 0)], writes=[("tmp",)])
    S.act(lambda e: e.activation(out=w[:], in_=tmp[:], func=AF.Exp), reads=[("tmp",)], writes=[("w",)])
    S.dve(lambda e: e.tensor_scalar(out=w[:], in0=w[:], scalar1=ISQ, scalar2=None, op0=ALU.mult), reads=[("w",)], writes=[("w",)])
    S.act(lambda e: e.activation(out=ee[:], in_=pb[:, 0:NB], func=AF.Exp), reads=[("ps", 0)], writes=[("e",)])
    S.act(lambda e: e.activation(out=aa[:], in_=pbl[:, 0:NB], func=AF.Exp), reads=[("ps", 1)], writes=[("a",)])
    S.dve(lambda e: e.tensor_tensor(out=kw, in0=ktok, in1=bc(w[:].unsqueeze(2), [128, NB, 128]), op=ALU.mult),
          reads=[("B", 2), ("w",)], writes=[("B", 5)])
    for c in range(NB):
        pu = PS[2 + c % 2]
        S.pe(lambda e, c=c, pu=pu: e.matmul(out=pu[:, 0:129], lhsT=kw[:, c, :], rhs=v1[:, c, :], start=True, stop=True),
             reads=[("B", 5), ("B", 3)], writes=[("ps", 2 + c % 2)])
        S.dve(lambda e, c=c, pu=pu: e.tensor_tensor(out=Tt[:], in0=Sst[:, c, :], in1=pu[:, 0:129], op=ALU.add),
              reads=[("B", 6, c), ("ps", 2 + c % 2)], writes=[("m_Tt",)])
        S.dve(lambda e, c=c: e.tensor_scalar(out=Sst[:, c + 1, :], in0=Tt[:], scalar1=aa[:, c:c + 1], scalar2=None, op0=ALU.mult),
              reads=[("m_Tt",), ("a",)], writes=[("B", 6, c + 1)])
    def emit_scores(c):
        sl = slice(c * 128, (c + 1) * 128)
        ps_s = PS[4 + c % 2]
        S.pe(lambda e: e.matmul(out=ps_s[:, 0:128], lhsT=kT[:, sl], rhs=qT[:, sl], start=True, stop=True),
             reads=[("B", 1), ("B", 0)], writes=[("ps", 4 + c % 2)])
    emit_scores(0)
    for c in range(NB):
        ps_s = PS[4 + c % 2]
        ps_o = PS[6 + c % 2]
        pt = PT[c % 2]
        sl = slice(c * 128, (c + 1) * 128)
        S.dve(lambda e, c=c, ps_s=ps_s, pt=pt: e.scalar_tensor_tensor(out=pt[:], in0=ps_s[:, 0:128], scalar=w[:, c:c + 1], in1=C[:, 1, :],
                                                                      op0=ALU.mult, op1=ALU.mult),
              reads=[("ps", 4 + c % 2), ("w",), ("consts",)], writes=[("m_PT", c % 2)])
        if c + 1 < NB:
            emit_scores(c + 1)
        S.pe(lambda e, c=c, ps_o=ps_o, pt=pt: e.matmul(out=ps_o[:, 0:129], lhsT=pt[:], rhs=v1[:, c, :], start=True, stop=False),
             reads=[("m_PT", c % 2), ("B", 3)], writes=[("ps", 6 + c % 2)])
        S.pe(lambda e, c=c, sl=sl, ps_o=ps_o: e.matmul(out=ps_o[:, 0:129], lhsT=qT[:, sl], rhs=Sst[:, c, :], start=False, stop=True),
             reads=[("B", 0), ("B", 6, c)], writes=[("ps", 6 + c % 2)])
        S.act(lambda e, c=c, ps_o=ps_o: e.copy(out=hraw[:, c, :], in_=ps_o[:, 0:129]), reads=[("ps", 6 + c % 2)], writes=[("B", 7, c)])
    den = T["tmp"]
    S.dve(lambda e: e.tensor_tensor(out=den[:], in0=hraw[:, :, 128], in1=ee[:], op=ALU.mult), reads=[("B", 7), ("e",)], writes=[("tmp",)])
    nden = T["gg"]
    S.dve(lambda e: e.tensor_scalar(out=nden[:], in0=den[:], scalar1=-1.0, scalar2=None, op0=ALU.mult), reads=[("tmp",)], writes=[("gg",)])
    S.dve(lambda e: e.tensor_tensor(out=den[:], in0=den[:], in1=nden[:], op=ALU.max), reads=[("tmp",), ("gg",)], writes=[("tmp",)])
    S.dve(lambda e: e.tensor_scalar(out=den[:], in0=den[:], scalar1=1.0, scalar2=None, op0=ALU.max), reads=[("tmp",)], writes=[("tmp",)])
    S.dve(lambda e: e.reciprocal(out=den[:], in_=den[:]), reads=[("tmp",)], writes=[("tmp",)])
    S.dve(lambda e: e.tensor_tensor(out=den[:], in0=den[:], in1=ee[:], op=ALU.mult), reads=[("tmp",), ("e",)], writes=[("tmp",)])
    hn = kw
    S.dve(lambda e: e.tensor_tensor(out=hn, in0=hraw[:, :, 0:128], in1=bc(den[:].unsqueeze(2), [128, NB, 128]), op=ALU.mult),
          reads=[("B", 7), ("tmp",)], writes=[("B", 5)])
    mu, var = T["ti"], T["tf"]
    S.dve(lambda e: e.tensor_reduce(out=mu[:], in_=hn, axis=AX.X, op=ALU.add), reads=[("B", 5)], writes=[("ti",)])
    S.dve(lambda e: e.tensor_scalar(out=mu[:], in0=mu[:], scalar1=1.0 / 128, scalar2=None, op0=ALU.mult), reads=[("ti",)], writes=[("ti",)])
    S.dve(lambda e: e.tensor_tensor(out=hn, in0=hn, in1=bc(mu[:].unsqueeze(2), [128, NB, 128]), op=ALU.subtract),
          reads=[("B", 5), ("ti",)], writes=[("B", 5)])
    sq = ktok
    S.act(lambda e: e.activation(out=sq, in_=hn, func=AF.Square), reads=[("B", 5)], writes=[("B", 2)])
    S.dve(lambda e: e.tensor_reduce(out=var[:], in_=sq, axis=AX.X, op=ALU.add), reads=[("B", 2)], writes=[("tf",)])
    S.dve(lambda e: e.tensor_scalar(out=var[:], in0=var[:], scalar1=1.0 / 128, scalar2=EPS, op0=ALU.mult, op1=ALU.add), reads=[("tf",)], writes=[("tf",)])
    S.act(lambda e: e.activation(out=var[:], in_=var[:], func=AF.Sqrt), reads=[("tf",)], writes=[("tf",)])
    S.dve(lambda e: e.reciprocal(out=var[:], in_=var[:]), reads=[("tf",)], writes=[("tf",)])
    S.dve(lambda e: e.tensor_tensor(out=hn, in0=hn, in1=bc(var[:].unsqueeze(2), [128, NB, 128]), op=ALU.mult),
          reads=[("B", 5), ("tf",)], writes=[("B", 5)])
    S.dve(lambda e: e.tensor_tensor(out=hn, in0=hn, in1=bc(R["normw"][:, 0, :].unsqueeze(1), [128, NB, 128]), op=ALU.mult),
          reads=[("B", 5), ("normw",)], writes=[("B", 5)])
    S.act(lambda e: e.activation(out=mo, in_=mo, func=AF.Sigmoid), reads=[("B", 4)], writes=[("B", 4)])
    S.dve(lambda e: e.tensor_tensor(out=hn, in0=hn, in1=mo, op=ALU.mult),
          reads=[("B", 5), ("B", 4)], writes=[("B", 5)])
    S.dma("sp", lambda e: e.dma_start(out=ytok.rearrange("(b p) c -> p b c", p=128)[:, :, 0:128], in_=hn),
          reads=[("B", 5)], writes=[("ytok_d", 0)])


def consts_np():
    i = np.arange(128)
    c = np.zeros((128, 5, 128), np.float32)
    c[:, 0, :] = np.eye(128)
    c[:, 1, :] = (i[:, None] <= i[None, :])
    c[:, 2, :] = 1.0
    c[:, 3, :] = (i[None, :] < i[:, None])
    c[:, 4, :] = (i[None, :] <= i[:, None])
    return c


def build_mix_test(NB, which):
    nc = bass.Bass("TRN2", target_bir_lowering=False)
    SEQ = NB * 128
    dr = {}
    dr["m_qT"] = nc.dram_tensor("m_qT", [128, SEQ], F32, kind="ExternalInput").ap()
    dr["m_kT"] = nc.dram_tensor("m_kT", [128, SEQ], F32, kind="ExternalInput").ap()
    dr["tokm"] = nc.dram_tensor("tokm", [SEQ, 768], F32, kind="ExternalInput").ap()
    dr["f_qkT"] = nc.dram_tensor("f_qkT", [4, 128, SEQ], BF16, kind="ExternalInput").ap()
    dr["g_T"] = nc.dram_tensor("g_T", [3, 128, SEQ], F32, kind="ExternalInput").ap()
    gates_d = nc.dram_tensor("gates_d", [SEQ, 6], F32, kind="ExternalInput").ap()
    params = nc.dram_tensor("params", [8], F32, kind="ExternalInput").ap()
    normw_d = nc.dram_tensor("normw", [4 * 128], F32, kind="ExternalInput").ap()
    convw_d = nc.dram_tensor("convw", [128, 12], F32, kind="ExternalInput").ap()
    consts_d = nc.dram_tensor("consts", [128, 5, 128], F32, kind="ExternalInput").ap()
    ytok = nc.dram_tensor("ytok", [SEQ, 512], F32, kind="ExternalOutput").ap()
    S = Sched(nc)
    with contextlib.ExitStack() as st:
        A = Alloc(nc, st)
        R = {}
        R["consts"] = A.sb([128, 5, 128], F32)
        R["parB"] = A.sb([128, 8], F32)
        R["gates"] = A.sb([128, NB, 6], F32)
        R["normw"] = A.sb([128, 4, 128], F32)
        R["convw"] = A.sb([128, 12], F32)
        R["big"] = Big(A, 9)
        PS = [A.ps([128, 512], F32) for _ in range(8)]
        S.dma("sp", lambda e: e.dma_start(out=R["consts"][:], in_=consts_d), writes=[("consts",)])
        S.dma("sp", lambda e: e.dma_start(out=R["parB"][:], in_=params.partition_broadcast(128)), writes=[("parB",)])
        S.dma("sp", lambda e: e.dma_start(out=R["gates"][:], in_=gates_d.rearrange("(b p) c -> p b c", p=128)), writes=[("gates",)])
        S.dma("sp", lambda e: e.dma_start(out=R["normw"][:].rearrange("p a b -> p (a b)"), in_=normw_d.partition_broadcast(128)), writes=[("normw",)])
        S.dma("sp", lambda e: e.dma_start(out=R["convw"][:], in_=convw_d), writes=[("convw",)])
        T = mixer_prelude(S, A, NB, R)
        if "m" in which:
            mlstm_phase(S, A, NB, R, T, PS, dr, ytok)
        if "f" in which:
            fox_phase(S, A, NB, R, T, PS, dr, ytok)
        if "g" in which:
            gdn_phase(S, A, NB, R, T, PS, dr, ytok)
        S.emit()
    return nc


def fox_phase(S, A, NB, R, T, PS, dr, ytok):
    C = R["consts"]
    parB, gates, B = R["parB"], R["gates"], R["big"]
    SEQ = NB * 128
    qk2 = [B.t[2 + i][:, 0:SEQ].bitcast(BF16).rearrange("p (a b) -> p a b", a=2) for i in range(2)]

    class _QK:
        def __getitem__(self, idx):
            p, j, sl = idx
            return qk2[j // 2][p, j % 2, sl]
    qk = _QK()
    v1b = B.t[4][:, 0:NB * 130].bitcast(BF16).rearrange("p (h n c) -> p h n c", h=2, n=NB)
    NSB = 6
    SBK = [0, 1, 2, 5, 6, 7]
    PTb = [A.sb([128, 128], BF16) for _ in range(NSB)]
    bias = A.sb([128, NB, NB], F32)
    rr = A.sb([128, 1], F32)
    tokv = dr["tokm"].rearrange("(b p) c -> p b c", p=128)
    yv = ytok.rearrange("(b p) c -> p b c", p=128)
    for j in range(4):
        S.dma("sp", lambda e, j=j: e.dma_start(out=qk[:, j, :], in_=dr["f_qkT"][j]), reads=[("f_qkT_d",)], writes=[("B", 2 + j // 2, j % 2)])
    for hh in range(2):
        S.dma("pool", lambda e, hh=hh: e.dma_start(out=v1b[:, hh, :, 0:128], in_=tokv[:, :, 384 + 128 * hh:512 + 128 * hh]),
              reads=[("tokm_d",)], writes=[("B", 4, hh, 0)])
        S.dve(lambda e, hh=hh: e.memset(v1b[:, hh, :, 128:130], 1.0), writes=[("B", 4, hh, 1)])
    lf, cc, cum, tot = T["ff_lf"], T["ff_c"], T["ff_cum"], T["ff_tot"]
    for hh in range(2):
        oacc = B.v(hh, NB, 128)
        S.dve(lambda e, hh=hh: e.tensor_scalar(out=lf[:], in0=gates[:, :, 2 + hh], scalar1=parB[:, 2 + hh:3 + hh], scalar2=None, op0=ALU.add),
              reads=[("gates",), ("parB",)], writes=[("ff_lf",)])
        logsig_inplace(S, lf[:], ("ff_lf",))
        S.pe(lambda e: e.matmul(out=PS[0][:, 0:NB], lhsT=C[:, 1, :], rhs=lf[:], start=True, stop=True),
             reads=[("consts",), ("ff_lf",)], writes=[("ps", 0)])
        S.pe(lambda e: e.matmul(out=PS[1][:, 0:NB], lhsT=C[:, 2, :], rhs=lf[:], start=True, stop=True),
             reads=[("consts",), ("ff_lf",)], writes=[("ps", 1)])
        S.act(lambda e: e.copy(out=tot[:], in_=PS[1][:, 0:NB]), reads=[("ps", 1)], writes=[("ff_tot",)])
        S.dve(lambda e: e.tensor_copy(out=cum[:], in_=tot[:]), reads=[("ff_tot",)], writes=[("ff_cum",)])
        for j in range(1, NB):
            S.dve(lambda e, j=j: e.tensor_tensor(out=cum[:, j:j + 1], in0=cum[:, j - 1:j], in1=tot[:, j:j + 1], op=ALU.add),
                  reads=[("ff_cum",), ("ff_tot",)], writes=[("ff_cum",)])
        S.dve(lambda e: e.tensor_tensor(out=cc[:], in0=cum[:], in1=PS[0][:, 0:NB], op=ALU.add), reads=[("ff_cum",), ("ps", 0)], writes=[("ff_c",)])
        S.dve(lambda e: e.tensor_tensor(out=cc[:], in0=cc[:], in1=tot[:], op=ALU.subtract), reads=[("ff_c",), ("ff_tot",)], writes=[("ff_c",)])
        for kb in range(NB):
            S.dve(lambda e, kb=kb: e.tensor_scalar(out=bias[:, kb, :], in0=cum[:], scalar1=cc[:, kb:kb + 1], scalar2=None, op0=ALU.subtract),
                  reads=[("ff_cum",), ("ff_c",)], writes=[("f_bias", kb)])
        pairs = [(qb, kb) for qb in range(NB) for kb in range(qb + 1)]

        def emit_s(i, hh=hh):
            qb, kb = pairs[i]
            S.pe(lambda e: e.matmul(out=PS[SBK[i % NSB]][:, 0:128], lhsT=qk[:, 2 + hh, kb * 128:(kb + 1) * 128],
                                    rhs=qk[:, hh, qb * 128:(qb + 1) * 128], start=True, stop=True),
                 reads=[("B", 3, hh), ("B", 2, hh)], writes=[("ps", SBK[i % NSB])])
        LA = 4
        for i0 in range(min(LA, len(pairs))):
            emit_s(i0)
        for i, (qb, kb) in enumerate(pairs):
            pt = PTb[i % NSB]
            S.act(lambda e, i=i, qb=qb, kb=kb, pt=pt: e.activation(out=pt[:], in_=PS[SBK[i % NSB]][:, 0:128], func=AF.Exp, scale=ISQ,
                                                                 bias=bias[:, kb, qb:qb + 1]),
                  reads=[("ps", SBK[i % NSB]), ("f_bias", kb)], writes=[("f_PT", i % NSB)])
            if kb == qb:
                S.dve(lambda e, pt=pt: e.tensor_tensor(out=pt[:], in0=pt[:], in1=C[:, 1, :], op=ALU.mult),
                      reads=[("f_PT", i % NSB), ("consts",)], writes=[("f_PT", i % NSB)])
            ob = 3 + qb % 2
            S.pe(lambda e, pt=pt, hh=hh, kb=kb, qb=qb, ob=ob: e.matmul(out=PS[ob][:, 0:129], lhsT=pt[:], rhs=v1b[:, hh, kb, 0:129],
                                                                     start=(kb == 0), stop=(kb == qb)),
                 reads=[("f_PT", i % NSB), ("B", 4, hh)], writes=[("ps", ob)])
            if i + LA < len(pairs):
                emit_s(i + LA)
            if kb == qb:
                S.dve(lambda e, ob=ob: e.reciprocal(out=rr[:], in_=PS[ob][:, 128:129]), reads=[("ps", ob)], writes=[("f_rr",)])
                S.dve(lambda e, ob=ob, qb=qb, oacc=oacc: e.tensor_scalar(out=oacc[:, qb, :], in0=PS[ob][:, 0:128], scalar1=rr[:], scalar2=None,
                                                                        op0=ALU.mult),
                      reads=[("ps", ob), ("f_rr",)], writes=[("B", hh, qb)])
        sq = B.v(5, NB, 128)
        ssq = T["tmp"]
        S.act(lambda e, oacc=oacc: e.activation(out=sq, in_=oacc, func=AF.Square), reads=[("B", hh)], writes=[("B", 5)])
        S.dve(lambda e: e.tensor_reduce(out=ssq[:], in_=sq, axis=AX.X, op=ALU.add), reads=[("B", 5)], writes=[("tmp",)])
        S.dve(lambda e: e.tensor_scalar(out=ssq[:], in0=ssq[:], scalar1=1.0 / 128, scalar2=EPS, op0=ALU.mult, op1=ALU.add), reads=[("tmp",)], writes=[("tmp",)])
        S.act(lambda e: e.activation(out=ssq[:], in_=ssq[:], func=AF.Sqrt), reads=[("tmp",)], writes=[("tmp",)])
        S.dve(lambda e: e.reciprocal(out=ssq[:], in_=ssq[:]), reads=[("tmp",)], writes=[("tmp",)])
        S.dve(lambda e, oacc=oacc: e.tensor_tensor(out=oacc, in0=oacc, in1=bc(ssq[:].unsqueeze(2), [128, NB, 128]), op=ALU.mult),
              reads=[("B", hh), ("tmp",)], writes=[("B", hh)])
        S.dve(lambda e, oacc=oacc, hh=hh: e.tensor_tensor(out=oacc, in0=oacc, in1=bc(R["normw"][:, 1 + hh, :].unsqueeze(1), [128, NB, 128]), op=ALU.mult),
              reads=[("B", hh), ("normw",)], writes=[("B", hh)])
        S.dma("sp", lambda e, oacc=oacc, hh=hh: e.dma_start(out=yv[:, :, 128 + 128 * hh:256 + 128 * hh], in_=oacc),
              reads=[("B", hh)], writes=[("ytok_d", 1 + hh)])


def gdn_phase(S, A, NB, R, T, PS, dr, ytok):
    C = R["consts"]
    parB, gates, B, convw = R["parB"], R["gates"], R["big"], R["convw"]
    SEQ = NB * 128
    G = 2
    tokv = dr["tokm"].rearrange("(b p) c -> p b c", p=128)
    yv = ytok.rearrange("(b p) c -> p b c", p=128)
    ident, Uincl, ones, Lstrict, Lincl = (C[:, i, :] for i in range(5))
    gg, beta, gc, gl = T["gg"], T["gbeta"], T["ggc"], T["ggl"]
    be, kes, egl, na = T["ti"], T["tf"], T["lf"], T["par15"]
    S.act(lambda e: e.activation(out=na[:, 7:8], in_=parB[:, 4:5], func=AF.Exp), reads=[("parB",), ("par15",)], writes=[("par15",)])
    S.act(lambda e: e.activation(out=gg[:], in_=gates[:, :, 4], func=AF.Exp, bias=parB[:, 5:6]), reads=[("gates",), ("parB",)], writes=[("gg",)])
    S.act(lambda e: e.activation(out=gg[:], in_=gg[:], func=AF.Ln, bias=1.0), reads=[("gg",)], writes=[("gg",)])
    S.dve(lambda e: e.tensor_scalar(out=gg[:], in0=gg[:], scalar1=na[:, 7:8], scalar2=-1.0, op0=ALU.mult, op1=ALU.mult),
          reads=[("gg",), ("par15",)], writes=[("gg",)])
    S.act(lambda e: e.activation(out=beta[:], in_=gates[:, :, 5], func=AF.Sigmoid), reads=[("gates",)], writes=[("gbeta",)])
    S.pe(lambda e: e.matmul(out=PS[0][:, 0:NB], lhsT=Uincl, rhs=gg[:], start=True, stop=True), reads=[("consts",), ("gg",)], writes=[("ps", 0)])
    S.pe(lambda e: e.matmul(out=PS[1][:, 0:NB], lhsT=ones, rhs=gg[:], start=True, stop=True), reads=[("consts",), ("gg",)], writes=[("ps", 1)])
    S.act(lambda e: e.copy(out=gc[:], in_=PS[0][:, 0:NB]), reads=[("ps", 0)], writes=[("ggc",)])
    S.act(lambda e: e.copy(out=gl[:], in_=PS[1][:, 0:NB]), reads=[("ps", 1)], writes=[("ggl",)])
    S.act(lambda e: e.activation(out=be[:], in_=gc[:], func=AF.Exp), reads=[("ggc",)], writes=[("ti",)])
    S.dve(lambda e: e.tensor_tensor(out=be[:], in0=be[:], in1=beta[:], op=ALU.mult), reads=[("ti",), ("gbeta",)], writes=[("ti",)])
    S.dve(lambda e: e.tensor_tensor(out=kes[:], in0=gl[:], in1=gc[:], op=ALU.subtract), reads=[("ggl",), ("ggc",)], writes=[("tf",)])
    S.act(lambda e: e.activation(out=kes[:], in_=kes[:], func=AF.Exp), reads=[("tf",)], writes=[("tf",)])
    S.act(lambda e: e.activation(out=egl[:], in_=gl[:], func=AF.Exp), reads=[("ggl",)], writes=[("lf",)])
    for t in range(3):
        xin = B.t[0 if t % 2 == 0 else 4]
        xk = ("B", 0 if t % 2 == 0 else 4)
        acc = B.v(1 + t, SEQ)
        ak = ("B", 1 + t)
        S.dve(lambda e, xin=xin: e.memset(xin[:, 0:3], 0.0), writes=[xk + (0,)])
        S.dma("sp", lambda e, xin=xin, t=t: e.dma_start(out=xin[:, 3:3 + SEQ], in_=dr["g_T"][t]), reads=[("fm_d",)], writes=[xk + (1,)])
        S.dve(lambda e, xin=xin, acc=acc, t=t: e.tensor_scalar(out=acc, in0=xin[:, 3:3 + SEQ], scalar1=convw[:, 4 * t + 3:4 * t + 4], scalar2=None, op0=ALU.mult),
              reads=[xk, ("convw",)], writes=[ak])
        for j in range(3):
            S.dve(lambda e, xin=xin, acc=acc, t=t, j=j: e.scalar_tensor_tensor(out=acc, in0=xin[:, j:j + SEQ], scalar=convw[:, 4 * t + j:4 * t + j + 1],
                                                                               in1=acc, op0=ALU.mult, op1=ALU.add),
                  reads=[xk, ("convw",), ak], writes=[ak])
        S.act(lambda e, acc=acc: e.activation(out=acc, in_=acc, func=AF.Silu), reads=[ak], writes=[ak])
    sq = B.v(5, SEQ)
    rn = B.v(6, SEQ)
    for t in range(2):
        src = B.v(1 + t, SEQ)
        S.act(lambda e, src=src: e.activation(out=sq, in_=src, func=AF.Square), reads=[("B", 1 + t)], writes=[("B", 5)])
        for sl in range(0, SEQ, 512):
            pb = PS[(sl // 512) % 2]
            S.pe(lambda e, sl=sl, pb=pb: e.matmul(out=pb[:, 0:512], lhsT=ones, rhs=sq[:, sl:sl + 512], start=True, stop=True),
                 reads=[("consts",), ("B", 5)], writes=[("ps", (sl // 512) % 2)])
            S.dve(lambda e, sl=sl, pb=pb: e.tensor_scalar(out=rn[:, sl:sl + 512], in0=pb[:, 0:512], scalar1=EPS, scalar2=None, op0=ALU.add),
                  reads=[("ps", (sl // 512) % 2)], writes=[("B", 6, sl)])
        S.act(lambda e: e.activation(out=rn, in_=rn, func=AF.Sqrt), reads=[("B", 6)], writes=[("B", 6)])
        S.dve(lambda e: e.reciprocal(out=rn, in_=rn), reads=[("B", 6)], writes=[("B", 6)])
        if t == 0:
            S.dve(lambda e, src=src: e.scalar_tensor_tensor(out=src, in0=src, scalar=ISQ, in1=rn, op0=ALU.mult, op1=ALU.mult),
                  reads=[("B", 1), ("B", 6)], writes=[("B", 1)])
        else:
            S.dve(lambda e, src=src: e.tensor_tensor(out=src, in0=src, in1=rn, op=ALU.mult), reads=[("B", 2), ("B", 6)], writes=[("B", 2)])
    qT, kT, vT = B.v(1, SEQ), B.v(2, SEQ), B.v(3, SEQ)
    ktok, vtok = B.v(4, NB, 128), B.v(0, NB, 128)
    for (src, dst, sk, dk) in ((kT, ktok, ("B", 2), ("B", 4)), (vT, vtok, ("B", 3), ("B", 0))):
        for g0 in range(0, NB, 4):
            pb = PS[2 + (g0 // 4) % 2]
            for j in range(4):
                S.pe(lambda e, pb=pb, j=j, g0=g0, src=src: e.transpose(out=pb[:, j * 128:(j + 1) * 128], in_=src[:, (g0 + j) * 128:(g0 + j + 1) * 128], identity=ident),
                     reads=[sk, ("consts",)], writes=[("ps", 2 + (g0 // 4) % 2)])
            S.act(lambda e, pb=pb, g0=g0, dst=dst: e.copy(out=dst[:, g0:g0 + 4, :], in_=pb[:, 0:512].rearrange("p (a b) -> p a b", a=4)),
                  reads=[("ps", 2 + (g0 // 4) % 2)], writes=[dk + (g0,)])
    diagG = B.v(6, NB, 128)
    Grow = B.v(5, SEQ)
    S.dve(lambda e: e.tensor_tensor(out=diagG, in0=bc(ident.unsqueeze(1), [128, NB, 128]), in1=bc(gc[:].unsqueeze(2), [128, NB, 128]), op=ALU.mult),
          reads=[("consts",), ("ggc",)], writes=[("B", 6)])
    dflat = B.v(6, SEQ)
    for sl in range(0, SEQ, 512):
        pb = PS[(sl // 512) % 2]
        S.pe(lambda e, sl=sl, pb=pb: e.matmul(out=pb[:, 0:512], lhsT=ones, rhs=dflat[:, sl:sl + 512], start=True, stop=True),
             reads=[("consts",), ("B", 6)], writes=[("ps", (sl // 512) % 2)])
        S.act(lambda e, sl=sl, pb=pb: e.copy(out=Grow[:, sl:sl + 512], in_=pb[:, 0:512]), reads=[("ps", (sl // 512) % 2)], writes=[("B", 5, sl)])
    G3 = B.v(5, NB, 128)
    Dm, DTm = B.v(6, NB, 128), B.v(7, NB, 128)
    gcb = bc(gc[:].unsqueeze(2), [128, NB, 128])
    S.dve(lambda e: e.tensor_tensor(out=Dm, in0=gcb, in1=G3, op=ALU.subtract), reads=[("ggc",), ("B", 5)], writes=[("B", 6)])
    S.dve(lambda e: e.tensor_scalar(out=Dm, in0=Dm, scalar1=0.0, scalar2=None, op0=ALU.min), reads=[("B", 6)], writes=[("B", 6)])
    S.act(lambda e: e.activation(out=Dm, in_=Dm, func=AF.Exp), reads=[("B", 6)], writes=[("B", 6)])
    S.dve(lambda e: e.tensor_tensor(out=Dm, in0=Dm, in1=bc(Lstrict.unsqueeze(1), [128, NB, 128]), op=ALU.mult), reads=[("B", 6), ("consts",)], writes=[("B", 6)])
    S.dve(lambda e: e.tensor_tensor(out=Dm, in0=Dm, in1=bc(beta[:].unsqueeze(2), [128, NB, 128]), op=ALU.mult), reads=[("B", 6), ("gbeta",)], writes=[("B", 6)])
    S.dve(lambda e: e.tensor_tensor(out=DTm, in0=G3, in1=gcb, op=ALU.subtract), reads=[("ggc",), ("B", 5)], writes=[("B", 7)])
    S.dve(lambda e: e.tensor_scalar(out=DTm, in0=DTm, scalar1=0.0, scalar2=None, op0=ALU.min), reads=[("B", 7)], writes=[("B", 7)])
    S.act(lambda e: e.activation(out=DTm, in_=DTm, func=AF.Exp), reads=[("B", 7)], writes=[("B", 7)])
    S.dve(lambda e: e.tensor_tensor(out=DTm, in0=DTm, in1=bc(Uincl.unsqueeze(1), [128, NB, 128]), op=ALU.mult), reads=[("B", 7), ("consts",)], writes=[("B", 7)])
    S.act(lambda e: e.activation(out=Grow, in_=Grow, func=AF.Exp), reads=[("B", 5)], writes=[("B", 5)])
    qdT = B.v(3, SEQ)
    S.dve(lambda e: e.tensor_tensor(out=qdT, in0=qT, in1=Grow, op=ALU.mult), reads=[("B", 1), ("B", 5)], writes=[("B", 3)])
    zt = B.v(5, NB, 128)
    S.dma("sp", lambda e: e.dma_start(out=zt, in_=tokv[:, :, 640:768]), reads=[("tokm_d",)], writes=[("B", 5)])
    oall = B.v(8, NB, 128)
    Sg = A.sb([128, 128], F32)
    S.dve(lambda e: e.memset(Sg[:], 0.0), writes=[("g_S",)])
    names = ["Ag", "ATg", "P0", "P1", "PT0", "PT1", "Rg", "attnT", "kbe", "vb", "kend", "Wval", "WkT"]
    X = {n: A.sb([128, G, 128], F32) for n in names}
    ub = [A.sb([128, 128], F32) for _ in range(2)]

    def mm4(out_bank, bank_idx, lhs_fn, rhs_fn, reads):
        for j in range(G):
            l_, r_ = lhs_fn(j), rhs_fn(j)
            S.pe(lambda e, j=j, l_=l_, r_=r_: e.matmul(out=out_bank[:, j * 128:(j + 1) * 128], lhsT=l_, rhs=r_, start=True, stop=True),
                 reads=reads, writes=[("ps", bank_idx)])

    def v4(bank):
        return bank[:, 0:G * 128].rearrange("p (a b) -> p a b", a=G)
    for c0 in range(0, NB, G):
        blk = lambda j: slice((c0 + j) * 128, (c0 + j + 1) * 128)
        mm4(PS[0], 0, lambda j: kT[:, blk(j)], lambda j: kT[:, blk(j)], [("B", 2)])
        S.dve(lambda e, c0=c0: e.tensor_tensor(out=X["Ag"][:], in0=v4(PS[0]), in1=Dm[:, c0:c0 + G, :], op=ALU.mult),
              reads=[("ps", 0), ("B", 6)], writes=[("Ag",)])
        for j in range(G):
            S.pe(lambda e, j=j: e.transpose(out=PS[2][:, j * 128:(j + 1) * 128], in_=X["Ag"][:, j, :], identity=ident),
                 reads=[("Ag",), ("consts",)], writes=[("ps", 2)])
        S.act(lambda e: e.copy(out=X["ATg"][:], in_=v4(PS[2])), reads=[("ps", 2)], writes=[("ATg",)])
        S.dve(lambda e: e.tensor_tensor(out=X["Rg"][:], in0=bc(ident.unsqueeze(1), [128, G, 128]), in1=X["ATg"][:], op=ALU.subtract),
              reads=[("consts",), ("ATg",)], writes=[("Rg",)])
        Pc, PTc = "Ag", "ATg"
        for lvl in range(1, 7):
            Pn = "P%d" % (lvl % 2)
            PTn = "PT%d" % (lvl % 2)
            mm4(PS[3], 3, lambda j, PTc=PTc: X[PTc][:, j, :], lambda j, Pc=Pc: X[Pc][:, j, :], [(Pc,), (PTc,)])
            S.act(lambda e, Pn=Pn: e.copy(out=X[Pn][:], in_=v4(PS[3])), reads=[("ps", 3)], writes=[(Pn,)])
            if lvl < 6:
                mm4(PS[4], 4, lambda j, Pc=Pc: X[Pc][:, j, :], lambda j, PTc=PTc: X[PTc][:, j, :], [(Pc,), (PTc,)])
                S.act(lambda e, PTn=PTn: e.copy(out=X[PTn][:], in_=v4(PS[4])), reads=[("ps", 4)], writes=[(PTn,)])
            mm4(PS[5], 5, lambda j, Pn=Pn: X[Pn][:, j, :], lambda j: X["Rg"][:, j, :], [(Pn,), ("Rg",)])
            S.dve(lambda e: e.tensor_tensor(out=X["Rg"][:], in0=X["Rg"][:], in1=v4(PS[5]), op=ALU.add), reads=[("Rg",), ("ps", 5)], writes=[("Rg",)])
            Pc, PTc = Pn, PTn
        mm4(PS[1], 1, lambda j: kT[:, blk(j)], lambda j: qT[:, blk(j)], [("B", 2), ("B", 1)])
        S.dve(lambda e, c0=c0: e.tensor_tensor(out=X["attnT"][:], in0=v4(PS[1]), in1=DTm[:, c0:c0 + G, :], op=ALU.mult),
              reads=[("ps", 1), ("B", 7)], writes=[("attnT",)])
        S.pool(lambda e, c0=c0: e.tensor_tensor(out=X["kbe"][:], in0=ktok[:, c0:c0 + G, :], in1=bc(be[:, c0:c0 + G].unsqueeze(2), [128, G, 128]), op=ALU.mult),
               reads=[("B", 4), ("ti",)], writes=[("kbe",)])
        S.pool(lambda e, c0=c0: e.tensor_tensor(out=X["vb"][:], in0=vtok[:, c0:c0 + G, :], in1=bc(beta[:, c0:c0 + G].unsqueeze(2), [128, G, 128]), op=ALU.mult),
               reads=[("B", 0), ("gbeta",)], writes=[("vb",)])
        S.pool(lambda e, c0=c0: e.tensor_tensor(out=X["kend"][:], in0=ktok[:, c0:c0 + G, :], in1=bc(kes[:, c0:c0 + G].unsqueeze(2), [128, G, 128]), op=ALU.mult),
               reads=[("B", 4), ("tf",)], writes=[("kend",)])
        mm4(PS[0], 0, lambda j: X["Rg"][:, j, :], lambda j: X["vb"][:, j, :], [("Rg",), ("vb",)])
        S.act(lambda e: e.copy(out=X["Wval"][:], in_=v4(PS[0])), reads=[("ps", 0)], writes=[("Wval",)])
        mm4(PS[1], 1, lambda j: X["kbe"][:, j, :], lambda j: X["Rg"][:, j, :], [("kbe",), ("Rg",)])
        S.act(lambda e: e.copy(out=X["WkT"][:], in_=v4(PS[1])), reads=[("ps", 1)], writes=[("WkT",)])
        for j in range(G):
            c = c0 + j
            u = ub[c % 2]
            uk = ("g_u", c % 2)
            S.pe(lambda e, j=j: e.matmul(out=PS[6][:, 0:128], lhsT=X["WkT"][:, j, :], rhs=Sg[:], start=True, stop=True),
                 reads=[("WkT",), ("g_S",)], writes=[("ps", 6)])
            S.dve(lambda e, j=j, u=u: e.tensor_tensor(out=u[:], in0=X["Wval"][:, j, :], in1=PS[6][:, 0:128], op=ALU.subtract),
                  reads=[("Wval",), ("ps", 6)], writes=[uk])
            S.pe(lambda e, c=c: e.matmul(out=PS[6][:, 128:256], lhsT=qdT[:, c * 128:(c + 1) * 128], rhs=Sg[:], start=True, stop=False),
                 reads=[("B", 3), ("g_S",)], writes=[("ps", 6)])
            S.pe(lambda e, j=j, u=u: e.matmul(out=PS[6][:, 128:256], lhsT=X["attnT"][:, j, :], rhs=u[:], start=False, stop=True),
                 reads=[("attnT",), uk], writes=[("ps", 6)])
            S.act(lambda e, c=c: e.copy(out=oall[:, c, :], in_=PS[6][:, 128:256]), reads=[("ps", 6)], writes=[("B", 8, c)])
            S.pe(lambda e, j=j, u=u: e.matmul(out=PS[7][:, 0:128], lhsT=X["kend"][:, j, :], rhs=u[:], start=True, stop=True),
                 reads=[("kend",), uk], writes=[("ps", 7)])
            S.dve(lambda e, c=c: e.scalar_tensor_tensor(out=Sg[:], in0=Sg[:], scalar=egl[:, c:c + 1], in1=PS[7][:, 0:128], op0=ALU.mult, op1=ALU.add),
                  reads=[("g_S",), ("lf",), ("ps", 7)], writes=[("g_S",)])
    sq2 = B.v(6, NB, 128)
    ssq = T["tmp"]
    S.act(lambda e: e.activation(out=sq2, in_=oall, func=AF.Square), reads=[("B", 8)], writes=[("B", 6)])
    S.dve(lambda e: e.tensor_reduce(out=ssq[:], in_=sq2, axis=AX.X, op=ALU.add), reads=[("B", 6)], writes=[("tmp",)])
    S.dve(lambda e: e.tensor_scalar(out=ssq[:], in0=ssq[:], scalar1=1.0 / 128, scalar2=EPS, op0=ALU.mult, op1=ALU.add), reads=[("tmp",)], writes=[("tmp",)])
    S.act(lambda e: e.activation(out=ssq[:], in_=ssq[:], func=AF.Sqrt), reads=[("tmp",)], writes=[("tmp",)])
    S.dve(lambda e: e.reciprocal(out=ssq[:], in_=ssq[:]), reads=[("tmp",)], writes=[("tmp",)])
    S.dve(lambda e: e.tensor_tensor(out=oall, in0=oall, in1=bc(ssq[:].unsqueeze(2), [128, NB, 128]), op=ALU.mult), reads=[("B", 8), ("tmp",)], writes=[("B", 8)])
    S.dve(lambda e: e.tensor_tensor(out=oall, in0=oall, in1=bc(R["normw"][:, 3, :].unsqueeze(1), [128, NB, 128]), op=ALU.mult),
          reads=[("B", 8), ("normw",)], writes=[("B", 8)])
    S.act(lambda e: e.activation(out=zt, in_=zt, func=AF.Silu), reads=[("B", 5)], writes=[("B", 5)])
    S.dve(lambda e: e.tensor_tensor(out=oall, in0=oall, in1=zt, op=ALU.mult), reads=[("B", 8), ("B", 5)], writes=[("B", 8)])
    S.dma("sp", lambda e: e.dma_start(out=yv[:, :, 384:512], in_=oall), reads=[("B", 8)], writes=[("ytok_d", 3)])


def pre_tiles(A):
    hst = [A.sb([128, 16, 128], BF16) for _ in range(2)]
    ss = [A.sb([128, 1], F32) for _ in range(2)]
    rs = [A.sb([128, 1], F32) for _ in range(2)]
    stf = [A.sb([128, 512], F32) for _ in range(2)]
    stb = [A.sb([128, 512], BF16) for _ in range(2)]
    stt_ = [A.sb([128, 896], F32) for _ in range(2)]
    return hst, ss, rs, stf, stb, stt_


def pre_phase(S, A, NB, R, PS, x, nw, w, dr, add_src=None, store_dst=None, xkey=None):
    C = R["consts"]
    B = R["big"]
    ident = C[:, 0, :]
    SEQ = NB * 128
    hTv = dr["hTd"].rearrange("k p s -> p k s")
    nwB = R["big"].t[8][:, 0:2048]
    hst, ss, rs, stf, stb, stt_ = R["pre_tiles"]
    S.dma("sp", lambda e: e.dma_start(out=nwB, in_=nw.partition_broadcast(128)), reads=[("B", 8)], writes=[("nwB",), ("B", 8)])
    wv = w.rearrange("(kc p) n -> p kc n", p=128)
    Wb = []
    for q in range(4):
        t = B.t[4 + q][:, 0:4096].bitcast(BF16).rearrange("p (a b) -> p a b", a=4)
        Wb.append(t)
        for h2 in range(2):
            S.dma("pool", lambda e, t=t, q=q, h2=h2: e.dma_start(out=t[:, 2 * h2:2 * h2 + 2, :], in_=wv[:, 4 * q + 2 * h2:4 * q + 2 * h2 + 2, :]),
                  writes=[("B", 4 + q, h2)])
    ev = [0]

    def stage_a(tt):
        b = tt % 2
        xt = B.t[b][:, 0:2048]
        hf = B.t[2 + b][:, 0:2048]
        ssb, rsb = ss[b], rs[b]
        S.dma("sp", lambda e: e.dma_start(out=xt, in_=x[tt * 128:(tt + 1) * 128, :]), reads=([xkey] if xkey else []), writes=[("B", b)])
        if add_src is not None:
            S.dma("sp", lambda e: e.dma_start(out=hf, in_=add_src[tt * 128:(tt + 1) * 128, :]), reads=[("sumb", tt // 4)], writes=[("B", 2 + b)])
            S.dve(lambda e: e.tensor_tensor(out=xt, in0=xt, in1=hf, op=ALU.add), reads=[("B", b), ("B", 2 + b)], writes=[("B", b)])
            S.dma("sp", lambda e: e.dma_start(out=store_dst[tt * 128:(tt + 1) * 128, :], in_=xt), reads=[("B", b)], writes=[("xa_d", tt)])
        S.act(lambda e: e.activation(out=hf, in_=xt, func=AF.Square, accum_out=ssb[:]), reads=[("B", b)], writes=[("B", 2 + b), ("ss", b)])
        S.dve(lambda e: e.tensor_scalar(out=rsb[:], in0=ssb[:], scalar1=1.0 / D_MODEL, scalar2=EPS, op0=ALU.mult, op1=ALU.add), reads=[("ss", b)], writes=[("rs", b)])
        S.act(lambda e: e.activation(out=rsb[:], in_=rsb[:], func=AF.Sqrt), reads=[("rs", b)], writes=[("rs", b)])
        S.dve(lambda e: e.reciprocal(out=rsb[:], in_=rsb[:]), reads=[("rs", b)], writes=[("rs", b)])
        S.dve(lambda e: e.scalar_tensor_tensor(out=hf, in0=xt, scalar=rsb[:], in1=nwB, op0=ALU.mult, op1=ALU.mult),
              reads=[("B", b), ("rs", b), ("nwB",)], writes=[("B", 2 + b)])

    def stage_b(tt):
        b = tt % 2
        hf = B.t[2 + b][:, 0:2048]
        for g in range(4):
            pb = g % 2
            for j in range(4):
                kc = g * 4 + j
                S.pe(lambda e, pb=pb, j=j, kc=kc: e.transpose(out=PS[pb][:, j * 128:(j + 1) * 128], in_=hf[:, kc * 128:(kc + 1) * 128], identity=ident),
                     reads=[("B", 2 + b), ("consts",)], writes=[("ps", pb)])
            src = PS[pb][:, 0:512].rearrange("p (a b) -> p a b", a=4)
            dst = hst[b][:, g * 4:(g + 1) * 4, :]
            if ev[0] % 2 == 0:
                S.act(lambda e, src=src, dst=dst: e.copy(out=dst, in_=src), reads=[("ps", pb)], writes=[("hst", b, g)])
            else:
                S.dve(lambda e, src=src, dst=dst: e.tensor_copy(out=dst, in_=src), reads=[("ps", pb)], writes=[("hst", b, g)])
            ev[0] += 1
        S.dma("sp", lambda e: e.dma_start(out=hTv[:, :, tt * 128:(tt + 1) * 128], in_=hst[b][:]), reads=[("hst", b)], writes=[("hTd", tt)])
    for t in range(NB + 1):
        if t < NB:
            stage_a(t)
        if t >= 1:
            stage_b(t - 1)
    tokv = dr["tokm"]
    it = 0
    for sb in range(SEQ // 512):
        hs = B.t[sb % 2][:, 0:4096].bitcast(BF16).rearrange("p (a b) -> p a b", a=16)
        hk = ("B", sb % 2)
        S.dma("sp", lambda e, sb=sb, hs=hs: e.dma_start(out=hs, in_=hTv[:, :, sb * 512:(sb + 1) * 512]), reads=[("hTd",)], writes=[hk])
        cs = slice(sb * 512, (sb + 1) * 512)
        for fg in range(9):
            pb = 2 + fg % 2
            for kc in range(16):
                S.pe(lambda e, pb=pb, kc=kc, fg=fg, hs=hs: e.matmul(out=PS[pb][:, 0:512], lhsT=Wb[kc // 4][:, kc % 4, fg * 128:(fg + 1) * 128],
                                                                  rhs=hs[:, kc, :], start=(kc == 0), stop=(kc == 15)),
                     reads=[("B", 4 + kc // 4), hk], writes=[("ps", pb)])
            it += 1
            if 2 <= fg <= 5:
                st_ = stb[it % 2]
                S.act(lambda e, pb=pb, st_=st_: e.copy(out=st_[:], in_=PS[pb][:, 0:512]), reads=[("ps", pb)], writes=[("stb", it % 2)])
                S.dma("sp", lambda e, st_=st_, fg=fg, cs=cs: e.dma_start(out=dr["f_qkT"][fg - 2][:, cs], in_=st_[:]), reads=[("stb", it % 2)], writes=[("f_qkT_d", sb, fg)])
            else:
                st_ = stf[it % 2]
                dst = dr["m_qT"] if fg == 0 else dr["m_kT"] if fg == 1 else dr["g_T"][fg - 6]
                S.dve(lambda e, pb=pb, st_=st_: e.tensor_copy(out=st_[:], in_=PS[pb][:, 0:512]), reads=[("ps", pb)], writes=[("stf", it % 2)])
                S.dma("sp", lambda e, st_=st_, dst=dst, cs=cs: e.dma_start(out=dst[:, cs], in_=st_[:]), reads=[("stf", it % 2)], writes=[("fm_d", sb, fg)])
        for t4 in range(4):
            blk = sb * 4 + t4
            st_ = stt_[blk % 2]
            for (pb, c0, cw, o0) in ((4, 1152, 512, 0), (5, 1664, 384, 512)):
                for kc in range(16):
                    S.pe(lambda e, pb=pb, kc=kc, c0=c0, cw=cw, t4=t4, hs=hs: e.matmul(out=PS[pb][:, 0:cw], lhsT=hs[:, kc, t4 * 128:(t4 + 1) * 128],
                                                                                   rhs=Wb[kc // 4][:, kc % 4, c0:c0 + cw], start=(kc == 0), stop=(kc == 15)),
                         reads=[("B", 4 + kc // 4), hk], writes=[("ps", pb)])
                if pb == 4:
                    S.act(lambda e, pb=pb, st_=st_, cw=cw, o0=o0: e.copy(out=st_[:, o0:o0 + cw], in_=PS[pb][:, 0:cw]), reads=[("ps", pb)], writes=[("stt", blk % 2, 0)])
                else:
                    S.dve(lambda e, pb=pb, st_=st_, cw=cw, o0=o0: e.tensor_copy(out=st_[:, o0:o0 + cw], in_=PS[pb][:, 0:cw]), reads=[("ps", pb)], writes=[("stt", blk % 2, 1)])
            S.dma("sp", lambda e, st_=st_, blk=blk: e.dma_start(out=tokv[blk * 128:(blk + 1) * 128, :], in_=st_[:, 0:768]), reads=[("stt", blk % 2)], writes=[("tokm_d", blk)])
            S.act(lambda e, st_=st_, blk=blk: e.copy(out=R["gates"][:, blk, :], in_=st_[:, 768:774]), reads=[("stt", blk % 2)], writes=[("gates", blk)])


def build_A(NB=32):
    nc = bass.Bass("TRN2", target_bir_lowering=False)
    SEQ = NB * 128
    x = nc.dram_tensor("x", [SEQ, D_MODEL], F32, kind="ExternalInput").ap()
    nw = nc.dram_tensor("nw", [D_MODEL], F32, kind="ExternalInput").ap()
    w = nc.dram_tensor("w", [D_MODEL, 2048], F32, kind="ExternalInput").ap()
    params = nc.dram_tensor("params", [8], F32, kind="ExternalInput").ap()
    normw_d = nc.dram_tensor("normw", [4 * 128], F32, kind="ExternalInput").ap()
    convw_d = nc.dram_tensor("convw", [128, 12], F32, kind="ExternalInput").ap()
    consts_d = nc.dram_tensor("consts", [128, 5, 128], F32, kind="ExternalInput").ap()
    ytok = nc.dram_tensor("ytok", [SEQ, 512], F32, kind="ExternalOutput").ap()
    dr = {}
    dr["hTd"] = nc.dram_tensor("hTd", [16, 128, SEQ], BF16, kind="Internal").ap()
    dr["m_qT"] = nc.dram_tensor("m_qT", [128, SEQ], F32, kind="Internal").ap()
    dr["m_kT"] = nc.dram_tensor("m_kT", [128, SEQ], F32, kind="Internal").ap()
    dr["tokm"] = nc.dram_tensor("tokm", [SEQ, 768], F32, kind="Internal").ap()
    dr["f_qkT"] = nc.dram_tensor("f_qkT", [4, 128, SEQ], BF16, kind="Internal").ap()
    dr["g_T"] = nc.dram_tensor("g_T", [3, 128, SEQ], F32, kind="Internal").ap()
    S = Sched(nc)
    with contextlib.ExitStack() as st:
        A = Alloc(nc, st)
        R = {}
        R["consts"] = A.sb([128, 5, 128], F32)
        R["parB"] = A.sb([128, 8], F32)
        R["gates"] = A.sb([128, NB, 6], F32)
        R["normw"] = A.sb([128, 4, 128], F32)
        R["convw"] = A.sb([128, 12], F32)
        R["big"] = Big(A, 9)
        R["pre_tiles"] = pre_tiles(A)
        PS = [A.ps([128, 512], F32) for _ in range(8)]
        S.dma("sp", lambda e: e.dma_start(out=R["consts"][:], in_=consts_d), writes=[("consts",)])
        S.dma("sp", lambda e: e.dma_start(out=R["parB"][:], in_=params.partition_broadcast(128)), writes=[("parB",)])
        S.dma("sp", lambda e: e.dma_start(out=R["normw"][:].rearrange("p a b -> p (a b)"), in_=normw_d.partition_broadcast(128)), writes=[("normw",)])
        S.dma("sp", lambda e: e.dma_start(out=R["convw"][:], in_=convw_d), writes=[("convw",)])
        pre_phase(S, A, NB, R, PS, x, nw, w, dr)
        for k in ("m_qT", "m_kT", "tokm", "f_qkT", "g_T"):
            pass
        T = mixer_prelude(S, A, NB, R)
        mlstm_phase(S, A, NB, R, T, PS, dr, ytok)
        fox_phase(S, A, NB, R, T, PS, dr, ytok)
        gdn_phase(S, A, NB, R, T, PS, dr, ytok)
        S.emit()
    return nc


def build_B(NTOK=8192, FINAL=False):
    nc = bass.Bass("TRN2", target_bir_lowering=False)
    D, F = D_MODEL, D_FF
    x = nc.dram_tensor("x", [NTOK, D], F32, kind="ExternalInput").ap()
    y = nc.dram_tensor("y", [NTOK, D], F32, kind="ExternalInput").ap()
    wo = nc.dram_tensor("wo", [D, D], F32, kind="ExternalInput").ap()
    wg = nc.dram_tensor("wg", [D, F], F32, kind="ExternalInput").ap()
    wu = nc.dram_tensor("wu", [D, F], F32, kind="ExternalInput").ap()
    wd = nc.dram_tensor("wd", [F, D], F32, kind="ExternalInput").ap()
    nw2 = nc.dram_tensor("nw2", [D], F32, kind="ExternalInput").ap()
    fnw = nc.dram_tensor("fnw", [D], F32, kind="ExternalInput").ap()
    identd = nc.dram_tensor("ident", [128, 128], F32, kind="ExternalInput").ap()
    xo = nc.dram_tensor("xo", [NTOK, D], F32, kind="ExternalOutput").ap()
    FC = F // 128
    S = Sched(nc)
    with contextlib.ExitStack() as st:
        A = Alloc(nc, st)
        ident = A.sb([128, 128], F32)
        nwB = A.sb([128, D], F32)
        fnB = A.sb([128, D], F32)
        x1 = A.sb([128, 4, D], F32)
        yt = [A.sb([128, D], F32) for _ in range(2)]
        yT = A.sb([128, 16, 512], BF16)
        hT = A.sb([128, 16, 512], BF16)
        aT = A.sb([128, FC, 512], BF16)
        wot = [A.sb([128, 16, 512], BF16) for _ in range(1)]
        wgt = [A.sb([128, 16, 128], BF16) for _ in range(2)]
        wut = [A.sb([128, 16, 128], BF16) for _ in range(2)]
        wdt = [A.sb([128, FC, 256], BF16) for _ in range(1)]
        sg = [A.sb([128, 512], BF16) for _ in range(2)]
        ss = A.sb([128, 1], F32)
        rs = A.sb([128, 1], F32)
        PS = [A.ps([128, 512], F32) for _ in range(8)]
        S.dma("sp", lambda e: e.dma_start(out=ident[:], in_=identd), writes=[("ident",)])
        S.dma("sp", lambda e: e.dma_start(out=nwB[:], in_=nw2.partition_broadcast(128)), writes=[("nwB",)])
        S.dma("sp", lambda e: e.dma_start(out=fnB[:], in_=fnw.partition_broadcast(128)), writes=[("fnB",)])
        wov = wo.rearrange("(kc p) n -> p kc n", p=128)
        wgv = wg.rearrange("(kc p) n -> p kc n", p=128)
        wuv = wu.rearrange("(kc p) n -> p kc n", p=128)
        wdv = wd.rearrange("(fc p) n -> p fc n", p=128)
        ev = [0]

        def transposes(src, srckey, dstT, dstkey, tt):
            for g in range(4):
                pb = g % 2
                for j in range(4):
                    kc = g * 4 + j
                    S.pe(lambda e, pb=pb, j=j, kc=kc: e.transpose(out=PS[pb][:, j * 128:(j + 1) * 128], in_=src[:, kc * 128:(kc + 1) * 128], identity=ident[:]),
                         reads=[srckey, ("ident",)], writes=[("ps", pb)])
                s_ = PS[pb][:, 0:512].rearrange("p (a b) -> p a b", a=4)
                d_ = dstT[:, g * 4:(g + 1) * 4, tt * 128:(tt + 1) * 128]
                if ev[0] % 2 == 0:
                    S.act(lambda e, s_=s_, d_=d_: e.copy(out=d_, in_=s_), reads=[("ps", pb)], writes=[dstkey + (tt, g)])
                else:
                    S.dve(lambda e, s_=s_, d_=d_: e.tensor_copy(out=d_, in_=s_), reads=[("ps", pb)], writes=[dstkey + (tt, g)])
                ev[0] += 1

        def rms(src, srckey, wB, wkey, dst, dstkey):
            S.act(lambda e: e.activation(out=dst, in_=src, func=AF.Square, accum_out=ss[:]), reads=[srckey], writes=[dstkey, ("ss",)])
            S.dve(lambda e: e.tensor_scalar(out=rs[:], in0=ss[:], scalar1=1.0 / D, scalar2=EPS, op0=ALU.mult, op1=ALU.add), reads=[("ss",)], writes=[("rs",)])
            S.act(lambda e: e.activation(out=rs[:], in_=rs[:], func=AF.Sqrt), reads=[("rs",)], writes=[("rs",)])
            S.dve(lambda e: e.reciprocal(out=rs[:], in_=rs[:]), reads=[("rs",)], writes=[("rs",)])
            S.dve(lambda e: e.scalar_tensor_tensor(out=dst, in0=src, scalar=rs[:], in1=wB[:], op0=ALU.mult, op1=ALU.mult),
                  reads=[srckey, ("rs",), wkey], writes=[dstkey])
        it = 0
        for sl in range(NTOK // 512):
            s0 = sl * 512
            for tt in range(4):
                r0 = s0 + tt * 128
                b = tt % 2
                S.dma("sp", lambda e, r0=r0, b=b: e.dma_start(out=yt[b][:], in_=y[r0:r0 + 128, :]), writes=[("yt", b)])
                S.dma("sp", lambda e, r0=r0, tt=tt: e.dma_start(out=x1[:, tt, :], in_=x[r0:r0 + 128, :]), writes=[("x1", tt)])
                transposes(yt[b], ("yt", b), yT, ("yT",), tt)
            for ct in range(4):
                S.dma("pool", lambda e, ct=ct: e.dma_start(out=wot[0][:], in_=wov[:, :, ct * 512:(ct + 1) * 512]), writes=[("wot",)])
                for tt in range(4):
                    pb = 2 + it % 2
                    it += 1
                    for kc in range(16):
                        S.pe(lambda e, pb=pb, kc=kc, tt=tt: e.matmul(out=PS[pb][:, 0:512], lhsT=yT[:, kc, tt * 128:(tt + 1) * 128], rhs=wot[0][:, kc, :],
                                                                   start=(kc == 0), stop=(kc == 15)),
                             reads=[("yT", tt), ("wot",)], writes=[("ps", pb)])
                    S.dve(lambda e, pb=pb, tt=tt, ct=ct: e.tensor_tensor(out=x1[:, tt, ct * 512:(ct + 1) * 512], in0=x1[:, tt, ct * 512:(ct + 1) * 512],
                                                                       in1=PS[pb][:, 0:512], op=ALU.add),
                          reads=[("x1", tt), ("ps", pb)], writes=[("x1", tt)])
            for tt in range(4):
                b = tt % 2
                rms(x1[:, tt, :], ("x1", tt), nwB, ("nwB",), yt[b][:], ("yt", b))
                transposes(yt[b], ("yt", b), hT, ("hT",), tt)
            for fc in range(FC):
                b = fc % 2
                S.dma("pool", lambda e, fc=fc, b=b: e.dma_start(out=wgt[b][:], in_=wgv[:, :, fc * 128:(fc + 1) * 128]), writes=[("wgt", b)])
                S.dma("pool", lambda e, fc=fc, b=b: e.dma_start(out=wut[b][:], in_=wuv[:, :, fc * 128:(fc + 1) * 128]), writes=[("wut", b)])
                pg, pu = 4 + b, 6 + b
                for kc in range(16):
                    S.pe(lambda e, pg=pg, kc=kc, b=b: e.matmul(out=PS[pg][:, 0:512], lhsT=wgt[b][:, kc, :], rhs=hT[:, kc, :], start=(kc == 0), stop=(kc == 15)),
                         reads=[("wgt", b), ("hT",)], writes=[("ps", pg)])
                for kc in range(16):
                    S.pe(lambda e, pu=pu, kc=kc, b=b: e.matmul(out=PS[pu][:, 0:512], lhsT=wut[b][:, kc, :], rhs=hT[:, kc, :], start=(kc == 0), stop=(kc == 15)),
                         reads=[("wut", b), ("hT",)], writes=[("ps", pu)])
                S.act(lambda e, pg=pg, b=b: e.activation(out=sg[b][:], in_=PS[pg][:, 0:512], func=AF.Silu), reads=[("ps", pg)], writes=[("sg", b)])
                S.dve(lambda e, pu=pu, b=b, fc=fc: e.tensor_tensor(out=aT[:, fc, :], in0=sg[b][:], in1=PS[pu][:, 0:512], op=ALU.mult),
                      reads=[("sg", b), ("ps", pu)], writes=[("aT", fc)])
            for ct in range(8):
                S.dma("pool", lambda e, ct=ct: e.dma_start(out=wdt[0][:], in_=wdv[:, :, ct * 256:(ct + 1) * 256]), writes=[("wdt",)])
                for tt in range(4):
                    pb = 2 + it % 2
                    it += 1
                    for fc in range(FC):
                        S.pe(lambda e, pb=pb, fc=fc, tt=tt: e.matmul(out=PS[pb][:, 0:256], lhsT=aT[:, fc, tt * 128:(tt + 1) * 128], rhs=wdt[0][:, fc, :],
                                                                   start=(fc == 0), stop=(fc == FC - 1)),
                             reads=[("aT",), ("wdt",)], writes=[("ps", pb)])
                    S.dve(lambda e, pb=pb, tt=tt, ct=ct: e.tensor_tensor(out=x1[:, tt, ct * 256:(ct + 1) * 256], in0=x1[:, tt, ct * 256:(ct + 1) * 256],
                                                                       in1=PS[pb][:, 0:256], op=ALU.add),
                          reads=[("x1", tt), ("ps", pb)], writes=[("x1", tt)])
            for tt in range(4):
                r0 = s0 + tt * 128
                b = tt % 2
                if FINAL:
                    rms(x1[:, tt, :], ("x1", tt), fnB, ("fnB",), yt[b][:], ("yt", b))
                    S.dma("sp", lambda e, r0=r0, b=b: e.dma_start(out=xo[r0:r0 + 128, :], in_=yt[b][:]), reads=[("yt", b)])
                else:
                    S.dma("sp", lambda e, r0=r0, tt=tt: e.dma_start(out=xo[r0:r0 + 128, :], in_=x1[:, tt, :]), reads=[("x1", tt)])
        S.emit()
    return nc


_OFF = dict(mq=0, mk=512, mv=1024, mo=1536, mi=2048, mf=2052, fq=2056, fk=3080, fv=4104, ff=5128, gq=5136, gk=5648, gv=6160, gz=6672, ga=7184, gb=7188)


def _wslice(w_in, g):
    c = lambda n, h: list(range(_OFF[n] + 128 * h, _OFF[n] + 128 * (h + 1)))
    cols = (c("mq", g) + c("mk", g) + c("fq", 2 * g) + c("fq", 2 * g + 1) + c("fk", 2 * g) + c("fk", 2 * g + 1) + c("gq", g) + c("gk", g) + c("gv", g)
            + c("mk", g) + c("mv", g) + c("mo", g) + c("fv", 2 * g) + c("fv", 2 * g + 1) + c("gz", g)
            + [_OFF["mi"] + g, _OFF["mf"] + g, _OFF["ff"] + 2 * g, _OFF["ff"] + 2 * g + 1, _OFF["ga"] + g, _OFF["gb"] + g])
    out = np.zeros((D_MODEL, 2048), np.float32)
    out[:, :len(cols)] = w_in[:, cols]
    return out


_CACHE = {}


def kernel_unfused(x, mix_norm_w, w_in, mlstm_i_bias, mlstm_f_bias, fox_f_bias, gdn_conv_w, gdn_a_log, gdn_dt_bias,
           mlstm_out_norm_w, fox_out_norm_w, gdn_out_norm_w, w_out, ffn_norm_w, w_gate, w_up, w_down, final_norm_w):
    f = lambda a: np.ascontiguousarray(np.asarray(a, dtype=np.float32))
    x = f(x)
    Bsz, SEQ, D = x.shape
    cur = x.reshape(Bsz * SEQ, D)
    consts = consts_np()
    if "A" not in _CACHE:
        _CACHE["A"] = build_A(SEQ // 128)
    depth = np.asarray(w_in).shape[0]
    for l in range(depth):
        in_maps = []
        for c in range(8):
            b, g = c // 4, c % 4
            sl = lambda a, h: f(a)[l][128 * h:128 * (h + 1)]
            cw = f(gdn_conv_w)[l]
            in_maps.append({
                "x": np.ascontiguousarray(cur[b * SEQ:(b + 1) * SEQ]),
                "nw": f(mix_norm_w)[l],
                "w": _wslice(f(w_in)[l], g),
                "params": np.array([f(mlstm_i_bias)[l][g], f(mlstm_f_bias)[l][g], f(fox_f_bias)[l][2 * g], f(fox_f_bias)[l][2 * g + 1],
                                    f(gdn_a_log)[l][g], f(gdn_dt_bias)[l][g], 0, 0], np.float32),
                "normw": np.concatenate([sl(mlstm_out_norm_w, g), sl(fox_out_norm_w, 2 * g), sl(fox_out_norm_w, 2 * g + 1), sl(gdn_out_norm_w, g)]),
                "convw": np.ascontiguousarray(np.stack([cw[:, t * 512 + g * 128:t * 512 + (g + 1) * 128].T for t in range(3)], 1).reshape(128, 12)),
                "consts": consts,
            })
        res = run_bass_kernel_spmd(_CACHE["A"], in_maps, core_ids=list(range(8)))
        y = np.zeros((Bsz * SEQ, D), np.float32)
        for c in range(8):
            b, g = c // 4, c % 4
            yt = res.results[c]["ytok"]
            rows = slice(b * SEQ, (b + 1) * SEQ)
            y[rows, g * 128:(g + 1) * 128] = yt[:, 0:128]
            y[rows, 512 + g * 256:512 + (g + 1) * 256] = yt[:, 128:384]
            y[rows, 1536 + g * 128:1536 + (g + 1) * 128] = yt[:, 384:512]
        final = (l == depth - 1)
        key = "B%d" % int(final)
        if key not in _CACHE:
            _CACHE[key] = build_B(Bsz * SEQ, FINAL=final)
        resb = run_bass_kernel_spmd(_CACHE[key], [{
            "x": np.ascontiguousarray(cur), "y": y, "wo": f(w_out)[l], "wg": f(w_gate)[l], "wu": f(w_up)[l], "wd": f(w_down)[l],
            "nw2": f(ffn_norm_w)[l], "fnw": f(final_norm_w), "ident": np.eye(128, dtype=np.float32)}], core_ids=[0])
        cur = resb.results[0]["xo"]
    return np.ascontiguousarray(cur.reshape(Bsz, SEQ, D).astype(np.float32))


def allreduce_chunk(S, dr, k):
    rows = slice(k * 512, (k + 1) * 512)
    S.cc(lambda e: e.collective_compute("AllReduce", ALU.add, replica_groups=G4, ins=[dr["part"][rows, :]], outs=[dr["sumb"][rows, :]]),
         reads=[("part_d", 4 * k + j) for j in range(4)], writes=[("sumb", k)])


def wo_phase(S, A, NB, R, PS, wo_l, dr):
    B = R["big"]
    ident = R["consts"][:, 0, :]
    A.begin("wo")
    yT = [A.sb([128, 4, 128], BF16) for _ in range(2)]
    A.end()
    hst, ss, rs, stf, stb, stt_ = R["pre_tiles"]
    wob = B.t[0][:, 0:4096].bitcast(BF16).rearrange("p (a b) -> p a b", a=4)
    S.dma("pool", lambda e: e.dma_start(out=wob, in_=wo_l.rearrange("(kc p) n -> p kc n", p=128)), writes=[("B", 0)])
    it = 0
    for tt in range(NB):
        b = tt % 2
        yt = B.t[1 + b][:, 0:512]
        S.dma("sp", lambda e, tt=tt, yt=yt: e.dma_start(out=yt, in_=dr["ytok_d"][tt * 128:(tt + 1) * 128, :]), reads=[("ytok_d",)], writes=[("B", 1 + b)])
        for j in range(4):
            S.pe(lambda e, j=j, yt=yt, b=b: e.transpose(out=PS[b][:, j * 128:(j + 1) * 128], in_=yt[:, j * 128:(j + 1) * 128], identity=ident),
                 reads=[("B", 1 + b), ("consts",)], writes=[("ps", b)])
        S.act(lambda e, b=b: e.copy(out=yT[b][:], in_=PS[b][:, 0:512].rearrange("p (a b) -> p a b", a=4)), reads=[("ps", b)], writes=[("yT", b)])
        for ct in range(4):
            pb = 2 + it % 4
            st_ = stf[it % 2]
            for kc in range(4):
                S.pe(lambda e, pb=pb, kc=kc, ct=ct, b=b: e.matmul(out=PS[pb][:, 0:512], lhsT=yT[b][:, kc, :], rhs=wob[:, kc, ct * 512:(ct + 1) * 512],
                                                                start=(kc == 0), stop=(kc == 3)),
                     reads=[("yT", b), ("B", 0)], writes=[("ps", pb)])
            if it % 2 == 0:
                S.act(lambda e, pb=pb, st_=st_: e.copy(out=st_[:], in_=PS[pb][:, 0:512]), reads=[("ps", pb)], writes=[("stf", it % 2)])
            else:
                S.dve(lambda e, pb=pb, st_=st_: e.tensor_copy(out=st_[:], in_=PS[pb][:, 0:512]), reads=[("ps", pb)], writes=[("stf", it % 2)])
            S.dma("sp", lambda e, st_=st_, tt=tt, ct=ct: e.dma_start(out=dr["part"][tt * 128:(tt + 1) * 128, ct * 512:(ct + 1) * 512], in_=st_[:]),
                  reads=[("stf", it % 2)], writes=[("part_d", tt, ct)])
            it += 1
        if tt % 4 == 3:
            allreduce_chunk(S, dr, tt // 4)


def ffn_precast(S, wg_l, wu_l, wd_l, dr):
    for (src, dst, key) in ((wg_l, dr["wg_bf"], "wg_bf"), (wu_l, dr["wu_bf"], "wu_bf"), (wd_l, dr["wd_bf"], "wd_bf")):
        S.dma("pool", lambda e, src=src, dst=dst: e.dma_start(out=dst, in_=src), reads=[(key,)], writes=[(key,)])


def ffn_phase(S, A, NB, R, PS, xsrc, xkey, xdst, nw2_l, wg_l, wu_l, wd_l, dr, final):
    B = R["big"]
    ident = R["consts"][:, 0, :]
    FS = wg_l.shape[1]
    FCN = FS // 128
    hst, ss, rs, stf, stb, stt_ = R["pre_tiles"]
    A.begin("ffn")
    sg = [A.sb([128, 512], BF16) for _ in range(2)]
    A.end()
    nwB = B.t[8][:, 0:2048]
    S.dma("sp", lambda e: e.dma_start(out=nwB, in_=nw2_l.partition_broadcast(128)), reads=[("nwB",)], writes=[("B", 8, 0), ("nwB",)])
    wgv = dr["wg_bf"].rearrange("(kc p) n -> p kc n", p=128)
    wuv = dr["wu_bf"].rearrange("(kc p) n -> p kc n", p=128)
    wdv = dr["wd_bf"].rearrange("(fc p) n -> p fc n", p=128)
    NH = 2 if NB >= 8 else 1
    hT = [B.t[h][:, 0:4096].bitcast(BF16).rearrange("p (a b) -> p a b", a=16) for h in range(2)]
    aT = [B.t[2 + h][:, 0:FCN * 256].bitcast(BF16).rearrange("p (a b) -> p a b", a=FCN) for h in range(2)]
    wgt = [B.t[4][:, 1024 * i:1024 * (i + 1)].bitcast(BF16).rearrange("p (a b) -> p a b", a=16) for i in range(2)]
    wut = [B.t[4][:, 2048 + 1024 * i:2048 + 1024 * (i + 1)].bitcast(BF16).rearrange("p (a b) -> p a b", a=16) for i in range(2)]
    wdt = [B.t[5 + i][:, 0:FCN * 256].bitcast(BF16).rearrange("p (a b) -> p a b", a=FCN) for i in range(2)]
    xts = [B.t[7][:, 0:2048], B.t[8][:, 2048:4096]]
    xks = [("B", 7, 0), ("B", 8, 1)]
    hf = B.t[7][:, 2048:4096]
    hfk = ("B", 7, 1)
    ev = 0
    it = 0
    hfs = [B.t[5][:, 0:2048], B.t[6][:, 0:2048]]
    hfks = [("B", 5), ("B", 6)]

    def x1_a(h, t4, tt):
        b = tt % 2
        xt, xk = xts[b], xks[b]
        hf_, hfk_ = hfs[b], hfks[b]
        ssb, rsb = ss[b], rs[b]
        rows = slice(tt * 128, (tt + 1) * 128)
        S.dma("sp", lambda e: e.dma_start(out=xt, in_=xsrc[rows, :]), reads=([xkey] if xkey else []), writes=[xk])
        for hc in range(4):
            S.dma("sp", lambda e, hc=hc: e.dma_start(out=stf[hc % 2][:], in_=dr["sumb"][rows, hc * 512:(hc + 1) * 512]),
                  reads=[("sumb", tt // 4)], writes=[("stf", hc % 2)])
            S.dve(lambda e, hc=hc: e.tensor_tensor(out=xt[:, hc * 512:(hc + 1) * 512], in0=xt[:, hc * 512:(hc + 1) * 512], in1=stf[hc % 2][:], op=ALU.add),
                  reads=[xk, ("stf", hc % 2)], writes=[xk])
        S.dma("sp", lambda e: e.dma_start(out=xdst[rows, :], in_=xt), reads=[xk], writes=[("xb_d", tt)])
        S.act(lambda e: e.activation(out=hf_, in_=xt, func=AF.Square, accum_out=ssb[:]), reads=[xk], writes=[hfk_, ("ss", b)])
        S.dve(lambda e: e.tensor_scalar(out=rsb[:], in0=ssb[:], scalar1=1.0 / D_MODEL, scalar2=EPS, op0=ALU.mult, op1=ALU.add), reads=[("ss", b)], writes=[("rs", b)])
        S.act(lambda e: e.activation(out=rsb[:], in_=rsb[:], func=AF.Sqrt), reads=[("rs", b)], writes=[("rs", b)])
        S.dve(lambda e: e.reciprocal(out=rsb[:], in_=rsb[:]), reads=[("rs", b)], writes=[("rs", b)])
        S.dve(lambda e: e.scalar_tensor_tensor(out=hf_, in0=xt, scalar=rsb[:], in1=nwB, op0=ALU.mult, op1=ALU.mult),
              reads=[xk, ("rs", b), ("nwB",)], writes=[hfk_])

    def x1_b(h, t4, tt):
        b = tt % 2
        hf_, hfk_ = hfs[b], hfks[b]
        for g in range(4):
            pb = g % 2
            for j in range(4):
                kc = g * 4 + j
                S.pe(lambda e, pb=pb, j=j, kc=kc: e.transpose(out=PS[pb][:, j * 128:(j + 1) * 128], in_=hf_[:, kc * 128:(kc + 1) * 128], identity=ident),
                     reads=[hfk_, ("consts",)], writes=[("ps", pb)])
            s_ = PS[pb][:, 0:512].rearrange("p (a b) -> p a b", a=4)
            d_ = hT[h][:, g * 4:(g + 1) * 4, t4 * 128:(t4 + 1) * 128]
            if evc[0] % 2 == 0:
                S.act(lambda e, s_=s_, d_=d_: e.copy(out=d_, in_=s_), reads=[("ps", pb)], writes=[("B", h, t4, g)])
            else:
                S.dve(lambda e, s_=s_, d_=d_: e.tensor_copy(out=d_, in_=s_), reads=[("ps", pb)], writes=[("B", h, t4, g)])
            evc[0] += 1
    evc = [0]
    for ss_ in range(NB // (4 * NH)):
        tiles = [(h, t4, (ss_ * NH + h) * 4 + t4) for h in range(NH) for t4 in range(4)]
        for i in range(len(tiles) + 1):
            if i < len(tiles):
                x1_a(*tiles[i])
            if i >= 1:
                x1_b(*tiles[i - 1])
        for fc in range(FCN):
            b = fc % 2
            S.dma("sp", lambda e, fc=fc, b=b: e.dma_start(out=wgt[b], in_=wgv[:, :, fc * 128:(fc + 1) * 128]), reads=[("wg_bf",)], writes=[("B", 4, 0, b)])
            S.dma("sp", lambda e, fc=fc, b=b: e.dma_start(out=wut[b], in_=wuv[:, :, fc * 128:(fc + 1) * 128]), reads=[("wu_bf",)], writes=[("B", 4, 1, b)])
            for h in range(NH):
                pg, pu = 2 + (fc * NH + h) % 2, 4 + (fc * NH + h) % 2
                sgi = (fc * NH + h) % 2
                for kc in range(16):
                    S.pe(lambda e, pg=pg, kc=kc, b=b, h=h: e.matmul(out=PS[pg][:, 0:512], lhsT=wgt[b][:, kc, :], rhs=hT[h][:, kc, :], start=(kc == 0), stop=(kc == 15)),
                         reads=[("B", 4, 0, b), ("B", h)], writes=[("ps", pg)])
                for kc in range(16):
                    S.pe(lambda e, pu=pu, kc=kc, b=b, h=h: e.matmul(out=PS[pu][:, 0:512], lhsT=wut[b][:, kc, :], rhs=hT[h][:, kc, :], start=(kc == 0), stop=(kc == 15)),
                         reads=[("B", 4, 1, b), ("B", h)], writes=[("ps", pu)])
                S.act(lambda e, pg=pg, sgi=sgi: e.activation(out=sg[sgi][:], in_=PS[pg][:, 0:512], func=AF.Silu), reads=[("ps", pg)], writes=[("sg", sgi)])
                S.dve(lambda e, pu=pu, sgi=sgi, fc=fc, h=h: e.tensor_tensor(out=aT[h][:, fc, :], in0=sg[sgi][:], in1=PS[pu][:, 0:512], op=ALU.mult),
                      reads=[("sg", sgi), ("ps", pu)], writes=[("B", 2 + h, fc)])
        for ct in range(4):
            wb = ct % 2
            S.dma("sp", lambda e, ct=ct, wb=wb: e.dma_start(out=wdt[wb], in_=wdv[:, :, ct * 512:(ct + 1) * 512]), reads=[("wd_bf",)], writes=[("B", 5 + wb)])
            for h in range(NH):
                sl = ss_ * NH + h
                for t4 in range(4):
                    tt = sl * 4 + t4
                    rows = slice(tt * 128, (tt + 1) * 128)
                    pb = 6 + it % 2
                    st_ = stt_[it % 2]
                    for fc in range(FCN):
                        S.pe(lambda e, pb=pb, fc=fc, t4=t4, wb=wb, h=h: e.matmul(out=PS[pb][:, 0:512], lhsT=aT[h][:, fc, t4 * 128:(t4 + 1) * 128], rhs=wdt[wb][:, fc, :],
                                                                               start=(fc == 0), stop=(fc == FCN - 1)),
                             reads=[("B", 2 + h), ("B", 5 + wb)], writes=[("ps", pb)])
                    if final:
                        S.dma("sp", lambda e, st_=st_, rows=rows, ct=ct: e.dma_start(out=st_[:, 0:512], in_=xdst[rows, ct * 512:(ct + 1) * 512]),
                              reads=[("xb_d", tt)], writes=[("stt", it % 2)])
                        S.dve(lambda e, st_=st_, pb=pb: e.scalar_tensor_tensor(out=st_[:, 0:512], in0=st_[:, 0:512], scalar=R["parB"][:, 6:7], in1=PS[pb][:, 0:512],
                                                                             op0=ALU.mult, op1=ALU.add),
                              reads=[("stt", it % 2), ("parB",), ("ps", pb)], writes=[("stt", it % 2)])
                    elif it % 2 == 0:
                        S.act(lambda e, st_=st_, pb=pb: e.copy(out=st_[:, 0:512], in_=PS[pb][:, 0:512]), reads=[("ps", pb)], writes=[("stt", it % 2)])
                    else:
                        S.dve(lambda e, st_=st_, pb=pb: e.tensor_copy(out=st_[:, 0:512], in_=PS[pb][:, 0:512]), reads=[("ps", pb)], writes=[("stt", it % 2)])
                    if final:
                        TPB = NB // 4
                        pb_ = (tt % TPB) * 4 + tt // TPB
                        prows = slice(pb_ * 128, (pb_ + 1) * 128)
                    else:
                        pb_ = tt
                        prows = rows
                    S.dma("sp", lambda e, st_=st_, prows=prows, ct=ct: e.dma_start(out=dr["part"][prows, ct * 512:(ct + 1) * 512], in_=st_[:, 0:512]),
                          reads=[("stt", it % 2)], writes=[("part_d", pb_, ct)])
                    it += 1
        if not final:
            for h in range(NH):
                allreduce_chunk(S, dr, ss_ * NH + h)
    if final:
        for k in range(NB // 4):
            S.cc(lambda e, k=k: e.collective_compute("ReduceScatter", ALU.add, replica_groups=G4, ins=[dr["part"][k * 512:(k + 1) * 512, :]],
                                                     outs=[dr["rsout"][k * 128:(k + 1) * 128, :]]),
                 reads=[("part_d", 4 * k + j) for j in range(4)], writes=[("rsout", k)])


def build_fused(NB=32, DEPTH=2, DEBUG=False):
    nc = bass.Bass("TRN2", target_bir_lowering=False)
    SEQ = NB * 128
    D = D_MODEL
    FS = D_FF // 4
    ein = lambda name, shape: nc.dram_tensor(name, list(shape), F32, kind="ExternalInput").ap()
    x = ein("x", [SEQ, D])
    nw = ein("nw", [DEPTH, D])
    w = ein("w", [DEPTH, D, 2048])
    params = ein("params", [DEPTH, 8])
    normw_d = ein("normw", [DEPTH, 512])
    convw_d = ein("convw", [DEPTH, 128, 12])
    consts_d = ein("consts", [128, 5, 128])
    wo = ein("wo", [DEPTH, 512, D])
    nw2 = ein("nw2", [DEPTH, D])
    wg = ein("wg", [DEPTH, D, FS])
    wu = ein("wu", [DEPTH, D, FS])
    wd = ein("wd", [DEPTH, FS, D])
    fnw = ein("fnw", [D])
    out = nc.dram_tensor("out", [SEQ // 4, D], F32, kind="ExternalOutput").ap()
    it_ = lambda name, shape, dt=F32, **kw: nc.dram_tensor(name, list(shape), dt, kind="Internal", **kw).ap()
    dr = {}
    dr["hTd"] = it_("hTd", [16, 128, SEQ], BF16)
    dr["m_qT"] = it_("m_qT", [128, SEQ])
    dr["m_kT"] = it_("m_kT", [128, SEQ])
    dr["tokm"] = it_("tokm", [SEQ, 768])
    dr["f_qkT"] = it_("f_qkT", [4, 128, SEQ], BF16)
    dr["g_T"] = it_("g_T", [3, 128, SEQ])
    dr["ytok_d"] = it_("ytok_d", [SEQ, 512])
    dr["part"] = it_("part", [SEQ, D])
    dr["sumb"] = it_("sumb", [SEQ, D], addr_space="Local")
    dr["rsout"] = it_("rsout", [SEQ // 4, D], addr_space="Local")
    dr["xa"] = it_("xa", [SEQ, D])
    dr["xb"] = it_("xb", [SEQ, D])
    dr["wg_bf"] = it_("wg_bf", [D, FS], BF16)
    dr["wu_bf"] = it_("wu_bf", [D, FS], BF16)
    dr["wd_bf"] = it_("wd_bf", [FS, D], BF16)
    S = Sched(nc)
    with contextlib.ExitStack() as st:
        A = Alloc(nc, st)
        R = {}
        R["consts"] = A.sb([128, 5, 128], F32)
        R["parB"] = A.sb([128, 8], F32)
        R["gates"] = A.sb([128, NB, 6], F32)
        R["normw"] = A.sb([128, 4, 128], F32)
        R["convw"] = A.sb([128, 12], F32)
        R["big"] = Big(A, 9)
        R["pre_tiles"] = pre_tiles(A)
        PS = [A.ps([128, 512], F32) for _ in range(8)]
        B = R["big"]
        S.dma("sp", lambda e: e.dma_start(out=R["consts"][:], in_=consts_d), writes=[("consts",)])
        for l in range(DEPTH):
            final = (l == DEPTH - 1)
            S.dma("sp", lambda e, l=l: e.dma_start(out=R["parB"][:], in_=params[l].partition_broadcast(128)), writes=[("parB",)])
            S.dma("sp", lambda e, l=l: e.dma_start(out=R["normw"][:].rearrange("p a b -> p (a b)"), in_=normw_d[l].partition_broadcast(128)), writes=[("normw",)])
            S.dma("sp", lambda e, l=l: e.dma_start(out=R["convw"][:], in_=convw_d[l]), writes=[("convw",)])
            ffn_precast(S, wg[l], wu[l], wd[l], dr)
            if l == 0:
                pre_phase(S, A, NB, R, PS, x, nw[l], w[l], dr)
                xcur, xcur_key = x, None
            else:
                pre_phase(S, A, NB, R, PS, dr["xb"], nw[l], w[l], dr, add_src=dr["sumb"], store_dst=dr["xa"], xkey=("xb_d",))
                xcur, xcur_key = dr["xa"], ("xa_d",)
            A.begin("mix")
            T = mixer_prelude(S, A, NB, R)
            mlstm_phase(S, A, NB, R, T, PS, dr, dr["ytok_d"])
            fox_phase(S, A, NB, R, T, PS, dr, dr["ytok_d"])
            gdn_phase(S, A, NB, R, T, PS, dr, dr["ytok_d"])
            A.end()
            wo_phase(S, A, NB, R, PS, wo[l], dr)
            ffn_phase(S, A, NB, R, PS, xcur, xcur_key, dr["xb"], nw2[l], wg[l], wu[l], wd[l], dr, final)
        hst, ss, rs, stf, stb, stt_ = R["pre_tiles"]
        fnB = B.t[8][:, 0:2048]
        S.dma("sp", lambda e: e.dma_start(out=fnB, in_=fnw.partition_broadcast(128)), reads=[("nwB",)], writes=[("B", 8), ("nwB",)])
        for tt in range(NB // 4):
            b = tt % 2
            xt = B.t[b][:, 0:2048]
            hf = B.t[2 + b][:, 0:2048]
            rows = slice(tt * 128, (tt + 1) * 128)
            S.dma("sp", lambda e, xt=xt, rows=rows: e.dma_start(out=xt, in_=dr["rsout"][rows, :]), reads=[("rsout", tt)], writes=[("B", b)])
            S.act(lambda e, xt=xt, hf=hf, b=b: e.activation(out=hf, in_=xt, func=AF.Square, accum_out=ss[b][:]), reads=[("B", b)], writes=[("B", 2 + b), ("ss", b)])
            S.dve(lambda e, b=b: e.tensor_scalar(out=rs[b][:], in0=ss[b][:], scalar1=1.0 / D, scalar2=EPS, op0=ALU.mult, op1=ALU.add), reads=[("ss", b)], writes=[("rs", b)])
            S.act(lambda e, b=b: e.activation(out=rs[b][:], in_=rs[b][:], func=AF.Sqrt), reads=[("rs", b)], writes=[("rs", b)])
            S.dve(lambda e, b=b: e.reciprocal(out=rs[b][:], in_=rs[b][:]), reads=[("rs", b)], writes=[("rs", b)])
            S.dve(lambda e, xt=xt, hf=hf, b=b: e.scalar_tensor_tensor(out=hf, in0=xt, scalar=rs[b][:], in1=fnB, op0=ALU.mult, op1=ALU.mult),
                  reads=[("B", b), ("rs", b), ("nwB",)], writes=[("B", 2 + b)])
            S.dma("sp", lambda e, hf=hf, rows=rows: e.dma_start(out=out[rows, :], in_=hf), reads=[("B", 2 + b)])
        if DEBUG:
            for name, src, key in (("dbg_sumb", dr["sumb"], ("sumb",)), ("dbg_xb", dr["xb"], ("xb_d",)), ("dbg_part", dr["part"], ("part_d",)),
                                   ("dbg_ytok", dr["ytok_d"], ("ytok_d",))):
                dst = nc.dram_tensor(name, list(src.shape), F32, kind="ExternalOutput").ap()
                S.dma("sp", lambda e, dst=dst, src=src: e.dma_start(out=dst, in_=src), reads=[key])
        S.emit()
    return nc


def _worows(g):
    return list(range(g * 128, (g + 1) * 128)) + list(range(512 + g * 256, 512 + (g + 1) * 256)) + list(range(1536 + g * 128, 1536 + (g + 1) * 128))


def make_in_maps(x, mix_norm_w, w_in, mlstm_i_bias, mlstm_f_bias, fox_f_bias, gdn_conv_w, gdn_a_log, gdn_dt_bias,
                 mlstm_out_norm_w, fox_out_norm_w, gdn_out_norm_w, w_out, ffn_norm_w, w_gate, w_up, w_down, final_norm_w):
    f = lambda a: np.ascontiguousarray(np.asarray(a, dtype=np.float32))
    x = f(x)
    Bsz, SEQ, D = x.shape
    depth = np.asarray(w_in).shape[0]
    FS = D_FF // 4
    consts = consts_np()
    w_in, w_out, w_gate, w_up, w_down, cwall = f(w_in), f(w_out), f(w_gate), f(w_up), f(w_down), f(gdn_conv_w)
    in_maps = []
    for c in range(8):
        b, g = c // 4, c % 4
        sl = lambda a, l, h: f(a)[l][128 * h:128 * (h + 1)]
        m = {"x": np.ascontiguousarray(x[b]), "nw": f(mix_norm_w), "consts": consts, "nw2": f(ffn_norm_w), "fnw": f(final_norm_w)}
        m["w"] = np.stack([_wslice(w_in[l], g) for l in range(depth)])
        m["params"] = np.stack([np.array([f(mlstm_i_bias)[l][g], f(mlstm_f_bias)[l][g], f(fox_f_bias)[l][2 * g], f(fox_f_bias)[l][2 * g + 1],
                                          f(gdn_a_log)[l][g], f(gdn_dt_bias)[l][g], 1.0 if g == 0 else 0.0, 0.0], np.float32) for l in range(depth)])
        m["normw"] = np.stack([np.concatenate([sl(mlstm_out_norm_w, l, g), sl(fox_out_norm_w, l, 2 * g), sl(fox_out_norm_w, l, 2 * g + 1),
                                               sl(gdn_out_norm_w, l, g)]) for l in range(depth)])
        m["convw"] = np.stack([np.ascontiguousarray(np.stack([cwall[l][:, t * 512 + g * 128:t * 512 + (g + 1) * 128].T for t in range(3)], 1).reshape(128, 12))
                               for l in range(depth)])
        m["wo"] = np.ascontiguousarray(w_out[:, _worows(g), :])
        m["wg"] = np.ascontiguousarray(w_gate[:, :, g * FS:(g + 1) * FS])
        m["wu"] = np.ascontiguousarray(w_up[:, :, g * FS:(g + 1) * FS])
        m["wd"] = np.ascontiguousarray(w_down[:, g * FS:(g + 1) * FS, :])
        in_maps.append(m)
    return in_maps, (Bsz, SEQ, D)


_DEBUG = [False, None]


def kernel(**inputs):
    in_maps, (Bsz, SEQ, D) = make_in_maps(**inputs)
    depth = in_maps[0]["w"].shape[0]
    key = ("F", SEQ, depth, _DEBUG[0])
    if key not in _CACHE:
        _CACHE[key] = build_fused(SEQ // 128, depth, DEBUG=_DEBUG[0])
    res = run_bass_kernel_spmd(_CACHE[key], in_maps, core_ids=list(range(8)))
    _DEBUG[1] = res
    out = np.zeros((Bsz, SEQ, D), np.float32)
    q = SEQ // 4
    for c in range(8):
        b, g = c // 4, c % 4
        out[b, g * q:(g + 1) * q] = res.results[c]["out"]
    return out
```
